# Optimizing a Trainium2 kernel written in Bass

```python
import jax, jax.numpy as jnp
from jax import lax
import numpy as np

D_MODEL = 1024
BATCH = 4
SEQ = 8192
DEPTH = 2

F32 = jnp.float32
CHUNK = 64
MIX_HALF = D_MODEL // 2
A_HEADS = 4
A_DK = MIX_HALF // A_HEADS
A_DV = MIX_HALF // A_HEADS
B_HEADS = 4
B_DK = MIX_HALF // (2 * B_HEADS)
B_DV = MIX_HALF // B_HEADS
GLA_LOWRANK = 16
GLA_GATE_NORMALIZER = 16.0
C_HEADS = 4
C_DK = MIX_HALF // (2 * C_HEADS)
C_DV = MIX_HALF // C_HEADS
ROPE_THETA = 10000.0
D_HEADS = 4
D_DK = MIX_HALF // D_HEADS
D_DV = MIX_HALF // D_HEADS
CONV_WIDTH = 3
N_EXPERTS = 16
N_GROUPS = 4
EXPERTS_PER_GROUP = N_EXPERTS // N_GROUPS
TOP_K = 2
D_EXPERT = D_MODEL
DISPATCH_BLOCK = 128
ALPHA = (2.0 * DEPTH) ** 0.25
BETA = (8.0 * DEPTH) ** -0.25
N_AB = (DEPTH + 1) // 2
N_CD = DEPTH // 2
LN_EPS = 1e-5
NORM_EPS = 1e-6

AB_SPLITS = (A_HEADS * A_DK, A_HEADS * A_DK, A_HEADS * A_DK, A_HEADS * A_DV, A_HEADS * A_DV,
             B_HEADS * B_DK, B_HEADS * B_DK, B_HEADS * B_DV, B_HEADS * B_DV, GLA_LOWRANK, GLA_LOWRANK)
AB_VALUE_COLS = (3, 7)
CD_SPLITS = (C_HEADS * C_DK, C_HEADS * C_DK, C_HEADS * C_DV, C_HEADS * C_DV,
             2 * D_HEADS * D_DK, D_HEADS * D_DV, D_HEADS * D_DV, 4 * D_HEADS)
CD_VALUE_COLS = (2, 5)
P_AB = sum(AB_SPLITS)
P_CD = sum(CD_SPLITS)
MIX_AB = A_HEADS * A_DV + B_HEADS * B_DV
MIX_CD = C_HEADS * C_DV + D_HEADS * D_DV

kernel_name = 'bidir_hybrid_hgrn2_gla_retnet_mlstm_moe'


def _split(p, sizes):
    return jnp.split(p, [int(c) for c in np.cumsum(sizes)[:-1]], axis=-1)


def _heads(a, n_heads):
    b, t, w = a.shape
    return a.reshape(b, t, n_heads, w // n_heads).transpose(0, 2, 1, 3)


def _merge_heads(a):
    b, h, t, d = a.shape
    return a.transpose(0, 2, 1, 3).reshape(b, t, h * d)


def _flip(a):
    return jnp.flip(a, axis=2)


def _chunk(a):
    b, h, t = a.shape[:3]
    return jnp.moveaxis(a.reshape(b, h, t // CHUNK, CHUNK, *a.shape[3:]), 2, 0)


def _unchunk(a):
    n, b, h, l = a.shape[:4]
    return jnp.moveaxis(a, 0, 2).reshape(b, h, n * l, *a.shape[4:])


def _lower_mask():
    return jnp.tril(jnp.ones((CHUNK, CHUNK), dtype=bool))


def _head_norm(o, gain):
    o = o * lax.rsqrt(jnp.mean(o * o, axis=-1, keepdims=True) + NORM_EPS)
    return _merge_heads(o) * gain.astype(F32)


def _layer_norm(x, gain, bias):
    xf = x.astype(F32)
    xc = xf - jnp.mean(xf, axis=-1, keepdims=True)
    var = jnp.mean(xc * xc, axis=-1, keepdims=True)
    return (xc * lax.rsqrt(var + LN_EPS) * gain.astype(F32) + bias.astype(F32)).astype(x.dtype)


def _rotary(a):
    t, d = a.shape[2], a.shape[3]
    half = d // 2
    freqs = ROPE_THETA ** (-jnp.arange(half, dtype=F32) / half)
    ang = jnp.arange(t, dtype=F32)[:, None] * freqs[None, :]
    cos, sin = jnp.cos(ang), jnp.sin(ang)
    a1, a2 = a[..., :half], a[..., half:]
    return jnp.concatenate([a1 * cos - a2 * sin, a1 * sin + a2 * cos], axis=-1)


def _conv_centred(a, w, b):
    c = a.shape[-1]
    out = lax.conv_general_dilated(a, w.astype(a.dtype)[:, None, :], window_strides=(1,), padding='SAME',
                                   dimension_numbers=('NWC', 'WIO', 'NWC'), feature_group_count=c)
    return out + b.astype(a.dtype)


def _gated_chunk_scan(q, k, v, log_f):
    b, h, _, dk = q.shape
    dv = v.shape[-1]
    qc, kc, vc, gc = _chunk(q), _chunk(k), _chunk(v), _chunk(log_f)
    g_cum = jnp.cumsum(gc, axis=-2)
    g_ref = g_cum[..., CHUNK // 2 - 1:CHUNK // 2, :]
    scores = jnp.einsum('nbhid,nbhjd->nbhij', qc * jnp.exp(g_cum - g_ref), kc * jnp.exp(g_ref - g_cum))
    scores = jnp.where(_lower_mask(), scores, 0.0)
    o_intra = jnp.einsum('nbhij,nbhjv->nbhiv', scores, vc)
    q_inter = qc * jnp.exp(g_cum)
    k_state = kc * jnp.exp(g_cum[..., -1:, :] - g_cum)
    chunk_decay = jnp.exp(g_cum[..., -1, :])

    def step(state, xs):
        qn, kn, vn, dn = xs
        out = jnp.einsum('bhid,bhdv->bhiv', qn, state)
        state = dn[..., None] * state + jnp.einsum('bhjd,bhjv->bhdv', kn, vn)
        return state, out

    state0 = jnp.zeros((b, h, dk, dv), F32)
    _, o_inter = lax.scan(step, state0, (q_inter, k_state, vc, chunk_decay))
    return _unchunk(o_intra + o_inter)


def _retention_log_decay(direction):
    exps = 5.0 + 2.0 * jnp.arange(C_HEADS, dtype=F32) + direction
    return jnp.log1p(-jnp.exp2(-exps))


def _retention_chunk_scan(q, k, v, log_gamma):
    b, h, _, dk = q.shape
    dv = v.shape[-1]
    qc, kc, vc = _chunk(q), _chunk(k), _chunk(v)
    pos = jnp.arange(CHUNK, dtype=F32)
    lg = log_gamma[:, None, None]
    dist = pos[:, None] - pos[None, :]
    decay_intra = jnp.where(_lower_mask(), jnp.exp(jnp.maximum(dist, 0.0)[None] * lg), 0.0)
    scores = jnp.einsum('nbhid,nbhjd->nbhij', qc, kc) * decay_intra
    o_intra = jnp.einsum('nbhij,nbhjv->nbhiv', scores, vc)
    q_inter = qc * jnp.exp((pos + 1.0)[None, :, None] * lg)
    k_state = kc * jnp.exp((CHUNK - 1.0 - pos)[None, :, None] * lg)
    chunk_decay = jnp.exp(CHUNK * log_gamma)[:, None, None]

    def step(state, xs):
        qn, kn, vn = xs
        out = jnp.einsum('bhid,bhdv->bhiv', qn, state)
        state = chunk_decay * state + jnp.einsum('bhjd,bhjv->bhdv', kn, vn)
        return state, out

    state0 = jnp.zeros((b, h, dk, dv), F32)
    _, o_inter = lax.scan(step, state0, (q_inter, k_state, vc))
    return _unchunk(o_intra + o_inter)


def _mlstm_chunk_scan(q, k, v, log_i, log_f):
    b, h, _, dk = q.shape
    dv = v.shape[-1]
    qc, kc, vc = _chunk(q), _chunk(k), _chunk(v)
    ic, fc = _chunk(log_i), _chunk(log_f)
    b_cum = jnp.cumsum(fc, axis=-1)
    log_d = jnp.where(_lower_mask(), b_cum[..., :, None] - b_cum[..., None, :] + ic[..., None, :], -jnp.inf)
    m_intra = jnp.max(log_d, axis=-1)
    qk = jnp.einsum('nbhid,nbhjd->nbhij', qc, kc)
    log_w = b_cum[..., -1:] - b_cum + ic

    def step(carry, xs):
        c_st, n_st, m_st = carry
        qn, kn, vn, bn, ldn, min_, qkn, lwn = xs
        a = bn + m_st[..., None]
        m_i = jnp.maximum(a, min_)
        w = qkn * jnp.exp(ldn - m_i[..., None])
        s_inter = jnp.exp(a - m_i)
        num = s_inter[..., None] * jnp.einsum('bhid,bhdv->bhiv', qn, c_st) + jnp.einsum('bhij,bhjv->bhiv', w, vn)
        den = s_inter * jnp.einsum('bhid,bhd->bhi', qn, n_st) + jnp.sum(w, axis=-1)
        h_out = num / jnp.maximum(jnp.abs(den), jnp.exp(-m_i))[..., None]
        m_new = jnp.maximum(bn[..., -1] + m_st, jnp.max(lwn, axis=-1))
        s_old = jnp.exp(bn[..., -1] + m_st - m_new)
        wk = kn * jnp.exp(lwn - m_new[..., None])[..., None]
        c_st = s_old[..., None, None] * c_st + jnp.einsum('bhjd,bhjv->bhdv', wk, vn)
        n_st = s_old[..., None] * n_st + jnp.sum(wk, axis=-2)
        return (c_st, n_st, m_new), h_out

    carry0 = (jnp.zeros((b, h, dk, dv), F32), jnp.zeros((b, h, dk), F32), jnp.zeros((b, h), F32))
    _, h_all = lax.scan(step, carry0, (qc, kc, vc, b_cum, log_d, m_intra, qk, log_w))
    return _unchunk(h_all)


def _mixer_ab(x, w_in, b_in, lb, gk_up, gk_b, norm_a, norm_b, w_out):
    p = (x @ w_in + b_in).astype(F32)
    qa_r, fa_fwd, fa_bwd, ia_r, ga_r, qb_r, kb_r, vb_r, gb_r, rb_fwd, rb_bwd = _split(p, AB_SPLITS)
    q_a = _heads(jax.nn.silu(qa_r), A_HEADS)
    v_a = _heads(ia_r, A_HEADS)

    def hgrn_gates(f_logit):
        f = lb + (1.0 - lb) * jax.nn.sigmoid(f_logit)
        return _heads(1.0 - f, A_HEADS), _heads(jnp.log(f), A_HEADS)

    k_af, lf_af = hgrn_gates(fa_fwd)
    k_ab, lf_ab = hgrn_gates(fa_bwd)
    o_a = (_gated_chunk_scan(q_a, k_af, v_a, lf_af)
           + _flip(_gated_chunk_scan(_flip(q_a), _flip(k_ab), _flip(v_a), _flip(lf_ab))))
    o_a = _head_norm(o_a, norm_a) * jax.nn.silu(ga_r)
    q_b = _heads(qb_r, B_HEADS) * (B_DK ** -0.5)
    k_b = _heads(kb_r, B_HEADS)
    v_b = _heads(vb_r, B_HEADS)
    lf_bf = _heads(jax.nn.log_sigmoid(rb_fwd @ gk_up[0] + gk_b[0]) / GLA_GATE_NORMALIZER, B_HEADS)
    lf_bb = _heads(jax.nn.log_sigmoid(rb_bwd @ gk_up[1] + gk_b[1]) / GLA_GATE_NORMALIZER, B_HEADS)
    o_b = (_gated_chunk_scan(q_b, k_b, v_b, lf_bf)
           + _flip(_gated_chunk_scan(_flip(q_b), _flip(k_b), _flip(v_b), _flip(lf_bb))))
    o_b = _head_norm(o_b, norm_b) * jax.nn.silu(gb_r)
    mixed = jnp.concatenate([o_a, o_b], axis=-1).astype(x.dtype)
    return mixed @ w_out


def _mixer_cd(x, w_in, b_in, conv_w, conv_b, fgate_b, norm_c, norm_d, w_out):
    p = (x @ w_in + b_in).astype(F32)
    qc_r, kc_r, vc_r, gc_r, qkd_r, vd_r, od_r, gates = _split(p, CD_SPLITS)
    q_c = _rotary(_heads(qc_r, C_HEADS)) * (C_DK ** -0.5)
    k_c = _rotary(_heads(kc_r, C_HEADS))
    v_c = _heads(vc_r, C_HEADS)
    o_c = (_retention_chunk_scan(q_c, k_c, v_c, _retention_log_decay(0.0))
           + _flip(_retention_chunk_scan(_flip(q_c), _flip(k_c), _flip(v_c), _retention_log_decay(1.0))))
    o_c = _head_norm(o_c, norm_c) * jax.nn.silu(gc_r)
    qk_d = jax.nn.silu(_conv_centred(qkd_r, conv_w, conv_b))
    qd_r, kd_r = jnp.split(qk_d, 2, axis=-1)
    q_d = _heads(qd_r, D_HEADS) * (D_DK ** -0.5)
    k_d = _heads(kd_r, D_HEADS)
    v_d = _heads(vd_r, D_HEADS)
    gates = gates.transpose(0, 2, 1)
    li_f, lf_f_raw, li_b, lf_b_raw = jnp.split(gates, 4, axis=1)
    lf_f = jax.nn.log_sigmoid(lf_f_raw + fgate_b[0][:, None])
    lf_b = jax.nn.log_sigmoid(lf_b_raw + fgate_b[1][:, None])
    h_d = (_mlstm_chunk_scan(q_d, k_d, v_d, li_f, lf_f)
           + _flip(_mlstm_chunk_scan(_flip(q_d), _flip(k_d), _flip(v_d), _flip(li_b), _flip(lf_b))))
    o_d = _head_norm(h_d, norm_d) * jax.nn.sigmoid(od_r)
    mixed = jnp.concatenate([o_c, o_d], axis=-1).astype(x.dtype)
    return mixed @ w_out


def _route(xt, router_w, router_b):
    scores = jax.nn.softmax((xt @ router_w).astype(F32), axis=-1)
    biased = scores + router_b.astype(F32)
    per_group = biased.reshape(-1, N_GROUPS, EXPERTS_PER_GROUP)
    group_score = jnp.sum(lax.top_k(per_group, TOP_K)[0], axis=-1)
    g_sel = jnp.argmax(group_score, axis=-1)
    in_group = jnp.take_along_axis(per_group, g_sel[:, None, None], axis=1)[:, 0]
    _, local = lax.top_k(in_group, TOP_K)
    expert_idx = g_sel[:, None] * EXPERTS_PER_GROUP + local
    sel = jnp.take_along_axis(scores, expert_idx, axis=1)
    return expert_idx, sel / jnp.sum(sel, axis=-1, keepdims=True)


def _moe(x, router_w, router_b, w1, w3, w2):
    bsz, t, d = x.shape
    xt = x.reshape(-1, d)
    n_tok = xt.shape[0]
    expert_idx, gates = _route(xt, router_w, router_b)
    n_assign = n_tok * TOP_K
    flat_e = expert_idx.reshape(-1)
    flat_tok = jnp.repeat(jnp.arange(n_tok, dtype=jnp.int32), TOP_K)
    flat_g = gates.reshape(-1)
    order = jnp.argsort(flat_e)
    e_sorted = flat_e[order]
    counts = jnp.bincount(flat_e, length=N_EXPERTS)
    padded = (counts + DISPATCH_BLOCK - 1) // DISPATCH_BLOCK * DISPATCH_BLOCK
    start_sorted = jnp.cumsum(counts) - counts
    pad_end = jnp.cumsum(padded)
    start_padded = pad_end - padded
    dest = start_padded[e_sorted] + (jnp.arange(n_assign) - start_sorted[e_sorted])
    n_rows = -(-n_assign // DISPATCH_BLOCK) * DISPATCH_BLOCK + N_EXPERTS * DISPATCH_BLOCK
    n_blocks = n_rows // DISPATCH_BLOCK
    buf_tok = jnp.full((n_rows,), n_tok, jnp.int32).at[dest].set(flat_tok[order])
    buf_gate = jnp.zeros((n_rows,), F32).at[dest].set(flat_g[order])
    block_expert = jnp.minimum(jnp.searchsorted(pad_end, jnp.arange(n_blocks) * DISPATCH_BLOCK, side='right'),
                               N_EXPERTS - 1)
    x_pad = jnp.concatenate([xt, jnp.zeros((1, d), xt.dtype)], axis=0)

    def expert_block(args):
        e, tok = args
        xb = x_pad[tok]
        hid = jax.nn.silu(xb @ w1[e]) * (xb @ w3[e])
        return hid @ w2[e]

    y_blocks = lax.map(expert_block, (block_expert, buf_tok.reshape(n_blocks, DISPATCH_BLOCK)))
    y = y_blocks.reshape(n_rows, d).astype(F32) * buf_gate[:, None]
    out = jax.ops.segment_sum(y, buf_tok, num_segments=n_tok + 1)[:n_tok]
    return out.reshape(bsz, t, d).astype(x.dtype)


def setup_inputs(seed: int = 0) -> dict:
    key = jax.random.key(seed)
    ks = jax.random.split(key, 32)

    def nrm(k, shape, scale):
        return jax.random.normal(k, shape, F32) * scale

    ab_scale = np.concatenate([np.full((s,), BETA if i in AB_VALUE_COLS else 1.0, np.float32)
                               for i, s in enumerate(AB_SPLITS)])
    cd_scale = np.concatenate([np.full((s,), BETA if i in CD_VALUE_COLS else 1.0, np.float32)
                               for i, s in enumerate(CD_SPLITS)])
    return {
        'x': nrm(ks[0], (BATCH, SEQ, D_MODEL), 1.0),
        'ab_w_in': nrm(ks[1], (N_AB, D_MODEL, P_AB), D_MODEL ** -0.5) * jnp.asarray(ab_scale),
        'ab_b_in': nrm(ks[2], (N_AB, P_AB), 0.02),
        'hgrn_lb': nrm(ks[3], (DEPTH + 1, A_HEADS * A_DK), 0.1),
        'gla_gk_up': nrm(ks[4], (N_AB, 2, GLA_LOWRANK, B_HEADS * B_DK), GLA_LOWRANK ** -0.5),
        'gla_gk_b': nrm(ks[5], (N_AB, 2, B_HEADS * B_DK), 0.1),
        'hgrn_norm': 1.0 + nrm(ks[6], (N_AB, A_HEADS * A_DV), 0.02),
        'gla_norm': 1.0 + nrm(ks[7], (N_AB, B_HEADS * B_DV), 0.02),
        'ab_w_out': nrm(ks[8], (N_AB, MIX_AB, D_MODEL), MIX_AB ** -0.5 * BETA),
        'cd_w_in': nrm(ks[9], (N_CD, D_MODEL, P_CD), D_MODEL ** -0.5) * jnp.asarray(cd_scale),
        'cd_b_in': nrm(ks[10], (N_CD, P_CD), 0.02),
        'mlstm_conv_w': nrm(ks[11], (N_CD, CONV_WIDTH, 2 * D_HEADS * D_DK), CONV_WIDTH ** -0.5),
        'mlstm_conv_b': nrm(ks[12], (N_CD, 2 * D_HEADS * D_DK), 0.02),
        'mlstm_fgate_b': jnp.linspace(3.0, 6.0, D_HEADS, dtype=F32)[None, None, :] + nrm(ks[13], (N_CD, 2, D_HEADS), 0.1),
        'ret_norm': 1.0 + nrm(ks[14], (N_CD, C_HEADS * C_DV), 0.02),
        'mlstm_norm': 1.0 + nrm(ks[15], (N_CD, D_HEADS * D_DV), 0.02),
        'cd_w_out': nrm(ks[16], (N_CD, MIX_CD, D_MODEL), MIX_CD ** -0.5 * BETA),
        'ln_mix_g': 1.0 + nrm(ks[17], (DEPTH, D_MODEL), 0.02),
        'ln_mix_b': nrm(ks[18], (DEPTH, D_MODEL), 0.02),
        'ln_ffn_g': 1.0 + nrm(ks[19], (DEPTH, D_MODEL), 0.02),
        'ln_ffn_b': nrm(ks[20], (DEPTH, D_MODEL), 0.02),
        'router_w': nrm(ks[21], (D_MODEL, N_EXPERTS), D_MODEL ** -0.5),
        'router_b': nrm(ks[22], (N_EXPERTS,), 0.01),
        'moe_w1': nrm(ks[23], (DEPTH, N_EXPERTS, D_MODEL, D_EXPERT), D_MODEL ** -0.5 * BETA),
        'moe_w3': nrm(ks[24], (DEPTH, N_EXPERTS, D_MODEL, D_EXPERT), D_MODEL ** -0.5 * BETA),
        'moe_w2': nrm(ks[25], (DEPTH, N_EXPERTS, D_EXPERT, D_MODEL), D_EXPERT ** -0.5 * BETA),
    }


def reference(x, ab_w_in, ab_b_in, hgrn_lb, gla_gk_up, gla_gk_b, hgrn_norm, gla_norm, ab_w_out,
              cd_w_in, cd_b_in, mlstm_conv_w, mlstm_conv_b, mlstm_fgate_b, ret_norm, mlstm_norm, cd_w_out,
              ln_mix_g, ln_mix_b, ln_ffn_g, ln_ffn_b, router_w, router_b, moe_w1, moe_w3, moe_w2):
    lower_bounds = jnp.cumsum(jax.nn.softmax(hgrn_lb.astype(F32), axis=0), axis=0)
    h = x
    for layer in range(DEPTH):
        j = layer // 2
        if layer % 2 == 0:
            y = _mixer_ab(h, ab_w_in[j], ab_b_in[j], lower_bounds[layer], gla_gk_up[j], gla_gk_b[j],
                          hgrn_norm[j], gla_norm[j], ab_w_out[j])
        else:
            y = _mixer_cd(h, cd_w_in[j], cd_b_in[j], mlstm_conv_w[j], mlstm_conv_b[j], mlstm_fgate_b[j],
                          ret_norm[j], mlstm_norm[j], cd_w_out[j])
        h = _layer_norm(ALPHA * h + y, ln_mix_g[layer], ln_mix_b[layer])
        y = _moe(h, router_w, router_b, moe_w1[layer], moe_w3[layer], moe_w2[layer])
        h = _layer_norm(ALPHA * h + y, ln_ffn_g[layer], ln_ffn_b[layer])
    return h
```

```python
from contextlib import ExitStack
import types
import numpy as np
import concourse.bass as bass
import concourse.mybir as mybir
from concourse.bass_utils import run_bass_kernel_spmd

F32 = mybir.dt.float32
BF16 = mybir.dt.bfloat16
AF = mybir.ActivationFunctionType
ALU = mybir.AluOpType
AX = mybir.AxisListType

ENGS = ("pe", "dve", "act", "pool", "sp")
D = 1024
NE = 16
ALPHA = (2.0 * 2) ** 0.25
LN_EPS = 1e-5
NORM_EPS = 1e-6


def _snap(fn):
    if fn.__closure__ is None:
        return fn
    cells = []
    for c_ in fn.__closure__:
        try:
            cells.append(types.CellType(c_.cell_contents))
        except ValueError:
            cells.append(c_)
    return types.FunctionType(fn.__code__, fn.__globals__, fn.__name__, fn.__defaults__, tuple(cells))


class Tok:
    __slots__ = ("name", "last_w", "readers", "sem", "semcnt", "excl", "uid")
    _n = [0]

    def __init__(self, name="", excl=False):
        Tok._n[0] += 1
        self.uid = Tok._n[0]
        self.name = name
        self.excl = excl
        self.last_w = None
        self.readers = []
        self.sem = None
        self.semcnt = 0


class KB:
    def __init__(self, nc, same_eng_sync=None):
        if same_eng_sync is None:
            same_eng_sync = globals().get("SAME_ENG_SYNC", ("dve", "act", "pool"))
        self.nc = nc
        self.ctx = ExitStack()
        self.prog = {e: [] for e in ENGS}
        self.seen = {e: {} for e in ENGS}
        self.same = set(same_eng_sync)
        self.sems = {}
        self.dma_toks = []
        self.nsem = 0
        self.ninst = 0
        self.scopes = []
        self.scope_dma = []
        self.free_sems = {"sw": [], "hw": [], "cc": []}
        self.uid = 0
        self.phase = -1
        self.new_phase()

    def new_phase(self):
        self.phase += 1
        self.esem = {}
        for e in ENGS:
            if e == "sp":
                continue
            self.esem[e] = self.ctx.enter_context(self.nc.semaphore("s_%s%d" % (e, self.phase)))
            self.sems[("e", e, self.phase)] = self.esem[e]
            self.nsem += 1
        self.ecnt = {e: 0 for e in ENGS}

    def scope(self):
        st = ExitStack()
        self.scopes.append(st)
        return st

    def end_scope(self):
        self.barrier()
        self.scopes.pop().close()
        for (owner, cls) in self.scope_dma:
            self.free_sems[cls].append((owner.sem[cls], owner.semcnt[cls]))
            self.dma_toks.remove((owner, cls))
        self.scope_dma = []
        self.new_phase()

    def _dma_sem(self, owner, cls):
        if owner.sem is None:
            owner.sem = {}
            owner.semcnt = {}
        if cls not in owner.sem:
            if self.free_sems[cls]:
                owner.sem[cls], owner.semcnt[cls] = self.free_sems[cls].pop()
            else:
                owner.sem[cls] = self.ctx.enter_context(self.nc.semaphore("d%d" % self.nsem))
                owner.semcnt[cls] = 0
                self.nsem += 1
            self.sems[("d", owner.uid, cls)] = owner.sem[cls]
            self.dma_toks.append((owner, cls))
            if self.scopes:
                self.scope_dma.append((owner, cls))

    def sb(self, name, shape, dtype):
        self.uid += 1
        st = self.scopes[-1] if self.scopes else self.ctx
        return st.enter_context(self.nc.sbuf_tensor("%s_%d" % (name, self.uid), list(shape), dtype))

    def ps(self, name, shape, dtype=F32):
        self.uid += 1
        st = self.scopes[-1] if self.scopes else self.ctx
        return st.enter_context(self.nc.psum_tensor("%s_%d" % (name, self.uid), list(shape), dtype))

    def _need(self, eng, waits, ev):
        if ev is None:
            return
        key, val = ev
        if key[0] == "e" and key[1] == eng and eng not in self.same:
            return
        if self.seen[eng].get(key, 0) >= val:
            return
        if waits.get(key, 0) < val:
            waits[key] = val

    def _emit_waits(self, eng, waits):
        for key, val in waits.items():
            self.prog[eng].append(("wait", self.sems[key], val))
            self.seen[eng][key] = val

    def _deps(self, eng, r, w):
        waits = {}
        for t in r:
            self._need(eng, waits, t.last_w)
        for t in w:
            self._need(eng, waits, t.last_w)
            for ev in t.readers:
                self._need(eng, waits, ev)
        self._emit_waits(eng, waits)

    def op(self, eng, fn, r=(), w=()):
        ex = [t for t in r if t.excl]
        if ex:
            r = [t for t in r if not t.excl]
            w = list(w) + [t for t in ex if t not in w]
        self._deps(eng, r, w)
        self.ecnt[eng] += 1
        ev = (("e", eng, self.phase), self.ecnt[eng])
        self.prog[eng].append(("op", _snap(fn), self.esem[eng], 1))
        for t in r:
            t.readers.append(ev)
        for t in w:
            t.last_w = ev
            t.readers = []
        self.ninst += 1

    def dma(self, eng, out, in_, owner, r=(), w=(), **kw):
        self._deps(eng, r, w)
        cls = "sw" if eng == "pool" else "hw"
        self._dma_sem(owner, cls)
        owner.semcnt[cls] += 16
        ev = (("d", owner.uid, cls), owner.semcnt[cls])
        self.prog[eng].append(("op", lambda e, o=out, i=in_, k=kw: e.dma_start(out=o, in_=i, **k), owner.sem[cls], 16))
        for t in r:
            t.readers.append(ev)
        for t in w:
            t.last_w = ev
            t.readers = []
        self.ninst += 1

    def coll(self, kind, ins, outs, groups, owner, r=(), w=()):
        self._deps("pool", r, w)
        cls = "cc"
        self._dma_sem(owner, cls)
        owner.semcnt[cls] += 1
        ev = (("d", owner.uid, cls), owner.semcnt[cls])
        self.prog["pool"].append(("op", lambda e: e.collective_compute(kind, ALU.bypass, replica_groups=groups, ins=ins, outs=outs),
                                  owner.sem[cls], 1))
        for t in r:
            t.readers.append(ev)
        for t in w:
            t.last_w = ev
            t.readers = []
        self.ninst += 1

    def barrier(self):
        for eng in ENGS:
            waits = {}
            for e2 in ENGS:
                if e2 != eng and self.ecnt[e2] > 0:
                    self._need(eng, waits, (("e", e2, self.phase), self.ecnt[e2]))
            for (t, cls) in self.dma_toks:
                self._need(eng, waits, (("d", t.uid, cls), t.semcnt[cls]))
            self._emit_waits(eng, waits)

    def finish(self):
        self.barrier()
        nc = self.nc
        engmap = {"pe": "tensor", "dve": "vector", "act": "scalar", "pool": "gpsimd", "sp": "sync"}
        with nc.Block() as block:
            for e in ENGS:
                items = self.prog[e]

                def body(engobj, items=items):
                    for it in items:
                        if it[0] == "wait":
                            engobj.wait_ge(it[1], it[2])
                        else:
                            it[1](engobj).then_inc(it[2], it[3])
                getattr(block, engmap[e])(body)
        self.ctx.close()


class Bufs:
    def __init__(self, k, name, shape, dtype, n, space="sb"):
        mk = k.sb if space == "sb" else k.ps
        self.t = [mk(name, shape, dtype) for _ in range(n)]
        self.tok = [Tok(name, excl=(space == "ps")) for _ in range(n)]
        self.i = -1

    def next(self):
        self.i = (self.i + 1) % len(self.t)
        return self.t[self.i], self.tok[self.i]


def make_consts(k):
    c = {}
    idf = k.sb("identf", [128, 128], F32)
    idb = k.sb("identb", [128, 128], BF16)
    t = Tok("ident")
    k.op("pool", lambda e: e.memset(idf[:], 0.0), w=[t])
    k.op("pool", lambda e: e.affine_select(out=idf[:], in_=idf[:], pattern=[[-1, 128]], compare_op=ALU.not_equal,
                                           fill=1.0, base=0, channel_multiplier=1), r=[t], w=[t])
    k.op("pool", lambda e: e.tensor_copy(idb[:], idf[:]), r=[t], w=[t])
    c["idf"], c["idb"], c["t_id"] = idf, idb, t
    return c


def emit_transpose_phase(k, c, h_tm, xt_fm, NT):
    k.scope()
    ntile = NT // 128
    hb = Bufs(k, "tp_h", [128, D], BF16, 2)
    pt = Bufs(k, "tp_pt", [128, 8, 128], BF16, 2, "ps")
    ob = Bufs(k, "tp_o", [128, 8, 128], BF16, 2)
    zc = k.sb("tp_z", [128, 8, 1], BF16)
    tzc = Tok("tp_z")
    k.op("pool", lambda e: e.memset(zc[:], 0.0), w=[tzc])
    xv = xt_fm.rearrange("(kc p) t -> p kc t", p=128)
    for col in (0, NT + 1):
        k.dma("sp", xv[:, :, col:col + 1], zc[:], tzc, r=[tzc], allow_slow_non_contiguous=True)
    for tt in range(ntile):
        h, th = hb.next()
        k.dma("pool", h[:], h_tm[tt * 128:(tt + 1) * 128, :], th, w=[th])
        p, tp = pt.next()
        for kc in range(8):
            k.op("pe", lambda e, p=p, h=h, kc=kc: e.transpose(p[:, kc, :], h[:, kc * 128:(kc + 1) * 128], c["idb"][:]),
                 r=[th, c["t_id"]], w=[tp])
        o, to = ob.next()
        k.op("act" if tt % 2 else "dve",
             (lambda e, o=o, p=p: e.copy(out=o[:], in_=p[:])) if tt % 2 else (lambda e, o=o, p=p: e.tensor_copy(o[:], p[:])),
             r=[tp], w=[to])
        dst = xt_fm.rearrange("(kc p) t -> p kc t", p=128)[:, :, 1 + tt * 128: 1 + (tt + 1) * 128]
        k.dma("sp", dst, o[:], to, r=[to])
    k.end_scope()


def emit_ln(k, z, tz, gbc, bbc, tgb, st, tst, out, tout, eng2="pool"):
    for hh in range(2):
        k.op("dve", lambda e, hh=hh: e.bn_stats(st[:, hh * 6:(hh + 1) * 6], z[:, hh * 512:(hh + 1) * 512]), r=[tz], w=[tst])
    k.op("dve", lambda e: e.bn_aggr(st[:, 12:14], st[:, 0:12]), r=[tst], w=[tst])
    k.op("act", lambda e: e.activation(out=st[:, 14:15], in_=st[:, 13:14], func=AF.Ln, bias=LN_EPS, scale=1.0), r=[tst], w=[tst])
    k.op("act", lambda e: e.activation(out=st[:, 14:15], in_=st[:, 14:15], func=AF.Exp, scale=-0.5), r=[tst], w=[tst])
    k.op("dve", lambda e: e.tensor_scalar(out=z[:], in0=z[:], scalar1=st[:, 12:13], scalar2=st[:, 14:15],
                                          op0=ALU.subtract, op1=ALU.mult), r=[tz, tst], w=[tz])
    k.op(eng2, lambda e: e.tensor_tensor(out=z[:], in0=z[:], in1=gbc[:], op=ALU.mult), r=[tz, tgb], w=[tz])
    k.op(eng2, lambda e: e.tensor_tensor(out=out[:], in0=z[:], in1=bbc[:], op=ALU.add), r=[tz, tgb], w=[tout])


def emit_moe_phase(k, c, h_tm, out_tm, w1, w3, w2, router_w, router_b, ln_g, ln_b, NT, TQ=1024, ne=NE):
    k.scope()
    nq = NT // TQ
    ntile = TQ // 128
    nblk = TQ // 512
    tconst = Tok("moe_const")
    rw = k.sb("rw", [128, 8, NE], F32)
    k.dma("sp", rw[:], router_w.rearrange("(kc p) e -> p kc e", p=128), tconst, w=[tconst])
    rb = k.sb("rb", [128, NE], F32)
    k.dma("sp", rb[:], router_b.partition_broadcast(128), tconst, w=[tconst])
    gbc = k.sb("gbc", [128, D], F32)
    bbc = k.sb("bbc", [128, D], F32)
    k.dma("sp", gbc[:], ln_g.partition_broadcast(128), tconst, w=[tconst])
    k.dma("sp", bbc[:], ln_b.partition_broadcast(128), tconst, w=[tconst])

    xT = k.sb("xT", [128, 8, TQ], BF16)
    t_xT = [Tok("xT%d" % i) for i in range(ntile)]
    acc = k.sb("acc", [128, ntile, D], F32)
    t_acc = [Tok("acc%d" % i) for i in range(ntile)]
    lg = k.sb("lg", [128, ntile, NE], F32)
    t_lg = Tok("lg")
    gates = k.sb("gates", [128, ntile, NE], F32)
    t_gates = Tok("gates")
    hst = Bufs(k, "hst", [128, D], F32, 2)
    xTf = Bufs(k, "xTf", [128, 8, 128], F32, 2)
    ptr = Bufs(k, "ptr", [128, 4, 128], F32, 1, "ps")
    plg = Bufs(k, "plg", [128, 512], F32, 1, "ps")
    p13 = Bufs(k, "p13", [128, 2, 512], F32, 2, "ps")
    py = Bufs(k, "py", [128, 512], F32, 2, "ps")
    wst = Bufs(k, "wst", [128, 2, D], F32, 3)
    wring = Bufs(k, "wring", [128, 8, D], BF16, 4)
    hid = Bufs(k, "hid", [128, 8, 512], BF16, 2)
    sil = Bufs(k, "sil", [128, 512], BF16, 2)
    rt = [k.sb("rt%d" % i, [128, ntile, NE], F32) for i in range(3)]
    rs = [k.sb("rs%d" % i, [128, ntile, 4], F32) for i in range(4)]
    t_rt = Tok("rt")
    st = k.sb("lnst", [128, 16], F32)
    t_st = Tok("lnst")
    ob = Bufs(k, "moe_o", [128, D], F32, 2)
    gmx = k.sb("gmx", [128, ntile, 1], F32)

    def load_w(wd, e):
        wb, twb = wring.next()
        src = wd[e].rearrange("(kc p) n -> p kc n", p=128)
        for j in range(4):
            s, ts = wst.next()
            k.dma("sp", s[:], src[:, 2 * j:2 * j + 2, :], ts, w=[ts])
            if j == 3:
                k.op("pool", lambda e_, wb=wb, s=s, j=j: e_.tensor_copy(wb[:, 2 * j:2 * j + 2, :], s[:]), r=[ts], w=[twb])
            else:
                k.op("act", lambda e_, wb=wb, s=s, j=j: e_.copy(out=wb[:, 2 * j:2 * j + 2, :], in_=s[:]), r=[ts], w=[twb])
        return wb, twb

    for q in range(nq):
        t0 = q * TQ
        for tt in range(ntile):
            h, th = hst.next()
            k.dma("sp", h[:], h_tm[t0 + tt * 128: t0 + (tt + 1) * 128, :], th, w=[th])
            k.op("act", lambda e, h=h, tt=tt: e.mul(out=acc[:, tt, :], in_=h[:], mul=ALPHA), r=[th], w=[t_acc[tt]])
            xf, txf = xTf.next()
            for half in range(2):
                p, tp = ptr.next()
                for j in range(4):
                    kc = half * 4 + j
                    k.op("pe", lambda e, p=p, h=h, j=j, kc=kc: e.transpose(p[:, j, :], h[:, kc * 128:(kc + 1) * 128], c["idf"][:]),
                         r=[th, c["t_id"]], w=[tp])
                k.op("act", lambda e, xf=xf, p=p, half=half: e.copy(out=xf[:, half * 4:(half + 1) * 4, :], in_=p[:]), r=[tp], w=[txf])
                k.op("pool", lambda e, xf=xf, half=half, tt=tt: e.tensor_copy(xT[:, half * 4:(half + 1) * 4, tt * 128:(tt + 1) * 128],
                                                                             xf[:, half * 4:(half + 1) * 4, :]), r=[txf], w=[t_xT[tt]])
            pl, tpl = plg.next()
            for kc in range(8):
                k.op("pe", lambda e, pl=pl, xf=xf, kc=kc: e.matmul(pl[:, 0:NE], lhsT=xf[:, kc, :], rhs=rw[:, kc, :], start=(kc == 0), stop=(kc == 7)),
                     r=[txf, tconst], w=[tpl])
            k.op("dve", lambda e, pl=pl, tt=tt: e.tensor_copy(lg[:, tt, :], pl[:, 0:NE]), r=[tpl], w=[t_lg])
        R = [t_lg, t_rt, t_gates, tconst]

        def dv(fn):
            k.op("dve", fn, r=R, w=[t_rt, t_gates])
        mx, sm, den = rs[0][:, :, 0:1], rs[1][:, :, 0:1], rs[2][:, :, 0:1]
        bc16 = lambda a: a.broadcast_to([128, ntile, NE])
        dv(lambda e: e.tensor_reduce(out=mx, in_=lg[:], axis=AX.X, op=ALU.max))
        dv(lambda e: e.tensor_tensor(out=rt[0][:], in0=lg[:], in1=bc16(mx), op=ALU.subtract))
        k.op("act", lambda e: e.activation(out=rt[0][:], in_=rt[0][:], func=AF.Exp), r=R, w=[t_rt])
        dv(lambda e: e.tensor_reduce(out=sm, in_=rt[0][:], axis=AX.X, op=ALU.add))
        dv(lambda e: e.reciprocal(out=sm, in_=sm))
        dv(lambda e: e.tensor_tensor(out=rt[0][:], in0=rt[0][:], in1=bc16(sm), op=ALU.mult))
        dv(lambda e: e.tensor_tensor(out=rt[1][:], in0=rt[0][:], in1=rb[:].unsqueeze(1).broadcast_to([128, ntile, NE]), op=ALU.add))
        b4 = rt[1][:].rearrange("p t (g e) -> p t g e", e=4)
        w4 = rt[2][:].rearrange("p t (g e) -> p t g e", e=4)
        bc4 = lambda a: a.unsqueeze(3).broadcast_to([128, ntile, 4, 4])
        dv(lambda e: e.tensor_reduce(out=rs[0][:], in_=b4, axis=AX.X, op=ALU.max))
        dv(lambda e: e.tensor_tensor(out=w4, in0=b4, in1=bc4(rs[0][:]), op=ALU.is_equal))
        dv(lambda e: e.scalar_tensor_tensor(out=rt[2][:], in0=rt[2][:], scalar=-1e9, in1=rt[1][:], op0=ALU.mult, op1=ALU.add))
        dv(lambda e: e.tensor_reduce(out=rs[1][:], in_=w4, axis=AX.X, op=ALU.max))
        dv(lambda e: e.tensor_tensor(out=rs[2][:], in0=rs[0][:], in1=rs[1][:], op=ALU.add))
        dv(lambda e: e.tensor_reduce(out=gmx[:], in_=rs[2][:], axis=AX.X, op=ALU.max))
        dv(lambda e: e.tensor_tensor(out=rs[3][:], in0=rs[2][:], in1=gmx[:].broadcast_to([128, ntile, 4]), op=ALU.is_equal))
        dv(lambda e: e.tensor_tensor(out=w4, in0=b4, in1=bc4(rs[1][:]), op=ALU.is_ge))
        dv(lambda e: e.tensor_tensor(out=w4, in0=w4, in1=bc4(rs[3][:]), op=ALU.mult))
        dv(lambda e: e.tensor_tensor(out=rt[2][:], in0=rt[2][:], in1=rt[0][:], op=ALU.mult))
        dv(lambda e: e.tensor_reduce(out=den, in_=rt[2][:], axis=AX.X, op=ALU.add))
        dv(lambda e: e.reciprocal(out=den, in_=den))
        dv(lambda e: e.tensor_tensor(out=gates[:], in0=rt[2][:], in1=bc16(den), op=ALU.mult))
        for ex in range(ne):
            w1b, tw1 = load_w(w1, ex)
            w3b, tw3 = load_w(w3, ex)
            w2b, tw2 = load_w(w2, ex)
            for tb in range(nblk):
                hd, thd = hid.next()
                xtoks = t_xT[tb * 4:(tb + 1) * 4]
                for cc in range(8):
                    p, tp = p13.next()
                    for (wi, wb, tw) in ((0, w1b, tw1), (1, w3b, tw3)):
                        for kc in range(8):
                            k.op("pe", lambda e, p=p, wi=wi, wb=wb, kc=kc, cc=cc, tb=tb: e.matmul(
                                p[:, wi, :], lhsT=wb[:, kc, cc * 128:(cc + 1) * 128], rhs=xT[:, kc, tb * 512:(tb + 1) * 512],
                                start=(kc == 0), stop=(kc == 7)), r=[tw] + xtoks, w=[tp])
                    s, ts = sil.next()
                    k.op("act", lambda e, s=s, p=p: e.activation(out=s[:], in_=p[:, 0, :], func=AF.Silu), r=[tp], w=[ts])
                    k.op("dve", lambda e, hd=hd, cc=cc, s=s, p=p: e.tensor_tensor(out=hd[:, cc, :], in0=s[:], in1=p[:, 1, :], op=ALU.mult),
                         r=[ts, tp], w=[thd])
                for t4 in range(4):
                    tt = tb * 4 + t4
                    for half in range(2):
                        y, ty = py.next()
                        for cc in range(8):
                            k.op("pe", lambda e, y=y, hd=hd, cc=cc, t4=t4, half=half, w2b=w2b: e.matmul(
                                y[:], lhsT=hd[:, cc, t4 * 128:(t4 + 1) * 128], rhs=w2b[:, cc, half * 512:(half + 1) * 512],
                                start=(cc == 0), stop=(cc == 7)), r=[thd, tw2], w=[ty])
                        k.op("dve", lambda e, y=y, tt=tt, half=half, ex=ex: e.scalar_tensor_tensor(
                            out=acc[:, tt, half * 512:(half + 1) * 512], in0=y[:], scalar=gates[:, tt, ex:ex + 1],
                            in1=acc[:, tt, half * 512:(half + 1) * 512], op0=ALU.mult, op1=ALU.add),
                            r=[ty, t_gates, t_acc[tt]], w=[t_acc[tt]])
        for tt in range(ntile):
            o, to = ob.next()
            emit_ln(k, acc[:, tt, :], t_acc[tt], gbc, bbc, tconst, st, t_st, o, to)
            k.dma("sp", out_tm[t0 + tt * 128: t0 + (tt + 1) * 128, :], o[:], to, r=[to])
    k.end_scope()


L = 64
VP = 130


def mixer_layout(layer):
    fm = []
    sgs = []
    if layer == 0:
        for h in range(4):
            fm.append(("qa%d" % h, 128))
        for h in range(4):
            fm.append(("fa%d" % h, 128))
        for h in range(4):
            fm.append(("qb%d" % h, 64))
        for h in range(4):
            fm.append(("kb%d" % h, 64))
        fm.append(("rb", 16))
        for h in range(4):
            sgs.append(dict(typ="A", h=h, dk=128, hv=h, qscale=1.0, lfscale=1.0, dve=128))
        for h in range(4):
            sgs.append(dict(typ="B", h=h, dk=64, hv=4 + h, qscale=0.125, lfscale=-1.0 / 16.0, dve=128))
    else:
        for nm in ("qc", "qs", "kc", "ks"):
            for h in range(4):
                fm.append(("%s%d" % (nm, h), 64))
        for h in range(4):
            fm.append(("qd%d" % h, 128))
        for h in range(4):
            fm.append(("kd%d" % h, 128))
        fm.append(("gd", 8))
        for h in range(4):
            sgs.append(dict(typ="C", h=h, dk=64, hv=h, qscale=0.125, lfscale=1.0, dve=128))
        for h in range(4):
            sgs.append(dict(typ="D", h=h, dk=128, hv=4 + h, qscale=128.0 ** -0.5, lfscale=-1.0, dve=129))
    return fm, sgs


def emit_mixer_pass(k, c, layer, pidx, NT, xt_fm, P, T=256):
    k.scope()
    fm, sgs = mixer_layout(layer)
    asc = (pidx == 1)
    NCH = T // L
    NTL = T // 128
    nblk = NT // T
    goff = {}
    off = 0
    for gi, (nm, wd) in enumerate(fm):
        goff[nm] = (gi, off, wd)
        off += wd
    CF = off
    CT = 1024 if pidx == 1 else 2048
    tc_ = Tok("mx_const")

    wfm = k.sb("wfm", [128, 8, CF], BF16)
    for kc in range(8):
        k.dma("pool", wfm[:, kc, :], P["wfm"][kc * 128:(kc + 1) * 128, :], tc_, w=[tc_])
    bfm = k.sb("bfm", [128, len(fm)], F32)
    k.dma("sp", bfm[:], P["bfm"], tc_, w=[tc_])
    wtm = k.sb("wtm", [128, 8, CT], BF16)
    for kc in range(8):
        k.dma("pool", wtm[:, kc, :], P["wtm"][kc * 128:(kc + 1) * 128, 0:CT], tc_, w=[tc_])
    btm = k.sb("btm", [128, CT], F32)
    k.dma("sp", btm[:], P["btm"][:, 0:CT].partition_broadcast(128), tc_, w=[tc_])
    rmask = k.sb("rmask", [128, T], F32)
    k.op("pool", lambda e: e.memset(rmask[:], 1.0), w=[tc_])
    rpos = 0 if asc else L - 1
    k.op("pool", lambda e: e.memset(rmask[:].rearrange("p (c l) -> p c l", l=L)[:, :, rpos:rpos + 1], 0.0), w=[tc_])
    smask = k.sb("smask", [128, 128], F32)
    k.op("pool", lambda e: e.memset(smask[:], 1.0), w=[tc_])
    if asc:
        k.op("pool", lambda e: e.affine_select(out=smask[:], in_=smask[:], pattern=[[1, 128]], compare_op=ALU.is_ge, fill=0.0,
                                               base=0, channel_multiplier=-1), w=[tc_])
    else:
        k.op("pool", lambda e: e.affine_select(out=smask[:], in_=smask[:], pattern=[[-1, 128]], compare_op=ALU.is_ge, fill=0.0,
                                               base=0, channel_multiplier=1), w=[tc_])
    k.op("pool", lambda e: e.memset(smask[0:64, 64:128], 0.0), w=[tc_])
    k.op("pool", lambda e: e.memset(smask[64:128, 0:64], 0.0), w=[tc_])

    ex = {}
    if layer == 0:
        hl = k.sb("hl", [128, 4, 3], F32)
        k.dma("sp", hl[:], P["hgrn_lb"], tc_, w=[tc_])
        k.op("act", lambda e: e.activation(out=hl[:], in_=hl[:], func=AF.Exp), r=[tc_], w=[tc_])
        hs = k.sb("hs", [128, 4, 1], F32)
        lb = k.sb("lb", [128, 4, 1], F32)
        oml = k.sb("oml", [128, 4, 1], F32)
        k.op("dve", lambda e: e.tensor_reduce(out=hs[:], in_=hl[:], axis=AX.X, op=ALU.add), r=[tc_], w=[tc_])
        k.op("dve", lambda e: e.reciprocal(out=hs[:], in_=hs[:]), r=[tc_], w=[tc_])
        k.op("dve", lambda e: e.tensor_tensor(out=lb[:], in0=hl[:, :, 0:1], in1=hs[:], op=ALU.mult), r=[tc_], w=[tc_])
        k.op("dve", lambda e: e.tensor_scalar(out=oml[:], in0=lb[:], scalar1=-1.0, scalar2=1.0, op0=ALU.mult, op1=ALU.add), r=[tc_], w=[tc_])
        gku = k.sb("gku", [16, 256], F32)
        k.dma("sp", gku[:], P["gk_up"], tc_, w=[tc_])
        ngkb = k.sb("ngkb", [64, 4], F32)
        k.dma("sp", ngkb[:], P["gk_b"], tc_, w=[tc_])
        k.op("dve", lambda e: e.tensor_scalar(out=ngkb[:], in0=ngkb[:], scalar1=-1.0, scalar2=None, op0=ALU.mult), r=[tc_], w=[tc_])
        rbs = k.sb("rbs", [16, T], F32)
        t_rbs = Tok("rbs")
    else:
        lgam = k.sb("lgam", [64, 4], F32)
        k.dma("sp", lgam[:], P["lgam"], tc_, w=[tc_])
        cw = k.sb("cw", [128, 8, 3], F32)
        k.dma("sp", cw[:], P["conv_w"], tc_, w=[tc_])
        cb = k.sb("cb", [128, 8], F32)
        k.dma("sp", cb[:], P["conv_b"], tc_, w=[tc_])
        selm = k.sb("selm", [8, 8, 128], F32)
        k.dma("sp", selm[:], P["selm"], tc_, w=[tc_])
        nfb = k.sb("nfb", [128, 4], F32)
        k.dma("sp", nfb[:], P["fgb"].partition_broadcast(128), tc_, w=[tc_])
        lfb = k.sb("lfb", [128, 4], F32)
        k.dma("sp", lfb[:], P["lfb"].partition_broadcast(128), tc_, w=[tc_])
        k.op("dve", lambda e: e.tensor_tensor(out=nfb[:], in0=nfb[:], in1=lfb[:], op=ALU.add), r=[tc_], w=[tc_])
        k.op("dve", lambda e: e.tensor_scalar(out=nfb[:], in0=nfb[:], scalar1=-1.0, scalar2=None, op0=ALU.mult), r=[tc_], w=[tc_])
        lib = k.sb("lib", [128, 4], F32)
        k.dma("sp", lib[:], P["lib"].partition_broadcast(128), tc_, w=[tc_])
        gds = k.sb("gds", [8, T], F32)
        t_gds = Tok("gds")
        hal = k.sb("hal", [128, 8], F32)
        if "halo_pair" in P:
            hal2 = k.sb("hal2", [128, 2, 8], F32)
            for r_ in range(2):
                k.dma("sp", hal2[:, r_, :], P["halo_pair"][r_].rearrange("(kc p) -> p kc", p=128), tc_, r=P["cc_tok"], w=[tc_], allow_slow_non_contiguous=True)
            selh = k.sb("selh", [128, 2], F32)
            k.dma("sp", selh[:], P["sel"].partition_broadcast(128), tc_, w=[tc_])
            k.op("dve", lambda e: e.tensor_scalar(out=hal[:], in0=hal2[:, 0, :], scalar1=selh[:, 0:1], scalar2=None, op0=ALU.mult), r=[tc_], w=[tc_])
            k.op("dve", lambda e: e.scalar_tensor_tensor(out=hal[:], in0=hal2[:, 1, :], scalar=selh[:, 1:2], in1=hal[:], op0=ALU.mult, op1=ALU.add),
                 r=[tc_], w=[tc_])
        else:
            k.dma("sp", hal[:], P["halo"].rearrange("(kc p) -> p kc", p=128), tc_, w=[tc_], allow_slow_non_contiguous=True)
        cst = Bufs(k, "cst", [64, 2, T], F32, 2)

    PF = Bufs(k, "PF", [128, 512], F32, 2, "ps")
    PX = Bufs(k, "PX", [128, 512], F32, 3, "ps")
    PO = [k.ps("PO", [128, 512], F32) for _ in range(3)]
    t_PO = [Tok("PO%d" % i, True) for i in range(3)]
    if layer == 0:
        oslots = [(j // 4, (j % 4) * 128) for j in range(8)]
        stgroups = [[0, 1, 2, 3], [4, 5, 6, 7]]
    else:
        oslots = [(0, j * 128) for j in range(4)] + [(1, 0), (1, 256), (2, 0), (2, 256)]
        stgroups = [[0, 1, 2, 3], [4, 5], [6, 7]]
    ofirst = set()
    seenb = set()
    for j, (bk, _) in enumerate(oslots):
        if bk not in seenb:
            seenb.add(bk)
            ofirst.add(j)

    xT = Bufs(k, "mxT", [128, 8, T + 2], BF16, 2)
    NSET = globals().get("MIX_NSET") or {(0, 1): 4, (1, 1): 4, (0, 2): 4, (1, 2): 2}[(layer, pidx)]
    tsets = []
    for _ in range(NSET):
        st_ = []
        for nm_ in ("Tq", "Tk", "Tlf", "TG", "Te1", "Te2"):
            st_ += [k.sb(nm_, [128, T], F32), Tok(nm_)]
        st_ += [k.sb("Tsm", [128, 4, NCH], F32), Tok("Tsm")]
        if layer == 1:
            st_ += [k.sb("aext", [128, T + 2], F32), Tok("aext")]
        tsets.append(st_)
    nsg = len(sgs)
    NPAR = globals().get("MIX_NPAR", {(0, 1): 1, (1, 1): 1, (0, 2): 1, (1, 2): 1})[(layer, pidx)]
    QT = [[k.sb("QT", [128, T], BF16) for _ in range(nsg)] for _ in range(NPAR)]
    KT = [[k.sb("KT", [128, T], BF16) for _ in range(nsg)] for _ in range(NPAR)]
    QIP = [[k.sb("QIP", [128, NCH, 128], BF16) for _ in range(nsg)] for _ in range(NPAR)]
    KST = [[k.sb("KST", [128, T], BF16) for _ in range(nsg)] for _ in range(NPAR)]
    KS = [k.sb("KS", [128, NTL, 128], BF16) for _ in range(nsg)]
    DEC = [[k.sb("DEC", [128, NCH], F32) for _ in range(nsg)] for _ in range(NPAR)]
    t_sg = [[Tok("sg%d" % i) for i in range(nsg)] for _ in range(NPAR)]
    t_ks = [Tok("ks%d" % i) for i in range(nsg)]
    for par in range(NPAR):
        for i in range(nsg):
            k.op("pool", lambda e, i=i, par=par: e.memset(QIP[par][i][:], 0.0), w=[t_sg[par][i]])
    if "sel" in P:
        selb = k.sb("selb", [128, 2], F32)
        k.dma("sp", selb[:], P["sel"].partition_broadcast(128), tc_, w=[tc_])
        stmp = Bufs(k, "stmp", [128, 2, VP], F32, 2)
    S32 = [k.sb("S32", [128, VP], F32) for _ in range(nsg)]
    Sb = [k.sb("Sb", [128, VP], BF16) for _ in range(nsg)]
    t_S = [Tok("S%d" % i) for i in range(nsg)]
    t_Sb = [Tok("Sb%d" % i) for i in range(nsg)]
    for i in range(nsg):
        if pidx == 1:
            k.op("pool", lambda e, i=i: e.memset(S32[i][:], 0.0), w=[t_S[i]])
        elif "state_pair" in P:
            sa, tsa = stmp.next()
            k.dma("sp", sa[:], P["state_pair"][:, i].rearrange("r p v -> p r v"), tsa, r=P["cc_tok"], w=[tsa])
            k.op("dve", lambda e, i=i, sa=sa: e.tensor_scalar(out=S32[i][:], in0=sa[:, 0, :], scalar1=selb[:, 0:1], scalar2=None, op0=ALU.mult),
                 r=[tsa, tc_], w=[t_S[i]])
            k.op("dve", lambda e, i=i, sa=sa: e.scalar_tensor_tensor(out=S32[i][:], in0=sa[:, 1, :], scalar=selb[:, 1:2], in1=S32[i][:],
                                                                   op0=ALU.mult, op1=ALU.add), r=[tsa, tc_, t_S[i]], w=[t_S[i]])
        else:
            k.dma("sp", S32[i][:], P["state_in"][i], t_S[i], w=[t_S[i]])
        k.op("pool", lambda e, i=i: e.tensor_copy(Sb[i][:], S32[i][:]), r=[t_S[i]], w=[t_Sb[i]])
    V = Bufs(k, "V", [128, 8, VP], BF16, 2)
    for vb_ in V.t:
        k.op("pool", lambda e, vb_=vb_: e.memset(vb_[:], 1.0), w=[tc_])
    SCP = Bufs(k, "SCP", [128, 8, 128], BF16, 2)
    OT = Bufs(k, "OT", [128, 8, 128], F32, 2)
    rden = k.sb("rden", [128, 4, 1], F32); t_rden = Tok("rden")
    if pidx == 2:
        wout = k.sb("wout", [128, 8, D], BF16)
        for kc in range(8):
            k.dma("pool", wout[:, kc, :], P["w_out"][kc * 128:(kc + 1) * 128, :], tc_, w=[tc_])
        gnb = k.sb("gnb", [128, D], F32)
        k.dma("sp", gnb[:], P["norm_g"].partition_broadcast(128), tc_, w=[tc_])
        lgb = k.sb("lgb", [128, D], F32)
        lbb = k.sb("lbb", [128, D], F32)
        k.dma("sp", lgb[:], P["ln_g"].partition_broadcast(128), tc_, w=[tc_])
        k.dma("sp", lbb[:], P["ln_b"].partition_broadcast(128), tc_, w=[tc_])
        O1 = Bufs(k, "O1", [128, 8, 128], F32, 1)
        XR = Bufs(k, "XR", [128, D], F32, 1)
        GA = Bufs(k, "GA", [128, D], F32, 1)
        ssq = k.sb("ssq", [128, 8, 1], F32); t_ssq = Tok("ssq")
        MX = Bufs(k, "MX", [128, D], BF16, 1)
        MXT = Bufs(k, "MXT", [128, 8, 128], BF16, 1)
        ZT = Bufs(k, "ZT", [128, D], F32, 1)
        HO = Bufs(k, "HO", [128, D], F32, 1)
        lst = k.sb("mlnst", [128, 16], F32); t_lst = Tok("mlnst")

    def c3(ap, n=L):
        return ap.rearrange("p (c l) -> p c l", l=n)

    def rv(tile_, dk):
        return bass.AP(tile_, T - 1, [[T, dk], [-1, T]])

    def proj_fm(nm, x, tx, cols=None):
        gi, o, wd = goff[nm]
        p, tp = PF.next()
        n = T if cols is None else 2
        for kc in range(8):
            rhs = x[:, kc, 1:T + 1] if cols is None else bass.AP(x, kc * (T + 2), [[8 * (T + 2), 128], [T + 1, 2]])
            k.op("pe", lambda e, p=p, kc=kc, rhs=rhs, o=o, wd=wd, n=n: e.matmul(p[0:wd, 0:n], lhsT=wfm[:, kc, o:o + wd], rhs=rhs,
                                                                              start=(kc == 0), stop=(kc == 7)), r=[tx, tc_], w=[tp])
        return p, tp, gi, wd

    last = L - 1 if asc else 0
    ref = L // 2 - 1 if asc else L // 2

    blocks = list(range(nblk)) if asc else list(range(nblk - 1, -1, -1))
    xs = {}

    def gm_block(b, par):
        t0 = b * T
        x, tx = xT.next()
        xs[b] = (x, tx)
        k.dma("sp", x[:], xt_fm.rearrange("(kc p) t -> p kc t", p=128)[:, :, t0:t0 + T + 2], tx, w=[tx])
        if layer == 1 and b == nblk - 1:
            k.op("pool", lambda e, x=x: e.tensor_copy(x[:, :, T + 1:T + 2], hal[:].unsqueeze(2)), r=[tx, tc_], w=[tx])
        if layer == 0:
            p, tp, gi, wd = proj_fm("rb", x, tx)
            k.op("act", lambda e, p=p, gi=gi: e.activation(out=rbs[:], in_=p[0:16, 0:T], func=AF.Identity, bias=bfm[0:16, gi:gi + 1], scale=1.0),
                 r=[tp, tc_], w=[t_rbs])
        else:
            p, tp, gi, wd = proj_fm("gd", x, tx)
            k.op("act", lambda e, p=p: e.copy(out=gds[:], in_=p[0:8, 0:T]), r=[tp], w=[t_gds])
            cs, tcs = cst.next()
            k.dma("sp", cs[:], P["cs_tab"][:, :, t0:t0 + T].rearrange("a p t -> p a t"), tcs, w=[tcs])
        def sg_body(si, sg):
                dk, h, typ = sg["dk"], sg["h"], sg["typ"]
                tw = [t_sg[par][si]]
                ts_ = tsets[si % NSET]
                Tq, t_Tq, Tk, t_Tk, Tlf, t_Tlf, TG, t_TG, Te1, t_Te1, Te2, t_Te2, Tsm, t_Tsm = ts_[:14]
                if layer == 1:
                    aext, t_aext = ts_[14:16]
                if typ == "A":
                    p, tp, gi, wd = proj_fm("qa%d" % h, x, tx)
                    yield k.op("act", lambda e, p=p, gi=gi: e.activation(out=Tq[:], in_=p[:, 0:T], func=AF.Silu, bias=bfm[:, gi:gi + 1], scale=1.0),
                         r=[tp, tc_], w=[t_Tq])
                    p, tp, gi, wd = proj_fm("fa%d" % h, x, tx)
                    yield k.op("act", lambda e, p=p, gi=gi: e.activation(out=Tk[:], in_=p[:, 0:T], func=AF.Sigmoid, bias=bfm[:, gi:gi + 1], scale=1.0),
                         r=[tp, tc_], w=[t_Tk])
                    yield k.op("dve", lambda e, h=h: e.tensor_scalar(out=Tk[:], in0=Tk[:], scalar1=oml[:, h, :], scalar2=lb[:, h, :], op0=ALU.mult, op1=ALU.add),
                         r=[t_Tk, tc_], w=[t_Tk])
                    yield k.op("act", lambda e: e.activation(out=Tlf[:], in_=Tk[:], func=AF.Ln), r=[t_Tk], w=[t_Tlf])
                    yield k.op("dve", lambda e: e.tensor_scalar(out=Tk[:], in0=Tk[:], scalar1=-1.0, scalar2=1.0, op0=ALU.mult, op1=ALU.add),
                         r=[t_Tk, t_Tlf], w=[t_Tk])
                elif typ == "B":
                    p, tp, gi, wd = proj_fm("qb%d" % h, x, tx)
                    yield k.op("act", lambda e, p=p, gi=gi: e.activation(out=Tq[0:64, :], in_=p[0:64, 0:T], func=AF.Identity, bias=bfm[0:64, gi:gi + 1], scale=1.0),
                         r=[tp, tc_], w=[t_Tq])
                    p, tp, gi, wd = proj_fm("kb%d" % h, x, tx)
                    yield k.op("act", lambda e, p=p, gi=gi: e.activation(out=Tk[0:64, :], in_=p[0:64, 0:T], func=AF.Identity, bias=bfm[0:64, gi:gi + 1], scale=1.0),
                         r=[tp, tc_], w=[t_Tk])
                    p, tp = PF.next()
                    k.op("pe", lambda e, p=p, h=h: e.matmul(p[0:64, 0:T], lhsT=gku[:, h * 64:(h + 1) * 64], rhs=rbs[:], start=True, stop=True),
                         r=[t_rbs, tc_], w=[tp])
                    yield k.op("act", lambda e, p=p, h=h: e.activation(out=Tlf[0:64, :], in_=p[0:64, 0:T], func=AF.Exp, bias=ngkb[:, h:h + 1], scale=-1.0),
                         r=[tp, tc_], w=[t_Tlf])
                    yield k.op("act", lambda e: e.activation(out=Tlf[0:64, :], in_=Tlf[0:64, :], func=AF.Ln, bias=1.0, scale=1.0), r=[t_Tlf], w=[t_Tlf])
                elif typ == "C":
                    for (dst, tdst, n1, n2) in ((Tq, t_Tq, "qc", "qs"), (Tk, t_Tk, "kc", "ks")):
                        p, tp, gi, wd = proj_fm("%s%d" % (n1, h), x, tx)
                        yield k.op("dve", lambda e, p=p, gi=gi, dst=dst: e.scalar_tensor_tensor(out=dst[0:64, :], in0=p[0:64, 0:T], scalar=bfm[0:64, gi:gi + 1],
                                                                                       in1=cs[:, 0, :], op0=ALU.add, op1=ALU.mult), r=[tp, tc_, tcs], w=[tdst])
                        p, tp, gi, wd = proj_fm("%s%d" % (n2, h), x, tx)
                        yield k.op("dve", lambda e, p=p, gi=gi: e.scalar_tensor_tensor(out=Te1[0:64, :], in0=p[0:64, 0:T], scalar=bfm[0:64, gi:gi + 1],
                                                                              in1=cs[:, 1, :], op0=ALU.add, op1=ALU.mult), r=[tp, tc_, tcs], w=[t_Te1])
                        yield k.op("dve", lambda e, dst=dst: e.tensor_tensor(out=dst[0:64, :], in0=dst[0:64, :], in1=Te1[0:64, :], op=ALU.add), r=[tdst, t_Te1], w=[tdst])
                    yield k.op("dve", lambda e, h=h: e.tensor_copy(Tlf[0:64, :], lgam[:, h:h + 1].broadcast_to([64, T])), r=[tc_], w=[t_Tlf])
                else:
                    for (dst, tdst, nm, ci) in ((Tq, t_Tq, "qd", h), (Tk, t_Tk, "kd", 4 + h)):
                        p, tp, gi, wd = proj_fm("%s%d" % (nm, h), x, tx)
                        yield k.op("act", lambda e, p=p, gi=gi: e.activation(out=aext[:, 1:T + 1], in_=p[:, 0:T], func=AF.Identity, bias=bfm[:, gi:gi + 1], scale=1.0),
                             r=[tp, tc_], w=[t_aext])
                        p, tp, gi, wd = proj_fm("%s%d" % (nm, h), x, tx, cols=2)
                        yield k.op("act", lambda e, p=p, gi=gi: e.activation(out=bass.AP(aext, 0, [[T + 2, 128], [T + 1, 2]]), in_=p[:, 0:2], func=AF.Identity,
                                                                      bias=bfm[:, gi:gi + 1], scale=1.0), r=[tp, tc_], w=[t_aext])
                        if b == 0:
                            yield k.op("pool", lambda e: e.memset(aext[:, 0:1], 0.0), r=[t_aext], w=[t_aext])
                        yield k.op("dve", lambda e, ci=ci: e.tensor_scalar(out=Te1[:], in0=aext[:, 0:T], scalar1=cw[:, ci, 0:1], scalar2=cb[:, ci:ci + 1],
                                                                   op0=ALU.mult, op1=ALU.add), r=[t_aext, tc_], w=[t_Te1])
                        yield k.op("dve", lambda e, ci=ci: e.scalar_tensor_tensor(out=Te1[:], in0=aext[:, 1:T + 1], scalar=cw[:, ci, 1:2], in1=Te1[:],
                                                                          op0=ALU.mult, op1=ALU.add), r=[t_aext, tc_, t_Te1], w=[t_Te1])
                        yield k.op("dve", lambda e, ci=ci: e.scalar_tensor_tensor(out=Te1[:], in0=aext[:, 2:T + 2], scalar=cw[:, ci, 2:3], in1=Te1[:],
                                                                          op0=ALU.mult, op1=ALU.add), r=[t_aext, tc_, t_Te1], w=[t_Te1])
                        yield k.op("act", lambda e, dst=dst: e.activation(out=dst[:], in_=Te1[:], func=AF.Silu), r=[t_Te1], w=[tdst])
                    p, tp = PF.next()
                    k.op("pe", lambda e, p=p, h=h: e.matmul(p[:, 0:T], lhsT=selm[:, h, :], rhs=gds[:], start=True, stop=True), r=[t_gds, tc_], w=[tp])
                    yield k.op("act", lambda e, p=p, h=h: e.activation(out=Te1[:], in_=p[:, 0:T], func=AF.Exp, bias=lib[:, h:h + 1], scale=1.0), r=[tp, tc_], w=[t_Te1])
                    yield k.op("dve", lambda e: e.tensor_tensor(out=Tk[:], in0=Tk[:], in1=Te1[:], op=ALU.mult), r=[t_Tk, t_Te1], w=[t_Tk])
                    p, tp = PF.next()
                    k.op("pe", lambda e, p=p, h=h: e.matmul(p[:, 0:T], lhsT=selm[:, 4 + h, :], rhs=gds[:], start=True, stop=True), r=[t_gds, tc_], w=[tp])
                    yield k.op("act", lambda e, p=p, h=h: e.activation(out=Tlf[:], in_=p[:, 0:T], func=AF.Exp, bias=nfb[:, h:h + 1], scale=-1.0), r=[tp, tc_], w=[t_Tlf])
                    yield k.op("act", lambda e: e.activation(out=Tlf[:], in_=Tlf[:], func=AF.Ln, bias=1.0, scale=1.0), r=[t_Tlf], w=[t_Tlf])
                ls = sg["lfscale"]
                if asc:
                    yield k.op("dve", lambda e, dk=dk: e.tensor_tensor_scan(out=TG[0:dk, :], data0=rmask[0:dk, :], data1=Tlf[0:dk, :], initial=0.0,
                                                                     op0=ALU.mult, op1=ALU.add), r=[t_Tlf, tc_], w=[t_TG])
                else:
                    yield k.op("dve", lambda e, dk=dk: e.tensor_tensor_scan(out=rv(TG, dk), data0=rv(rmask, dk), data1=rv(Tlf, dk), initial=0.0,
                                                                     op0=ALU.mult, op1=ALU.add), r=[t_Tlf, tc_], w=[t_TG])
                G3 = c3(TG[0:dk, :])
                gref = G3[:, :, ref:ref + 1]
                glast = G3[:, :, last:last + 1]
                yield k.op("dve", lambda e, dk=dk, G3=G3, gref=gref: e.tensor_tensor(out=c3(Te1[0:dk, :]), in0=G3, in1=gref.broadcast_to([dk, NCH, L]), op=ALU.subtract),
                     r=[t_TG], w=[t_Te1])
                yield k.op("act", lambda e, dk=dk, ls=ls: e.activation(out=Te2[0:dk, :], in_=Te1[0:dk, :], func=AF.Exp, scale=-ls), r=[t_Te1], w=[t_Te2])
                yield k.op("act", lambda e, dk=dk, ls=ls: e.activation(out=Te1[0:dk, :], in_=Te1[0:dk, :], func=AF.Exp, scale=ls), r=[t_Te1], w=[t_Te1])
                sm = Tsm[0:dk]
                yield k.op("dve", lambda e, dk=dk, sm=sm, glast=glast, gref=gref: e.tensor_tensor(out=sm[:, 2, :].unsqueeze(2), in0=glast, in1=gref, op=ALU.subtract),
                     r=[t_TG, t_Tsm], w=[t_Tsm])
                yield k.op("act", lambda e, sm=sm, gref=gref, ls=ls: e.activation(out=sm[:, 0, :].unsqueeze(2), in_=gref, func=AF.Exp, scale=ls), r=[t_TG, t_Tsm], w=[t_Tsm])
                yield k.op("act", lambda e, sm=sm, ls=ls: e.activation(out=sm[:, 1, :], in_=sm[:, 2, :], func=AF.Exp, scale=ls), r=[t_Tsm], w=[t_Tsm])
                yield k.op("act", lambda e, dk=dk, si=si, glast=glast, ls=ls: e.activation(out=DEC[par][si][0:dk, :].unsqueeze(2), in_=glast, func=AF.Exp, scale=ls),
                     r=[t_TG] + tw, w=tw)
                yield k.op("dve", lambda e, dk=dk, si=si, qs=sg["qscale"]: e.scalar_tensor_tensor(out=QT[par][si][0:dk, :], in0=Tq[0:dk, :], scalar=qs, in1=Te1[0:dk, :],
                                                                                         op0=ALU.mult, op1=ALU.mult), r=[t_Tq, t_Te1] + tw, w=tw)
                yield k.op("pool", lambda e, dk=dk, si=si: e.tensor_tensor(out=KT[par][si][0:dk, :], in0=Tk[0:dk, :], in1=Te2[0:dk, :], op=ALU.mult),
                     r=[t_Tk, t_Te2] + tw, w=tw)
                for a in range(2):
                    qv = QIP[par][si][0:dk].rearrange("p (t a) (b l) -> p t a b l", a=2, b=2)[:, :, a, a, :]
                    yield k.op("pool", lambda e, dk=dk, si=si, a=a, qv=qv, sm=sm: e.tensor_tensor(
                        out=qv, in0=QT[par][si][0:dk, :].rearrange("p (t a l) -> p t a l", a=2, l=L)[:, :, a, :],
                        in1=sm[:, 0, :].rearrange("p (t a) -> p t a", a=2)[:, :, a:a + 1].broadcast_to([dk, NTL, L]), op=ALU.mult),
                        r=[t_Tsm] + tw, w=tw)
                yield k.op("dve", lambda e, dk=dk, si=si, sm=sm: e.tensor_tensor(out=c3(KST[par][si][0:dk, :]), in0=c3(KT[par][si][0:dk, :]),
                                                                          in1=sm[:, 1, :].unsqueeze(2).broadcast_to([dk, NCH, L]), op=ALU.mult),
                     r=[t_Tsm] + tw, w=tw)

        GW = globals().get("MIX_GW") or NSET
        for g0 in range(0, len(sgs), GW):
            gens = [sg_body(si, sgs[si]) for si in range(g0, min(g0 + GW, len(sgs)))]
            while gens:
                for g_ in list(gens):
                    try:
                        next(g_)
                    except StopIteration:
                        gens.remove(g_)
            yield

    def scan_block(b, par):
        x, tx = xs[b]
        t0 = b * T
        tiles = list(range(NTL)) if asc else list(range(NTL - 1, -1, -1))
        for tl in tiles:
            r0 = t0 + tl * 128
            v, tv = V.next()
            for half in range(2):
                p, tp = PF.next()
                for kc in range(8):
                    k.op("pe", lambda e, p=p, kc=kc, tl=tl, half=half: e.matmul(p[:], lhsT=x[:, kc, 1 + tl * 128:1 + (tl + 1) * 128],
                                                                             rhs=wtm[:, kc, half * 512:(half + 1) * 512], start=(kc == 0), stop=(kc == 7)),
                         r=[tx, tc_], w=[tp])
                k.op("dve", lambda e, p=p, v=v, half=half: e.tensor_tensor(out=v[:, half * 4:(half + 1) * 4, 0:128], in0=p[:].rearrange("p (h d) -> p h d", d=128),
                                                                        in1=btm[:, half * 512:(half + 1) * 512].rearrange("p (h d) -> p h d", d=128), op=ALU.add),
                     r=[tp, tc_], w=[tv])
            yield
            p, tp = PF.next()
            pb = p[:].bitcast(BF16).rearrange("p (s d) -> p s d", d=128)
            for si, sg in enumerate(sgs):
                dk = sg["dk"]
                k.op("pe", lambda e, pb=pb, si=si, dk=dk, tl=tl: e.transpose(pb[:, si, 0:dk], KST[par][si][0:dk, tl * 128:(tl + 1) * 128], c["idb"][0:dk, 0:dk]),
                     r=[t_sg[par][si], c["t_id"]], w=[tp])
            for hf in range(2):
                for si in range(hf * 4, hf * 4 + 4):
                    dk = sgs[si]["dk"]
                    k.op("act", lambda e, pb=pb, si=si, dk=dk, tl=tl: e.copy(out=KS[si][:, tl, 0:dk], in_=pb[:, si, 0:dk]), r=[tp], w=[t_ks[si]])
            yield
            sc, tsc = SCP.next()
            for hf in range(2):
                ps__, tps = PX.next()
                ps_ = ps__[:].rearrange("p (s d) -> p s d", d=128)
                for j in range(4):
                    si = hf * 4 + j
                    dk = sgs[si]["dk"]
                    k.op("pe", lambda e, ps_=ps_, j=j, si=si, dk=dk, tl=tl: e.matmul(ps_[:, j, :], lhsT=KT[par][si][0:dk, tl * 128:(tl + 1) * 128],
                                                                                  rhs=QT[par][si][0:dk, tl * 128:(tl + 1) * 128], start=True, stop=True),
                         r=[t_sg[par][si]], w=[tps])
                k.op("dve", lambda e, sc=sc, ps_=ps_, hf=hf: e.tensor_tensor(out=sc[:, hf * 4:(hf + 1) * 4, :], in0=ps_,
                                                                          in1=smask[:].unsqueeze(1).broadcast_to([128, 4, 128]), op=ALU.mult),
                     r=[tps, tc_], w=[tsc])
            yield
            corder = (0, 1) if asc else (1, 0)
            for si, sg in enumerate(sgs):
                bk, col = oslots[si]
                dve_ = sg["dve"]
                k.op("pe", lambda e, bk=bk, col=col, dve_=dve_, sc=sc, si=si, v=v, hv=sg["hv"], first=(si in ofirst): e.matmul(
                    PO[bk][:, col:col + dve_], lhsT=sc[:, si, :], rhs=v[:, hv, 0:dve_], start=first, stop=False, skip_group_check=True),
                    r=[tsc, tv], w=[t_PO[bk]])
            for ci, cc in enumerate(corder):
                yield
                ch = tl * 2 + cc
                for si, sg in enumerate(sgs):
                    bk, col = oslots[si]
                    dk, dve_ = sg["dk"], sg["dve"]
                    k.op("pe", lambda e, bk=bk, col=col, si=si, dk=dk, dve_=dve_, ch=ch, ci=ci: e.matmul(
                        PO[bk][:, col:col + dve_], lhsT=QIP[par][si][0:dk, ch, :], rhs=Sb[si][0:dk, 0:dve_],
                        start=False, stop=(ci == 1), skip_group_check=True), r=[t_sg[par][si], t_Sb[si]], w=[t_PO[bk]])
                for grp in stgroups:
                    pst, tpst = PX.next()
                    pitch = 512 // len(grp)
                    for gj, si in enumerate(grp):
                        sg = sgs[si]
                        dk, dve_ = sg["dk"], sg["dve"]
                        col = gj * pitch
                        k.op("pe", lambda e, pst=pst, col=col, si=si, dk=dk, dve_=dve_, cc=cc, tl=tl, v=v, hv=sg["hv"]: e.matmul(
                            pst[0:dk, col:col + dve_], lhsT=KS[si][cc * 64:(cc + 1) * 64, tl, 0:dk], rhs=v[cc * 64:(cc + 1) * 64, hv, 0:dve_],
                            start=True, stop=True), r=[t_ks[si], tv], w=[tpst])
                    for gj, si in enumerate(grp):
                        sg = sgs[si]
                        dk, dve_ = sg["dk"], sg["dve"]
                        col = gj * pitch
                        k.op("dve", lambda e, si=si, dk=dk, dve_=dve_, ch=ch, pst=pst, col=col: e.scalar_tensor_tensor(
                            out=S32[si][0:dk, 0:dve_], in0=S32[si][0:dk, 0:dve_], scalar=DEC[par][si][0:dk, ch:ch + 1], in1=pst[0:dk, col:col + dve_],
                            op0=ALU.mult, op1=ALU.add), r=[t_S[si], t_sg[par][si], tpst], w=[t_S[si]])
                        k.op("act", lambda e, si=si, dk=dk, dve_=dve_: e.copy(out=Sb[si][0:dk, 0:dve_], in_=S32[si][0:dk, 0:dve_]), r=[t_S[si]], w=[t_Sb[si]])
            yield
            ot, tot = OT.next()
            if pidx == 2:
                o1, to1 = O1.next()
                k.dma("sp", o1[:], P["o1"][r0:r0 + 128, :].rearrange("p (h d) -> p h d", d=128), to1, w=[to1])
            for hf in range(2):
                if layer == 1 and hf == 1:
                    for bnk in range(2):
                        pv = PO[1 + bnk][:].rearrange("p (s d) -> p s d", d=256)
                        k.op("act", lambda e, pv=pv, bnk=bnk: e.activation(out=rden[:, bnk * 2:bnk * 2 + 2, :], in_=pv[:, :, 128:129], func=AF.Abs),
                             r=[t_PO[1 + bnk], t_rden], w=[t_rden])
                        k.op("dve", lambda e, bnk=bnk: e.tensor_scalar(out=rden[:, bnk * 2:bnk * 2 + 2, :], in0=rden[:, bnk * 2:bnk * 2 + 2, :], scalar1=1.0, scalar2=None,
                                                                      op0=ALU.max), r=[t_rden], w=[t_rden])
                        k.op("dve", lambda e, bnk=bnk: e.reciprocal(out=rden[:, bnk * 2:bnk * 2 + 2, :], in_=rden[:, bnk * 2:bnk * 2 + 2, :]), r=[t_rden], w=[t_rden])
                        k.op("dve", lambda e, pv=pv, bnk=bnk, ot=ot: e.tensor_tensor(out=ot[:, 4 + bnk * 2:6 + bnk * 2, :], in0=pv[:, :, 0:128],
                                                                                  in1=rden[:, bnk * 2:bnk * 2 + 2, :].broadcast_to([128, 2, 128]), op=ALU.mult),
                             r=[t_PO[1 + bnk], t_rden], w=[tot])
                        if pidx == 2:
                            k.op("pool", lambda e, bnk=bnk, ot=ot, o1=o1: e.tensor_tensor(out=ot[:, 4 + bnk * 2:6 + bnk * 2, :], in0=ot[:, 4 + bnk * 2:6 + bnk * 2, :],
                                                                                       in1=o1[:, 4 + bnk * 2:6 + bnk * 2, :], op=ALU.add), r=[to1, tot], w=[tot])
                else:
                    pv = PO[hf][:].rearrange("p (s d) -> p s d", d=128)
                    if pidx == 1:
                        k.op("act", lambda e, pv=pv, hf=hf, ot=ot: e.copy(out=ot[:, hf * 4:(hf + 1) * 4, :], in_=pv), r=[t_PO[hf]], w=[tot])
                    else:
                        k.op("dve", lambda e, pv=pv, hf=hf, ot=ot, o1=o1: e.tensor_tensor(out=ot[:, hf * 4:(hf + 1) * 4, :], in0=pv, in1=o1[:, hf * 4:(hf + 1) * 4, :], op=ALU.add),
                             r=[t_PO[hf], to1], w=[tot])
            if pidx == 1:
                k.dma("sp", P["o1"][r0:r0 + 128, :].rearrange("p (h d) -> p h d", d=128), ot[:], tot, r=[tot])
                continue
            yield
            xr, txr = XR.next()
            k.dma("sp", xr[:], P["x_tm"][r0:r0 + 128, :], txr, w=[txr])
            ga, tga = GA.next()
            for half in range(2):
                p, tp = PF.next()
                for kc in range(8):
                    k.op("pe", lambda e, p=p, kc=kc, tl=tl, half=half: e.matmul(p[:], lhsT=x[:, kc, 1 + tl * 128:1 + (tl + 1) * 128],
                                                                             rhs=wtm[:, kc, 1024 + half * 512:1024 + (half + 1) * 512], start=(kc == 0), stop=(kc == 7)),
                         r=[tx, tc_], w=[tp])
                k.op("dve", lambda e, p=p, ga=ga, half=half: e.tensor_tensor(out=ga[:, half * 512:(half + 1) * 512], in0=p[:],
                                                                          in1=btm[:, 1024 + half * 512:1024 + (half + 1) * 512], op=ALU.add), r=[tp, tc_], w=[tga])
                fn = AF.Sigmoid if (layer == 1 and half == 1) else AF.Silu
                k.op("act", lambda e, ga=ga, half=half, fn=fn: e.activation(out=ga[:, half * 512:(half + 1) * 512], in_=ga[:, half * 512:(half + 1) * 512], func=fn),
                     r=[tga], w=[tga])
            yield
            z, tz = ZT.next()
            SQ = z[:].rearrange("p (h d) -> p h d", d=128)
            k.op("pool", lambda e, ot=ot, SQ=SQ: e.tensor_tensor(out=SQ, in0=ot[:], in1=ot[:], op=ALU.mult), r=[tot], w=[tz])
            k.op("dve", lambda e, SQ=SQ: e.tensor_reduce(out=ssq[:], in_=SQ, axis=AX.X, op=ALU.add), r=[tz], w=[t_ssq])
            k.op("act", lambda e: e.activation(out=ssq[:], in_=ssq[:], func=AF.Ln, bias=NORM_EPS, scale=1.0 / 128.0), r=[t_ssq], w=[t_ssq])
            k.op("act", lambda e: e.activation(out=ssq[:], in_=ssq[:], func=AF.Exp, scale=-0.5), r=[t_ssq], w=[t_ssq])
            k.op("dve", lambda e, ot=ot: e.tensor_tensor(out=ot[:], in0=ot[:], in1=ssq[:].broadcast_to([128, 8, 128]), op=ALU.mult), r=[tot, t_ssq], w=[tot])
            otf = ot[:].rearrange("p h d -> p (h d)")
            k.op("pool", lambda e, otf=otf: e.tensor_tensor(out=otf, in0=otf, in1=gnb[:], op=ALU.mult), r=[tot, tc_], w=[tot])
            yield
            mx, tmx = MX.next()
            k.op("dve", lambda e, otf=otf, mx=mx, ga=ga: e.tensor_tensor(out=mx[:], in0=otf, in1=ga[:], op=ALU.mult), r=[tot, tga], w=[tmx])
            p, tp = PF.next()
            pb = p[:].bitcast(BF16).rearrange("p (s d) -> p s d", d=128)
            for kc in range(8):
                k.op("pe", lambda e, pb=pb, kc=kc, mx=mx: e.transpose(pb[:, kc, :], mx[:, kc * 128:(kc + 1) * 128], c["idb"][:]), r=[tmx, c["t_id"]], w=[tp])
            mt, tmt = MXT.next()
            k.op("act", lambda e, mt=mt, pb=pb: e.copy(out=mt[:], in_=pb), r=[tp], w=[tmt])
            for half in range(2):
                p, tp = PF.next()
                for kc in range(8):
                    k.op("pe", lambda e, p=p, kc=kc, mt=mt, half=half: e.matmul(p[:], lhsT=mt[:, kc, :], rhs=wout[:, kc, half * 512:(half + 1) * 512],
                                                                             start=(kc == 0), stop=(kc == 7)), r=[tmt, tc_], w=[tp])
                k.op("dve", lambda e, p=p, z=z, xr=xr, half=half: e.scalar_tensor_tensor(out=z[:, half * 512:(half + 1) * 512], in0=xr[:, half * 512:(half + 1) * 512],
                                                                                      scalar=ALPHA, in1=p[:], op0=ALU.mult, op1=ALU.add), r=[tp, txr], w=[tz])
            ho, tho = HO.next()
            emit_ln(k, z, tz, lgb, lbb, tc_, lst, t_lst, ho, tho)
            k.dma("sp", P["h_out"][r0:r0 + 128, :], ho[:], tho, r=[tho])
        yield

    def drive(gens):
        gens = list(gens)
        while gens:
            for g in list(gens):
                try:
                    next(g)
                except StopIteration:
                    gens.remove(g)

    drive([gm_block(blocks[0], 0)])
    for bi, b in enumerate(blocks):
        gens = [scan_block(b, bi % NPAR)]
        if NPAR == 1:
            drive(gens)
            gens = []
        if bi + 1 < len(blocks):
            gens.append(gm_block(blocks[bi + 1], (bi + 1) % NPAR))
        drive(gens)
    if pidx == 1:
        for i in range(nsg):
            k.dma("sp", P["state_out"][i], S32[i][:], t_S[i], r=[t_S[i]])
    k.end_scope()


AB_OFF = dict(qa=0, fa0=512, fa1=1024, ia=1536, ga=2048, qb=2560, kb=2816, vb=3072, gb=3584, rb0=4096, rb1=4112)
CD_OFF = dict(qc=0, kc=256, vc=512, gc=1024, qd=1536, kd=2048, vd=2560, od=3072, gates=3584)


def _ar(a, n):
    return np.arange(a, a + n)


def prep_mixer(inp, layer, d, flipped, pos, NT):
    f32 = np.float32
    fm, sgs = mixer_layout(layer)
    out = {}
    if layer == 0:
        w, b = inp["ab_w_in"][0], inp["ab_b_in"][0]
        cols = np.concatenate([_ar(AB_OFF["qa"], 512), _ar(AB_OFF["fa%d" % d], 512), _ar(AB_OFF["qb"], 256), _ar(AB_OFF["kb"], 256),
                               _ar(AB_OFF["rb%d" % d], 16)])
        tcols = np.concatenate([_ar(AB_OFF["ia"], 512), _ar(AB_OFF["vb"], 512), _ar(AB_OFF["ga"], 512), _ar(AB_OFF["gb"], 512)])
        out["hgrn_lb"] = np.ascontiguousarray(inp["hgrn_lb"].reshape(3, 4, 128).transpose(2, 1, 0)).astype(f32)
        out["gk_up"] = np.ascontiguousarray(inp["gla_gk_up"][0, d]).astype(f32)
        out["gk_b"] = np.ascontiguousarray(inp["gla_gk_b"][0, d].reshape(4, 64).T).astype(f32)
        out["norm_g"] = np.concatenate([inp["hgrn_norm"][0], inp["gla_norm"][0]])[None].astype(f32)
        out["w_out"] = np.ascontiguousarray(inp["ab_w_out"][0]).astype(f32)
    else:
        w, b = inp["cd_w_in"][0], inp["cd_b_in"][0]

        def sw(base):
            return np.concatenate([np.concatenate([_ar(base + h * 64 + 32, 32), _ar(base + h * 64, 32)]) for h in range(4)])
        g0 = CD_OFF["gates"]
        cols = np.concatenate([_ar(CD_OFF["qc"], 256), sw(CD_OFF["qc"]), _ar(CD_OFF["kc"], 256), sw(CD_OFF["kc"]),
                               _ar(CD_OFF["qd"], 512), _ar(CD_OFF["kd"], 512), _ar(g0 + 8 * d, 8)])
        tcols = np.concatenate([_ar(CD_OFF["vc"], 512), _ar(CD_OFF["vd"], 512), _ar(CD_OFF["gc"], 512), _ar(CD_OFF["od"], 512)])
        exps = 5.0 + 2.0 * np.arange(4, dtype=f32) + f32(d)
        lg = np.log1p(-np.exp2(-exps)).astype(f32)
        out["lgam"] = np.ascontiguousarray(np.broadcast_to(lg[None, :], (64, 4))).astype(f32)
        cwt = inp["mlstm_conv_w"][0]
        if flipped:
            cwt = cwt[::-1]
        out["conv_w"] = np.ascontiguousarray(cwt.reshape(3, 8, 128).transpose(2, 1, 0)).astype(f32)
        out["conv_b"] = np.ascontiguousarray(inp["mlstm_conv_b"][0].reshape(8, 128).T).astype(f32)
        sel = np.zeros((8, 8, 128), f32)
        for r in range(8):
            sel[r, r, :] = 1.0
        out["selm"] = sel
        out["fgb"] = np.ascontiguousarray(inp["mlstm_fgate_b"][0, d])[None].astype(f32)
        out["lib"] = np.ascontiguousarray(b[g0 + 8 * d: g0 + 8 * d + 4])[None].astype(f32)
        out["lfb"] = np.ascontiguousarray(b[g0 + 8 * d + 4: g0 + 8 * d + 8])[None].astype(f32)
        half = 32
        freqs = (f32(10000.0) ** (-np.arange(half, dtype=f32) / f32(half))).astype(f32)
        ang = (pos.astype(f32)[:, None] * freqs[None, :]).astype(f32)
        cosv, sinv = np.cos(ang).astype(f32), np.sin(ang).astype(f32)
        cs = np.zeros((2, 64, NT), f32)
        cs[0, :32], cs[0, 32:] = cosv.T, cosv.T
        cs[1, :32], cs[1, 32:] = -sinv.T, sinv.T
        out["cs_tab"] = cs
        out["norm_g"] = np.concatenate([inp["ret_norm"][0], inp["mlstm_norm"][0]])[None].astype(f32)
        out["w_out"] = np.ascontiguousarray(inp["cd_w_out"][0]).astype(f32)
    out["wfm"] = np.ascontiguousarray(w[:, cols]).astype(f32)
    bf = np.zeros((128, len(fm)), f32)
    o = 0
    for gi, (nm, wd) in enumerate(fm):
        bf[:wd, gi] = b[cols[o:o + wd]]
        o += wd
    out["bfm"] = bf
    out["wtm"] = np.ascontiguousarray(w[:, tcols]).astype(f32)
    out["btm"] = np.ascontiguousarray(b[tcols])[None].astype(f32)
    out["ln_g"] = np.ascontiguousarray(inp["ln_mix_g"][layer])[None].astype(f32)
    out["ln_b"] = np.ascontiguousarray(inp["ln_mix_b"][layer])[None].astype(f32)
    return out


MIX_SHAPES = {
    0: dict(hgrn_lb=[128, 4, 3], gk_up=[16, 256], gk_b=[64, 4]),
    1: dict(lgam=[64, 4], conv_w=[128, 8, 3], conv_b=[128, 8], selm=[8, 8, 128], fgb=[1, 4], lib=[1, 4], lfb=[1, 4]),
}


def declare_mixer_inputs(nc, layer, pidx, NT, pre):
    fm, sgs = mixer_layout(layer)
    CF = sum(wd for _, wd in fm)
    P = {}

    def inp(nm, shape):
        P[nm] = nc.dram_tensor(pre + nm, list(shape), F32, kind="ExternalInput").ap()
    inp("wfm", [D, CF])
    inp("bfm", [128, len(fm)])
    inp("wtm", [D, 2048])
    inp("btm", [1, 2048])
    for nm, shp in MIX_SHAPES[layer].items():
        inp(nm, shp)
    if layer == 1:
        inp("cs_tab", [2, 64, NT])
    if pidx == 2:
        inp("norm_g", [1, D])
        inp("w_out", [D, D])
        inp("ln_g", [1, D])
        inp("ln_b", [1, D])
    return P


def build_p1(layer, NT):
    nc = bass.Bass("TRN2", target_bir_lowering=False)
    h_in = nc.dram_tensor("h_in", [NT, D], F32, kind="ExternalInput").ap()
    P = declare_mixer_inputs(nc, layer, 1, NT, "m_")
    if layer == 1:
        P["halo"] = nc.dram_tensor("halo", [D], F32, kind="ExternalInput").ap()
    P["o1"] = nc.dram_tensor("o1", [NT, D], F32, kind="ExternalOutput").ap()
    P["state_out"] = nc.dram_tensor("st_out", [8, 128, VP], F32, kind="ExternalOutput").ap()
    xt = nc.dram_tensor("xt_scr", [D, NT + 2], BF16).ap()
    k = KB(nc)
    c = make_consts(k)
    emit_transpose_phase(k, c, h_in, xt, NT)
    emit_mixer_pass(k, c, layer, 1, NT, xt, P)
    k.finish()
    return nc, k


def build_p2(layer, NT, ne=NE, TQ=1024, with_moe=True):
    nc = bass.Bass("TRN2", target_bir_lowering=False)
    h_in = nc.dram_tensor("h_in", [NT, D], F32, kind="ExternalInput").ap()
    P = declare_mixer_inputs(nc, layer, 2, NT, "m_")
    if layer == 1:
        P["halo"] = nc.dram_tensor("halo", [D], F32, kind="ExternalInput").ap()
    P["o1"] = nc.dram_tensor("o1", [NT, D], F32, kind="ExternalInput").ap()
    P["state_in"] = nc.dram_tensor("st_in", [8, 128, VP], F32, kind="ExternalInput").ap()
    P["x_tm"] = h_in
    h_out = nc.dram_tensor("h_out", [NT, D], F32, kind="ExternalOutput").ap()
    xt = nc.dram_tensor("xt_scr", [D, NT + 2], BF16).ap()
    k = KB(nc)
    c = make_consts(k)
    emit_transpose_phase(k, c, h_in, xt, NT)
    if with_moe:
        h1 = nc.dram_tensor("h1_scr", [NT, D], F32).ap()
        P["h_out"] = h1
        w1 = nc.dram_tensor("w1", [ne, D, D], F32, kind="ExternalInput").ap()
        w3 = nc.dram_tensor("w3", [ne, D, D], F32, kind="ExternalInput").ap()
        w2 = nc.dram_tensor("w2", [ne, D, D], F32, kind="ExternalInput").ap()
        rw = nc.dram_tensor("rw", [D, NE], F32, kind="ExternalInput").ap()
        rb = nc.dram_tensor("rb", [1, NE], F32, kind="ExternalInput").ap()
        fg = nc.dram_tensor("fg", [1, D], F32, kind="ExternalInput").ap()
        fb = nc.dram_tensor("fb", [1, D], F32, kind="ExternalInput").ap()
        emit_mixer_pass(k, c, layer, 2, NT, xt, P)
        emit_moe_phase(k, c, h1, h_out, w1, w3, w2, rw, rb, fg, fb, NT, TQ=min(TQ, NT), ne=ne)
    else:
        P["h_out"] = h_out
        emit_mixer_pass(k, c, layer, 2, NT, xt, P)
    k.finish()
    return nc, k


def core_rows(inp_x, NT):
    B, S, _ = inp_x.shape
    rows, poss = [], []
    for b in range(B):
        rows.append(np.ascontiguousarray(inp_x[b, :NT]))
        poss.append(np.arange(NT))
        rows.append(np.ascontiguousarray(inp_x[b, S - 1:NT - 1:-1]))
        poss.append(S - 1 - np.arange(NT))
    return rows, poss


def run_layers(inputs, NT, ne=NE, layers=(0, 1), with_moe=True, runner=None):
    x = np.asarray(inputs["x"], np.float32)
    B, S, _ = x.shape
    ncores = 2 * B
    inp = {k_: np.asarray(v, np.float32) for k_, v in inputs.items()}
    h, poss = core_rows(x, NT)
    run = runner or (lambda nc, maps: run_bass_kernel_spmd(nc, maps, core_ids=list(range(ncores))).results)
    for layer in layers:
        halos = [h[c ^ 1][NT - 1].copy() for c in range(ncores)]
        nc1, _ = build_p1(layer, NT)
        maps = []
        for c in range(ncores):
            s = c % 2
            m = {"m_" + k_: v for k_, v in prep_mixer(inp, layer, s, s == 1, poss[c], NT).items()
                 if k_ not in ("norm_g", "w_out", "ln_g", "ln_b")}
            m["h_in"] = h[c]
            if layer == 1:
                m["halo"] = halos[c]
            maps.append(m)
        r1 = run(nc1, maps)
        run_layers.dbg.setdefault("r1", []).append(r1)
        nc2, _ = build_p2(layer, NT, ne=ne, with_moe=with_moe)
        maps = []
        for c in range(ncores):
            s = c % 2
            m = {"m_" + k_: v for k_, v in prep_mixer(inp, layer, 1 - s, s == 1, poss[c], NT).items()}
            m["h_in"] = h[c]
            if layer == 1:
                m["halo"] = halos[c]
            m["o1"] = r1[c]["o1"]
            m["st_in"] = r1[c ^ 1]["st_out"]
            if with_moe:
                m["w1"] = inp["moe_w1"][layer][:ne]
                m["w3"] = inp["moe_w3"][layer][:ne]
                m["w2"] = inp["moe_w2"][layer][:ne]
                m["rw"] = inp["router_w"]
                m["rb"] = inp["router_b"][None]
                m["fg"] = inp["ln_ffn_g"][layer][None]
                m["fb"] = inp["ln_ffn_b"][layer][None]
            maps.append(m)
        r2 = run(nc2, maps)
        h = [np.asarray(r2[c]["h_out"]) for c in range(ncores)]
        run_layers.dbg.setdefault("h", []).append(h)
    out = np.zeros((B, S, D), np.float32)
    for b in range(B):
        out[b, :NT] = h[2 * b]
        out[b, S - 1:NT - 1:-1] = h[2 * b + 1]
    return out


PAIRS = [[0, 1], [2, 3], [4, 5], [6, 7]]


def build_fused(NT, ne=NE, TQ=1024, groups=PAIRS, debug=False):
    nc = bass.Bass("TRN2", target_bir_lowering=False)
    x_in = nc.dram_tensor("x_in", [NT, D], F32, kind="ExternalInput").ap()
    sel = nc.dram_tensor("sel", [1, 2], F32, kind="ExternalInput").ap()
    out = nc.dram_tensor("out", [NT, D], F32, kind="ExternalOutput").ap()
    rw = nc.dram_tensor("rw", [D, NE], F32, kind="ExternalInput").ap()
    rb = nc.dram_tensor("rb", [1, NE], F32, kind="ExternalInput").ap()
    xt = nc.dram_tensor("xt_scr", [D, NT + 2], BF16).ap()
    h1 = nc.dram_tensor("h1_scr", [NT, D], F32).ap()
    hA = nc.dram_tensor("hA_scr", [NT, D], F32).ap()
    o1 = nc.dram_tensor("o1_scr", [NT, D], F32).ap()
    st_mine = nc.dram_tensor("st_mine", [8 * 128, VP], F32)
    st_pair = nc.dram_tensor("st_pair", [2 * 8 * 128, VP], F32)
    hl_mine = nc.dram_tensor("hl_mine", [1, D], F32)
    hl_pair = nc.dram_tensor("hl_pair", [2, D], F32)
    k = KB(nc)
    c = make_consts(k)
    for _ in range(globals().get("SALT", 0)):
        k.op("pool", lambda e: e.memset(c["idb"][0:1, 0:1], 1.0), r=[c["t_id"]], w=[c["t_id"]])
    t_cc = Tok("cc")
    t_hl = Tok("hl")
    h_cur = x_in
    for layer in range(2):
        h_nxt = hA if layer == 0 else out
        P1 = declare_mixer_inputs(nc, layer, 1, NT, "m%d1_" % layer)
        P2 = declare_mixer_inputs(nc, layer, 2, NT, "m%d2_" % layer)
        for P in (P1, P2):
            P["sel"] = sel
            P["o1"] = o1
            P["cc_tok"] = [t_cc]
            if layer == 1:
                P["halo_pair"] = hl_pair.ap()
        P1["state_out"] = st_mine.ap().rearrange("(s p) v -> s p v", p=128)
        P2["state_pair"] = st_pair.ap().rearrange("(r s p) v -> r s p v", r=2, p=128)
        P2["x_tm"] = h_cur
        P2["h_out"] = h1
        w1 = nc.dram_tensor("w1_%d" % layer, [ne, D, D], F32, kind="ExternalInput").ap()
        w3 = nc.dram_tensor("w3_%d" % layer, [ne, D, D], F32, kind="ExternalInput").ap()
        w2 = nc.dram_tensor("w2_%d" % layer, [ne, D, D], F32, kind="ExternalInput").ap()
        fg = nc.dram_tensor("fg_%d" % layer, [1, D], F32, kind="ExternalInput").ap()
        fb = nc.dram_tensor("fb_%d" % layer, [1, D], F32, kind="ExternalInput").ap()
        emit_transpose_phase(k, c, h_cur, xt, NT)
        emit_mixer_pass(k, c, layer, 1, NT, xt, P1)
        k.coll("AllGather", [st_mine.ap().opt()], [st_pair.ap().opt()], groups, t_cc, w=[t_cc])
        if debug and layer == 0:
            dbg = nc.dram_tensor("dbg_st", [2 * 8 * 128, VP], F32, kind="ExternalOutput").ap()
            dbg2 = nc.dram_tensor("dbg_o1", [NT, D], F32, kind="ExternalOutput").ap()
            td = Tok("dbg")
            k.dma("sp", dbg, st_pair.ap(), td, r=[t_cc], w=[td])
            k.dma("sp", dbg2, o1, td, w=[td])
        emit_mixer_pass(k, c, layer, 2, NT, xt, P2)
        if debug and layer == 0:
            dbg3 = nc.dram_tensor("dbg_h1", [NT, D], F32, kind="ExternalOutput").ap()
            k.dma("sp", dbg3, h1, td, w=[td])
        emit_moe_phase(k, c, h1, h_nxt, w1, w3, w2, rw, rb, fg, fb, NT, TQ=min(TQ, NT), ne=ne)
        if debug and layer == 0:
            dbg4 = nc.dram_tensor("dbg_hA", [NT, D], F32, kind="ExternalOutput").ap()
            k.dma("sp", dbg4, hA, td, w=[td])
        if layer == 0:
            k.dma("sp", hl_mine.ap(), hA[NT - 1:NT, :], t_hl, w=[t_hl])
            k.coll("AllGather", [hl_mine.ap().opt()], [hl_pair.ap().opt()], groups, t_cc, r=[t_hl], w=[t_cc])
        h_cur = h_nxt
    if globals().get("LATE_DEBUG", 0):
        td = Tok("dbgl")
        for nm, src in (("dbg_hA", hA), ("dbg_h1", h1), ("dbg_o1", o1)):
            dd = nc.dram_tensor(nm, [NT, D], F32, kind="ExternalOutput").ap()
            k.dma("sp", dd, src, td, w=[td])
        dd = nc.dram_tensor("dbg_st", [2 * 8 * 128, VP], F32, kind="ExternalOutput").ap()
        k.dma("sp", dd, st_pair.ap(), td, w=[td])
        dd = nc.dram_tensor("dbg_hl", [2, D], F32, kind="ExternalOutput").ap()
        k.dma("sp", dd, hl_pair.ap(), td, w=[td])
    k.finish()
    return nc, k


def fused_maps(inputs, NT, ne=NE):
    x = np.asarray(inputs["x"], np.float32)
    B, S, _ = x.shape
    ncores = 2 * B
    inp = {k_: np.asarray(v, np.float32) for k_, v in inputs.items()}
    h, poss = core_rows(x, NT)
    maps = []
    shared = {"rw": inp["router_w"], "rb": inp["router_b"][None]}
    for layer in range(2):
        shared["w1_%d" % layer] = inp["moe_w1"][layer][:ne]
        shared["w3_%d" % layer] = inp["moe_w3"][layer][:ne]
        shared["w2_%d" % layer] = inp["moe_w2"][layer][:ne]
        shared["fg_%d" % layer] = inp["ln_ffn_g"][layer][None]
        shared["fb_%d" % layer] = inp["ln_ffn_b"][layer][None]
    for c in range(ncores):
        s = c % 2
        m = dict(shared)
        m["x_in"] = h[c]
        m["sel"] = np.array([[1.0, 0.0]] if s == 1 else [[0.0, 1.0]], np.float32)
        for layer in range(2):
            for pidx, d in ((1, s), (2, 1 - s)):
                pm = prep_mixer(inp, layer, d, s == 1, poss[c], NT)
                for k_, v in pm.items():
                    if pidx == 1 and k_ in ("norm_g", "w_out", "ln_g", "ln_b"):
                        continue
                    m["m%d%d_%s" % (layer, pidx, k_)] = v
        maps.append(m)
    return maps, B, S


def run_fused(inputs, NT, ne=NE, TQ=1024, debug=False):
    maps, B, S = fused_maps(inputs, NT, ne)
    ncores = 2 * B
    nc, _ = build_fused(NT, ne=ne, TQ=TQ, groups=[[2 * b, 2 * b + 1] for b in range(B)], debug=debug)
    res = run_bass_kernel_spmd(nc, maps, core_ids=list(range(ncores))).results
    if debug:
        run_fused.dbg = res
    out = np.zeros((B, S, D), np.float32)
    for b in range(B):
        out[b, :NT] = res[2 * b]["out"]
        out[b, S - 1:NT - 1:-1] = res[2 * b + 1]["out"]
    return out


run_layers.dbg = {}


def kernel(**inputs):
    return run_fused(inputs, 4096)
```

```python
from contextlib import ExitStack
import types
import numpy as np
import concourse.bass as bass
import concourse.mybir as mybir
from concourse.bass_utils import run_bass_kernel_spmd

F32 = mybir.dt.float32
BF16 = mybir.dt.bfloat16
AF = mybir.ActivationFunctionType
ALU = mybir.AluOpType
AX = mybir.AxisListType

ENGS = ("pe", "dve", "act", "pool", "sp")
D = 1024
NE = 16
ALPHA = (2.0 * 2) ** 0.25
LN_EPS = 1e-5
NORM_EPS = 1e-6


def _snap(fn):
    if fn.__closure__ is None:
        return fn
    cells = []
    for c_ in fn.__closure__:
        try:
            cells.append(types.CellType(c_.cell_contents))
        except ValueError:
            cells.append(c_)
    return types.FunctionType(fn.__code__, fn.__globals__, fn.__name__, fn.__defaults__, tuple(cells))


class Tok:
    __slots__ = ("name", "last_w", "readers", "sem", "semcnt", "excl", "uid")
    _n = [0]

    def __init__(self, name="", excl=False):
        Tok._n[0] += 1
        self.uid = Tok._n[0]
        self.name = name
        self.excl = excl
        self.last_w = None
        self.readers = []
        self.sem = None
        self.semcnt = 0


class KB:
    def __init__(self, nc, same_eng_sync=None):
        if same_eng_sync is None:
            same_eng_sync = globals().get("SAME_ENG_SYNC", ("dve", "act", "pool"))
        self.nc = nc
        self.ctx = ExitStack()
        self.prog = {e: [] for e in ENGS}
        self.seen = {e: {} for e in ENGS}
        self.same = set(same_eng_sync)
        self.sems = {}
        self.dma_toks = []
        self.nsem = 0
        self.ninst = 0
        self.scopes = []
        self.scope_dma = []
        self.free_sems = {"sw": [], "hw": [], "cc": []}
        self.uid = 0
        self.phase = -1
        self.new_phase()

    def new_phase(self):
        self.phase += 1
        self.esem = {}
        for e in ENGS:
            if e == "sp":
                continue
            self.esem[e] = self.ctx.enter_context(self.nc.semaphore("s_%s%d" % (e, self.phase)))
            self.sems[("e", e, self.phase)] = self.esem[e]
            self.nsem += 1
        self.ecnt = {e: 0 for e in ENGS}

    def scope(self):
        st = ExitStack()
        self.scopes.append(st)
        return st

    def end_scope(self):
        self.barrier()
        self.scopes.pop().close()
        for (owner, cls) in self.scope_dma:
            self.free_sems[cls].append((owner.sem[cls], owner.semcnt[cls]))
            self.dma_toks.remove((owner, cls))
        self.scope_dma = []
        self.new_phase()

    def _dma_sem(self, owner, cls):
        if owner.sem is None:
            owner.sem = {}
            owner.semcnt = {}
        if cls not in owner.sem:
            if self.free_sems[cls]:
                owner.sem[cls], owner.semcnt[cls] = self.free_sems[cls].pop()
            else:
                owner.sem[cls] = self.ctx.enter_context(self.nc.semaphore("d%d" % self.nsem))
                owner.semcnt[cls] = 0
                self.nsem += 1
            self.sems[("d", owner.uid, cls)] = owner.sem[cls]
            self.dma_toks.append((owner, cls))
            if self.scopes:
                self.scope_dma.append((owner, cls))

    def sb(self, name, shape, dtype):
        self.uid += 1
        st = self.scopes[-1] if self.scopes else self.ctx
        return st.enter_context(self.nc.sbuf_tensor("%s_%d" % (name, self.uid), list(shape), dtype))

    def ps(self, name, shape, dtype=F32):
        self.uid += 1
        st = self.scopes[-1] if self.scopes else self.ctx
        return st.enter_context(self.nc.psum_tensor("%s_%d" % (name, self.uid), list(shape), dtype))

    def _need(self, eng, waits, ev):
        if ev is None:
            return
        key, val = ev
        if key[0] == "e" and key[1] == eng and eng not in self.same:
            return
        if self.seen[eng].get(key, 0) >= val:
            return
        if waits.get(key, 0) < val:
            waits[key] = val

    def _emit_waits(self, eng, waits):
        for key, val in waits.items():
            self.prog[eng].append(("wait", self.sems[key], val))
            self.seen[eng][key] = val

    def _deps(self, eng, r, w):
        waits = {}
        for t in r:
            self._need(eng, waits, t.last_w)
        for t in w:
            self._need(eng, waits, t.last_w)
            for ev in t.readers:
                self._need(eng, waits, ev)
        self._emit_waits(eng, waits)

    def op(self, eng, fn, r=(), w=()):
        ex = [t for t in r if t.excl]
        if ex:
            r = [t for t in r if not t.excl]
            w = list(w) + [t for t in ex if t not in w]
        self._deps(eng, r, w)
        self.ecnt[eng] += 1
        ev = (("e", eng, self.phase), self.ecnt[eng])
        self.prog[eng].append(("op", _snap(fn), self.esem[eng], 1))
        for t in r:
            t.readers.append(ev)
        for t in w:
            t.last_w = ev
            t.readers = []
        self.ninst += 1

    def dma(self, eng, out, in_, owner, r=(), w=(), **kw):
        self._deps(eng, r, w)
        cls = "sw" if eng == "pool" else "hw"
        self._dma_sem(owner, cls)
        owner.semcnt[cls] += 16
        ev = (("d", owner.uid, cls), owner.semcnt[cls])
        self.prog[eng].append(("op", lambda e, o=out, i=in_, k=kw: e.dma_start(out=o, in_=i, **k), owner.sem[cls], 16))
        for t in r:
            t.readers.append(ev)
        for t in w:
            t.last_w = ev
            t.readers = []
        self.ninst += 1

    def coll(self, kind, ins, outs, groups, owner, r=(), w=()):
        self._deps("pool", r, w)
        cls = "cc"
        self._dma_sem(owner, cls)
        owner.semcnt[cls] += 1
        ev = (("d", owner.uid, cls), owner.semcnt[cls])
        self.prog["pool"].append(("op", lambda e: e.collective_compute(kind, ALU.bypass, replica_groups=groups, ins=ins, outs=outs),
                                  owner.sem[cls], 1))
        for t in r:
            t.readers.append(ev)
        for t in w:
            t.last_w = ev
            t.readers = []
        self.ninst += 1

    def barrier(self):
        for eng in ENGS:
            waits = {}
            for e2 in ENGS:
                if e2 != eng and self.ecnt[e2] > 0:
                    self._need(eng, waits, (("e", e2, self.phase), self.ecnt[e2]))
            for (t, cls) in self.dma_toks:
                self._need(eng, waits, (("d", t.uid, cls), t.semcnt[cls]))
            self._emit_waits(eng, waits)

    def finish(self):
        self.barrier()
        nc = self.nc
        engmap = {"pe": "tensor", "dve": "vector", "act": "scalar", "pool": "gpsimd", "sp": "sync"}
        with nc.Block() as block:
            for e in ENGS:
                items = self.prog[e]

                def body(engobj, items=items):
                    for it in items:
                        if it[0] == "wait":
                            engobj.wait_ge(it[1], it[2])
                        else:
                            it[1](engobj).then_inc(it[2], it[3])
                getattr(block, engmap[e])(body)
        self.ctx.close()


class Bufs:
    def __init__(self, k, name, shape, dtype, n, space="sb"):
        mk = k.sb if space == "sb" else k.ps
        self.t = [mk(name, shape, dtype) for _ in range(n)]
        self.tok = [Tok(name, excl=(space == "ps")) for _ in range(n)]
        self.i = -1

    def next(self):
        self.i = (self.i + 1) % len(self.t)
        return self.t[self.i], self.tok[self.i]


def make_consts(k):
    c = {}
    idf = k.sb("identf", [128, 128], F32)
    idb = k.sb("identb", [128, 128], BF16)
    t = Tok("ident")
    k.op("pool", lambda e: e.memset(idf[:], 0.0), w=[t])
    k.op("pool", lambda e: e.affine_select(out=idf[:], in_=idf[:], pattern=[[-1, 128]], compare_op=ALU.not_equal,
                                           fill=1.0, base=0, channel_multiplier=1), r=[t], w=[t])
    k.op("pool", lambda e: e.tensor_copy(idb[:], idf[:]), r=[t], w=[t])
    c["idf"], c["idb"], c["t_id"] = idf, idb, t
    return c


def emit_transpose_phase(k, c, h_tm, xt_fm, NT):
    k.scope()
    ntile = NT // 128
    hb = Bufs(k, "tp_h", [128, D], BF16, 2)
    pt = Bufs(k, "tp_pt", [128, 8, 128], BF16, 2, "ps")
    ob = Bufs(k, "tp_o", [128, 8, 128], BF16, 2)
    zc = k.sb("tp_z", [128, 8, 1], BF16)
    tzc = Tok("tp_z")
    k.op("pool", lambda e: e.memset(zc[:], 0.0), w=[tzc])
    xv = xt_fm.rearrange("(kc p) t -> p kc t", p=128)
    for col in (0, NT + 1):
        k.dma("sp", xv[:, :, col:col + 1], zc[:], tzc, r=[tzc], allow_slow_non_contiguous=True)
    for tt in range(ntile):
        h, th = hb.next()
        k.dma("pool", h[:], h_tm[tt * 128:(tt + 1) * 128, :], th, w=[th])
        p, tp = pt.next()
        for kc in range(8):
            k.op("pe", lambda e, p=p, h=h, kc=kc: e.transpose(p[:, kc, :], h[:, kc * 128:(kc + 1) * 128], c["idb"][:]),
                 r=[th, c["t_id"]], w=[tp])
        o, to = ob.next()
        k.op("act" if tt % 2 else "dve",
             (lambda e, o=o, p=p: e.copy(out=o[:], in_=p[:])) if tt % 2 else (lambda e, o=o, p=p: e.tensor_copy(o[:], p[:])),
             r=[tp], w=[to])
        dst = xt_fm.rearrange("(kc p) t -> p kc t", p=128)[:, :, 1 + tt * 128: 1 + (tt + 1) * 128]
        k.dma("sp", dst, o[:], to, r=[to])
    k.end_scope()


def emit_ln(k, z, tz, gbc, bbc, tgb, st, tst, out, tout, eng2="pool"):
    for hh in range(2):
        k.op("dve", lambda e, hh=hh: e.bn_stats(st[:, hh * 6:(hh + 1) * 6], z[:, hh * 512:(hh + 1) * 512]), r=[tz], w=[tst])
    k.op("dve", lambda e: e.bn_aggr(st[:, 12:14], st[:, 0:12]), r=[tst], w=[tst])
    k.op("act", lambda e: e.activation(out=st[:, 14:15], in_=st[:, 13:14], func=AF.Ln, bias=LN_EPS, scale=1.0), r=[tst], w=[tst])
    k.op("act", lambda e: e.activation(out=st[:, 14:15], in_=st[:, 14:15], func=AF.Exp, scale=-0.5), r=[tst], w=[tst])
    k.op("dve", lambda e: e.tensor_scalar(out=z[:], in0=z[:], scalar1=st[:, 12:13], scalar2=st[:, 14:15],
                                          op0=ALU.subtract, op1=ALU.mult), r=[tz, tst], w=[tz])
    k.op(eng2, lambda e: e.tensor_tensor(out=z[:], in0=z[:], in1=gbc[:], op=ALU.mult), r=[tz, tgb], w=[tz])
    k.op(eng2, lambda e: e.tensor_tensor(out=out[:], in0=z[:], in1=bbc[:], op=ALU.add), r=[tz, tgb], w=[tout])


def emit_moe_phase(k, c, h_tm, out_tm, w1, w3, w2, router_w, router_b, ln_g, ln_b, NT, TQ=1024, ne=NE):
    k.scope()
    nq = NT // TQ
    ntile = TQ // 128
    nblk = TQ // 512
    tconst = Tok("moe_const")
    rw = k.sb("rw", [128, 8, NE], F32)
    k.dma("sp", rw[:], router_w.rearrange("(kc p) e -> p kc e", p=128), tconst, w=[tconst])
    rb = k.sb("rb", [128, NE], F32)
    k.dma("sp", rb[:], router_b.partition_broadcast(128), tconst, w=[tconst])
    gbc = k.sb("gbc", [128, D], F32)
    bbc = k.sb("bbc", [128, D], F32)
    k.dma("sp", gbc[:], ln_g.partition_broadcast(128), tconst, w=[tconst])
    k.dma("sp", bbc[:], ln_b.partition_broadcast(128), tconst, w=[tconst])

    xT = k.sb("xT", [128, 8, TQ], BF16)
    t_xT = [Tok("xT%d" % i) for i in range(ntile)]
    acc = k.sb("acc", [128, ntile, D], F32)
    t_acc = [Tok("acc%d" % i) for i in range(ntile)]
    lg = k.sb("lg", [128, ntile, NE], F32)
    t_lg = Tok("lg")
    gates = k.sb("gates", [128, ntile, NE], F32)
    t_gates = Tok("gates")
    hst = Bufs(k, "hst", [128, D], F32, 2)
    xTf = Bufs(k, "xTf", [128, 8, 128], F32, 2)
    ptr = Bufs(k, "ptr", [128, 4, 128], F32, 1, "ps")
    plg = Bufs(k, "plg", [128, 512], F32, 1, "ps")
    p13 = Bufs(k, "p13", [128, 2, 512], F32, 2, "ps")
    py = Bufs(k, "py", [128, 512], F32, 2, "ps")
    wst = Bufs(k, "wst", [128, 2, D], F32, MOE_NWST)
    wring = Bufs(k, "wring", [128, 8, D], BF16, MOE_NRING)
    hid = Bufs(k, "hid", [128, 8, 512], BF16, 2)
    sil = Bufs(k, "sil", [128, 512], BF16, 2)
    rt = [k.sb("rt%d" % i, [128, ntile, NE], F32) for i in range(3)]
    rs = [k.sb("rs%d" % i, [128, ntile, 4], F32) for i in range(4)]
    t_rt = Tok("rt")
    st = k.sb("lnst", [128, 16], F32)
    t_st = Tok("lnst")
    ob = Bufs(k, "moe_o", [128, D], F32, 2)
    gmx = k.sb("gmx", [128, ntile, 1], F32)

    def load_w(wd, e, direct=False):
        wb, twb = wring.next()
        src = wd[e].rearrange("(kc p) n -> p kc n", p=128)
        if direct:
            for j in range(MOE_DSPLIT):
                n_ = 8 // MOE_DSPLIT
                k.dma("pool", wb[:, n_ * j:n_ * (j + 1), :], src[:, n_ * j:n_ * (j + 1), :], twb, w=[twb])
            return wb, twb
        for j in range(4):
            s, ts = wst.next()
            k.dma("sp", s[:], src[:, 2 * j:2 * j + 2, :], ts, w=[ts])
            if j == 3:
                k.op("pool", lambda e_, wb=wb, s=s, j=j: e_.tensor_copy(wb[:, 2 * j:2 * j + 2, :], s[:]), r=[ts], w=[twb])
            else:
                k.op("act", lambda e_, wb=wb, s=s, j=j: e_.copy(out=wb[:, 2 * j:2 * j + 2, :], in_=s[:]), r=[ts], w=[twb])
        return wb, twb

    for q in range(nq):
        t0 = q * TQ
        for tt in range(ntile):
            h, th = hst.next()
            k.dma("sp", h[:], h_tm[t0 + tt * 128: t0 + (tt + 1) * 128, :], th, w=[th])
            k.op("act", lambda e, h=h, tt=tt: e.mul(out=acc[:, tt, :], in_=h[:], mul=ALPHA), r=[th], w=[t_acc[tt]])
            xf, txf = xTf.next()
            for half in range(2):
                p, tp = ptr.next()
                for j in range(4):
                    kc = half * 4 + j
                    k.op("pe", lambda e, p=p, h=h, j=j, kc=kc: e.transpose(p[:, j, :], h[:, kc * 128:(kc + 1) * 128], c["idf"][:]),
                         r=[th, c["t_id"]], w=[tp])
                k.op("act", lambda e, xf=xf, p=p, half=half: e.copy(out=xf[:, half * 4:(half + 1) * 4, :], in_=p[:]), r=[tp], w=[txf])
                k.op("pool", lambda e, xf=xf, half=half, tt=tt: e.tensor_copy(xT[:, half * 4:(half + 1) * 4, tt * 128:(tt + 1) * 128],
                                                                             xf[:, half * 4:(half + 1) * 4, :]), r=[txf], w=[t_xT[tt]])
            pl, tpl = plg.next()
            for kc in range(8):
                k.op("pe", lambda e, pl=pl, xf=xf, kc=kc: e.matmul(pl[:, 0:NE], lhsT=xf[:, kc, :], rhs=rw[:, kc, :], start=(kc == 0), stop=(kc == 7)),
                     r=[txf, tconst], w=[tpl])
            k.op("dve", lambda e, pl=pl, tt=tt: e.tensor_copy(lg[:, tt, :], pl[:, 0:NE]), r=[tpl], w=[t_lg])
        R = [t_lg, t_rt, t_gates, tconst]

        def dv(fn):
            k.op("dve", fn, r=R, w=[t_rt, t_gates])
        mx, sm, den = rs[0][:, :, 0:1], rs[1][:, :, 0:1], rs[2][:, :, 0:1]
        bc16 = lambda a: a.broadcast_to([128, ntile, NE])
        dv(lambda e: e.tensor_reduce(out=mx, in_=lg[:], axis=AX.X, op=ALU.max))
        dv(lambda e: e.tensor_tensor(out=rt[0][:], in0=lg[:], in1=bc16(mx), op=ALU.subtract))
        k.op("act", lambda e: e.activation(out=rt[0][:], in_=rt[0][:], func=AF.Exp), r=R, w=[t_rt])
        dv(lambda e: e.tensor_reduce(out=sm, in_=rt[0][:], axis=AX.X, op=ALU.add))
        dv(lambda e: e.reciprocal(out=sm, in_=sm))
        dv(lambda e: e.tensor_tensor(out=rt[0][:], in0=rt[0][:], in1=bc16(sm), op=ALU.mult))
        dv(lambda e: e.tensor_tensor(out=rt[1][:], in0=rt[0][:], in1=rb[:].unsqueeze(1).broadcast_to([128, ntile, NE]), op=ALU.add))
        b4 = rt[1][:].rearrange("p t (g e) -> p t g e", e=4)
        w4 = rt[2][:].rearrange("p t (g e) -> p t g e", e=4)
        bc4 = lambda a: a.unsqueeze(3).broadcast_to([128, ntile, 4, 4])
        dv(lambda e: e.tensor_reduce(out=rs[0][:], in_=b4, axis=AX.X, op=ALU.max))
        dv(lambda e: e.tensor_tensor(out=w4, in0=b4, in1=bc4(rs[0][:]), op=ALU.is_equal))
        dv(lambda e: e.scalar_tensor_tensor(out=rt[2][:], in0=rt[2][:], scalar=-1e9, in1=rt[1][:], op0=ALU.mult, op1=ALU.add))
        dv(lambda e: e.tensor_reduce(out=rs[1][:], in_=w4, axis=AX.X, op=ALU.max))
        dv(lambda e: e.tensor_tensor(out=rs[2][:], in0=rs[0][:], in1=rs[1][:], op=ALU.add))
        dv(lambda e: e.tensor_reduce(out=gmx[:], in_=rs[2][:], axis=AX.X, op=ALU.max))
        dv(lambda e: e.tensor_tensor(out=rs[3][:], in0=rs[2][:], in1=gmx[:].broadcast_to([128, ntile, 4]), op=ALU.is_equal))
        dv(lambda e: e.tensor_tensor(out=w4, in0=b4, in1=bc4(rs[1][:]), op=ALU.is_ge))
        dv(lambda e: e.tensor_tensor(out=w4, in0=w4, in1=bc4(rs[3][:]), op=ALU.mult))
        dv(lambda e: e.tensor_tensor(out=rt[2][:], in0=rt[2][:], in1=rt[0][:], op=ALU.mult))
        dv(lambda e: e.tensor_reduce(out=den, in_=rt[2][:], axis=AX.X, op=ALU.add))
        dv(lambda e: e.reciprocal(out=den, in_=den))
        dv(lambda e: e.tensor_tensor(out=gates[:], in0=rt[2][:], in1=bc16(den), op=ALU.mult))
        for ex in range(ne):
            w1b, tw1 = load_w(w1, ex, direct=MOE_DIRECT_ALL)
            w3b, tw3 = load_w(w3, ex, direct=MOE_DIRECT_ALL)
            w2b, tw2 = load_w(w2, ex, direct=MOE_DIRECT_W2)
            for tb in range(nblk):
                hd, thd = hid.next()
                xtoks = t_xT[tb * 4:(tb + 1) * 4]
                for cc in range(8):
                    p, tp = p13.next()
                    for (wi, wb, tw) in ((0, w1b, tw1), (1, w3b, tw3)):
                        for kc in range(8):
                            k.op("pe", lambda e, p=p, wi=wi, wb=wb, kc=kc, cc=cc, tb=tb: e.matmul(
                                p[:, wi, :], lhsT=wb[:, kc, cc * 128:(cc + 1) * 128], rhs=xT[:, kc, tb * 512:(tb + 1) * 512],
                                start=(kc == 0), stop=(kc == 7)), r=[tw] + xtoks, w=[tp])
                    s, ts = sil.next()
                    k.op("act", lambda e, s=s, p=p: e.activation(out=s[:], in_=p[:, 0, :], func=AF.Silu), r=[tp], w=[ts])
                    k.op("dve", lambda e, hd=hd, cc=cc, s=s, p=p: e.tensor_tensor(out=hd[:, cc, :], in0=s[:], in1=p[:, 1, :], op=ALU.mult),
                         r=[ts, tp], w=[thd])
                for t4 in range(4):
                    tt = tb * 4 + t4
                    for half in range(2):
                        y, ty = py.next()
                        for cc in range(8):
                            k.op("pe", lambda e, y=y, hd=hd, cc=cc, t4=t4, half=half, w2b=w2b: e.matmul(
                                y[:], lhsT=hd[:, cc, t4 * 128:(t4 + 1) * 128], rhs=w2b[:, cc, half * 512:(half + 1) * 512],
                                start=(cc == 0), stop=(cc == 7)), r=[thd, tw2], w=[ty])
                        k.op("dve", lambda e, y=y, tt=tt, half=half, ex=ex: e.scalar_tensor_tensor(
                            out=acc[:, tt, half * 512:(half + 1) * 512], in0=y[:], scalar=gates[:, tt, ex:ex + 1],
                            in1=acc[:, tt, half * 512:(half + 1) * 512], op0=ALU.mult, op1=ALU.add),
                            r=[ty, t_gates, t_acc[tt]], w=[t_acc[tt]])
        for tt in range(ntile):
            o, to = ob.next()
            emit_ln(k, acc[:, tt, :], t_acc[tt], gbc, bbc, tconst, st, t_st, o, to)
            k.dma("sp", out_tm[t0 + tt * 128: t0 + (tt + 1) * 128, :], o[:], to, r=[to])
    k.end_scope()


MOE_DIRECT_W2 = True
MOE_DIRECT_ALL = True
MOE_DSPLIT = 1
MOE_NWST = 1
MOE_NRING = 6
L = 64
VP = 130


def mixer_layout(layer):
    fm = []
    sgs = []
    if layer == 0:
        for h in range(4):
            fm.append(("qa%d" % h, 128))
        for h in range(4):
            fm.append(("fa%d" % h, 128))
        for h in range(4):
            fm.append(("qb%d" % h, 64))
        for h in range(4):
            fm.append(("kb%d" % h, 64))
        fm.append(("rb", 16))
        for h in range(4):
            sgs.append(dict(typ="A", h=h, dk=128, hv=h, qscale=1.0, lfscale=1.0, dve=128))
        for h in range(4):
            sgs.append(dict(typ="B", h=h, dk=64, hv=4 + h, qscale=0.125, lfscale=-1.0 / 16.0, dve=128))
    else:
        for nm in ("qc", "qs", "kc", "ks"):
            for h in range(4):
                fm.append(("%s%d" % (nm, h), 64))
        for h in range(4):
            fm.append(("qd%d" % h, 128))
        for h in range(4):
            fm.append(("kd%d" % h, 128))
        fm.append(("gd", 8))
        for h in range(4):
            sgs.append(dict(typ="C", h=h, dk=64, hv=h, qscale=0.125, lfscale=1.0, dve=128))
        for h in range(4):
            sgs.append(dict(typ="D", h=h, dk=128, hv=4 + h, qscale=128.0 ** -0.5, lfscale=-1.0, dve=129))
    return fm, sgs


def emit_mixer_pass(k, c, layer, pidx, NT, xt_fm, P, T=256):
    k.scope()
    fm, sgs = mixer_layout(layer)
    asc = (pidx == 1)
    NCH = T // L
    NTL = T // 128
    nblk = NT // T
    goff = {}
    off = 0
    for gi, (nm, wd) in enumerate(fm):
        goff[nm] = (gi, off, wd)
        off += wd
    CF = off
    CT = 1024 if pidx == 1 else 2048
    tc_ = Tok("mx_const")

    wfm = k.sb("wfm", [128, 8, CF], BF16)
    for kc in range(8):
        k.dma("pool", wfm[:, kc, :], P["wfm"][kc * 128:(kc + 1) * 128, :], tc_, w=[tc_])
    bfm = k.sb("bfm", [128, len(fm)], F32)
    k.dma("sp", bfm[:], P["bfm"], tc_, w=[tc_])
    wtm = k.sb("wtm", [128, 8, CT], BF16)
    for kc in range(8):
        k.dma("pool", wtm[:, kc, :], P["wtm"][kc * 128:(kc + 1) * 128, 0:CT], tc_, w=[tc_])
    btm = k.sb("btm", [128, CT], F32)
    k.dma("sp", btm[:], P["btm"][:, 0:CT].partition_broadcast(128), tc_, w=[tc_])
    rmask = k.sb("rmask", [128, T], F32)
    k.op("pool", lambda e: e.memset(rmask[:], 1.0), w=[tc_])
    rpos = 0 if asc else L - 1
    k.op("pool", lambda e: e.memset(rmask[:].rearrange("p (c l) -> p c l", l=L)[:, :, rpos:rpos + 1], 0.0), w=[tc_])
    smask = k.sb("smask", [128, 128], F32)
    k.op("pool", lambda e: e.memset(smask[:], 1.0), w=[tc_])
    if asc:
        k.op("pool", lambda e: e.affine_select(out=smask[:], in_=smask[:], pattern=[[1, 128]], compare_op=ALU.is_ge, fill=0.0,
                                               base=0, channel_multiplier=-1), w=[tc_])
    else:
        k.op("pool", lambda e: e.affine_select(out=smask[:], in_=smask[:], pattern=[[-1, 128]], compare_op=ALU.is_ge, fill=0.0,
                                               base=0, channel_multiplier=1), w=[tc_])
    k.op("pool", lambda e: e.memset(smask[0:64, 64:128], 0.0), w=[tc_])
    k.op("pool", lambda e: e.memset(smask[64:128, 0:64], 0.0), w=[tc_])

    ex = {}
    if layer == 0:
        hl = k.sb("hl", [128, 4, 3], F32)
        k.dma("sp", hl[:], P["hgrn_lb"], tc_, w=[tc_])
        k.op("act", lambda e: e.activation(out=hl[:], in_=hl[:], func=AF.Exp), r=[tc_], w=[tc_])
        hs = k.sb("hs", [128, 4, 1], F32)
        lb = k.sb("lb", [128, 4, 1], F32)
        oml = k.sb("oml", [128, 4, 1], F32)
        k.op("dve", lambda e: e.tensor_reduce(out=hs[:], in_=hl[:], axis=AX.X, op=ALU.add), r=[tc_], w=[tc_])
        k.op("dve", lambda e: e.reciprocal(out=hs[:], in_=hs[:]), r=[tc_], w=[tc_])
        k.op("dve", lambda e: e.tensor_tensor(out=lb[:], in0=hl[:, :, 0:1], in1=hs[:], op=ALU.mult), r=[tc_], w=[tc_])
        k.op("dve", lambda e: e.tensor_scalar(out=oml[:], in0=lb[:], scalar1=-1.0, scalar2=1.0, op0=ALU.mult, op1=ALU.add), r=[tc_], w=[tc_])
        gku = k.sb("gku", [16, 256], F32)
        k.dma("sp", gku[:], P["gk_up"], tc_, w=[tc_])
        ngkb = k.sb("ngkb", [64, 4], F32)
        k.dma("sp", ngkb[:], P["gk_b"], tc_, w=[tc_])
        k.op("dve", lambda e: e.tensor_scalar(out=ngkb[:], in0=ngkb[:], scalar1=-1.0, scalar2=None, op0=ALU.mult), r=[tc_], w=[tc_])
        rbs = k.sb("rbs", [16, T], F32)
        t_rbs = Tok("rbs")
    else:
        lgam = k.sb("lgam", [64, 4], F32)
        k.dma("sp", lgam[:], P["lgam"], tc_, w=[tc_])
        cw = k.sb("cw", [128, 8, 3], F32)
        k.dma("sp", cw[:], P["conv_w"], tc_, w=[tc_])
        cb = k.sb("cb", [128, 8], F32)
        k.dma("sp", cb[:], P["conv_b"], tc_, w=[tc_])
        selm = k.sb("selm", [8, 8, 128], F32)
        k.dma("sp", selm[:], P["selm"], tc_, w=[tc_])
        nfb = k.sb("nfb", [128, 4], F32)
        k.dma("sp", nfb[:], P["fgb"].partition_broadcast(128), tc_, w=[tc_])
        lfb = k.sb("lfb", [128, 4], F32)
        k.dma("sp", lfb[:], P["lfb"].partition_broadcast(128), tc_, w=[tc_])
        k.op("dve", lambda e: e.tensor_tensor(out=nfb[:], in0=nfb[:], in1=lfb[:], op=ALU.add), r=[tc_], w=[tc_])
        k.op("dve", lambda e: e.tensor_scalar(out=nfb[:], in0=nfb[:], scalar1=-1.0, scalar2=None, op0=ALU.mult), r=[tc_], w=[tc_])
        lib = k.sb("lib", [128, 4], F32)
        k.dma("sp", lib[:], P["lib"].partition_broadcast(128), tc_, w=[tc_])
        gds = k.sb("gds", [8, T], F32)
        t_gds = Tok("gds")
        hal = k.sb("hal", [128, 8], F32)
        if "halo_pair" in P:
            hal2 = k.sb("hal2", [128, 2, 8], F32)
            for r_ in range(2):
                k.dma("sp", hal2[:, r_, :], P["halo_pair"][r_].rearrange("(kc p) -> p kc", p=128), tc_, r=P["cc_tok"], w=[tc_], allow_slow_non_contiguous=True)
            selh = k.sb("selh", [128, 2], F32)
            k.dma("sp", selh[:], P["sel"].partition_broadcast(128), tc_, w=[tc_])
            k.op("dve", lambda e: e.tensor_scalar(out=hal[:], in0=hal2[:, 0, :], scalar1=selh[:, 0:1], scalar2=None, op0=ALU.mult), r=[tc_], w=[tc_])
            k.op("dve", lambda e: e.scalar_tensor_tensor(out=hal[:], in0=hal2[:, 1, :], scalar=selh[:, 1:2], in1=hal[:], op0=ALU.mult, op1=ALU.add),
                 r=[tc_], w=[tc_])
        else:
            k.dma("sp", hal[:], P["halo"].rearrange("(kc p) -> p kc", p=128), tc_, w=[tc_], allow_slow_non_contiguous=True)
        cst = Bufs(k, "cst", [64, 2, T], F32, 2)

    PF = Bufs(k, "PF", [128, 512], F32, 2, "ps")
    PX = Bufs(k, "PX", [128, 512], F32, 3, "ps")
    PO = [k.ps("PO", [128, 512], F32) for _ in range(3)]
    t_PO = [Tok("PO%d" % i, True) for i in range(3)]
    if layer == 0:
        oslots = [(j // 4, (j % 4) * 128) for j in range(8)]
        stgroups = [[0, 1, 2, 3], [4, 5, 6, 7]]
    else:
        oslots = [(0, j * 128) for j in range(4)] + [(1, 0), (1, 256), (2, 0), (2, 256)]
        stgroups = [[0, 1, 2, 3], [4, 5], [6, 7]]
    ofirst = set()
    seenb = set()
    for j, (bk, _) in enumerate(oslots):
        if bk not in seenb:
            seenb.add(bk)
            ofirst.add(j)

    xT = Bufs(k, "mxT", [128, 8, T + 2], BF16, 2)
    NSET = globals().get("MIX_NSET") or {(0, 1): 4, (1, 1): 4, (0, 2): 4, (1, 2): 2}[(layer, pidx)]
    tsets = []
    for _ in range(NSET):
        st_ = []
        for nm_ in ("Tq", "Tk", "Tlf", "TG", "Te1", "Te2"):
            st_ += [k.sb(nm_, [128, T], F32), Tok(nm_)]
        st_ += [k.sb("Tsm", [128, 4, NCH], F32), Tok("Tsm")]
        if layer == 1:
            st_ += [k.sb("aext", [128, T + 2], F32), Tok("aext")]
        tsets.append(st_)
    nsg = len(sgs)
    NPAR = globals().get("MIX_NPAR", {(0, 1): 1, (1, 1): 1, (0, 2): 1, (1, 2): 1})[(layer, pidx)]
    QT = [[k.sb("QT", [128, T], BF16) for _ in range(nsg)] for _ in range(NPAR)]
    KT = [[k.sb("KT", [128, T], BF16) for _ in range(nsg)] for _ in range(NPAR)]
    QIP = [[k.sb("QIP", [128, NCH, 128], BF16) for _ in range(nsg)] for _ in range(NPAR)]
    KST = [[k.sb("KST", [128, T], BF16) for _ in range(nsg)] for _ in range(NPAR)]
    KS = [k.sb("KS", [128, NTL, 128], BF16) for _ in range(nsg)]
    DEC = [[k.sb("DEC", [128, NCH], F32) for _ in range(nsg)] for _ in range(NPAR)]
    t_sg = [[Tok("sg%d" % i) for i in range(nsg)] for _ in range(NPAR)]
    t_ks = [Tok("ks%d" % i) for i in range(nsg)]
    for par in range(NPAR):
        for i in range(nsg):
            k.op("pool", lambda e, i=i, par=par: e.memset(QIP[par][i][:], 0.0), w=[t_sg[par][i]])
    if "sel" in P:
        selb = k.sb("selb", [128, 2], F32)
        k.dma("sp", selb[:], P["sel"].partition_broadcast(128), tc_, w=[tc_])
        stmp = Bufs(k, "stmp", [128, 2, VP], F32, 2)
    S32 = [k.sb("S32", [128, VP], F32) for _ in range(nsg)]
    Sb = [k.sb("Sb", [128, VP], BF16) for _ in range(nsg)]
    t_S = [Tok("S%d" % i) for i in range(nsg)]
    t_Sb = [Tok("Sb%d" % i) for i in range(nsg)]
    for i in range(nsg):
        if pidx == 1:
            k.op("pool", lambda e, i=i: e.memset(S32[i][:], 0.0), w=[t_S[i]])
        elif "state_pair" in P:
            sa, tsa = stmp.next()
            k.dma("sp", sa[:], P["state_pair"][:, i].rearrange("r p v -> p r v"), tsa, r=P["cc_tok"], w=[tsa])
            k.op("dve", lambda e, i=i, sa=sa: e.tensor_scalar(out=S32[i][:], in0=sa[:, 0, :], scalar1=selb[:, 0:1], scalar2=None, op0=ALU.mult),
                 r=[tsa, tc_], w=[t_S[i]])
            k.op("dve", lambda e, i=i, sa=sa: e.scalar_tensor_tensor(out=S32[i][:], in0=sa[:, 1, :], scalar=selb[:, 1:2], in1=S32[i][:],
                                                                   op0=ALU.mult, op1=ALU.add), r=[tsa, tc_, t_S[i]], w=[t_S[i]])
        else:
            k.dma("sp", S32[i][:], P["state_in"][i], t_S[i], w=[t_S[i]])
        k.op("pool", lambda e, i=i: e.tensor_copy(Sb[i][:], S32[i][:]), r=[t_S[i]], w=[t_Sb[i]])
    V = Bufs(k, "V", [128, 8, VP], BF16, 2)
    for vb_ in V.t:
        k.op("pool", lambda e, vb_=vb_: e.memset(vb_[:], 1.0), w=[tc_])
    SCP = Bufs(k, "SCP", [128, 8, 128], BF16, 2)
    OT = Bufs(k, "OT", [128, 8, 128], F32, 2)
    rden = k.sb("rden", [128, 4, 1], F32); t_rden = Tok("rden")
    if pidx == 2:
        wout = k.sb("wout", [128, 8, D], BF16)
        for kc in range(8):
            k.dma("pool", wout[:, kc, :], P["w_out"][kc * 128:(kc + 1) * 128, :], tc_, w=[tc_])
        gnb = k.sb("gnb", [128, D], F32)
        k.dma("sp", gnb[:], P["norm_g"].partition_broadcast(128), tc_, w=[tc_])
        lgb = k.sb("lgb", [128, D], F32)
        lbb = k.sb("lbb", [128, D], F32)
        k.dma("sp", lgb[:], P["ln_g"].partition_broadcast(128), tc_, w=[tc_])
        k.dma("sp", lbb[:], P["ln_b"].partition_broadcast(128), tc_, w=[tc_])
        O1 = Bufs(k, "O1", [128, 8, 128], F32, 1)
        XR = Bufs(k, "XR", [128, D], F32, 1)
        GA = Bufs(k, "GA", [128, D], F32, 1)
        ssq = k.sb("ssq", [128, 8, 1], F32); t_ssq = Tok("ssq")
        MX = Bufs(k, "MX", [128, D], BF16, 1)
        MXT = Bufs(k, "MXT", [128, 8, 128], BF16, 1)
        ZT = Bufs(k, "ZT", [128, D], F32, 1)
        HO = Bufs(k, "HO", [128, D], F32, 1)
        lst = k.sb("mlnst", [128, 16], F32); t_lst = Tok("mlnst")

    def c3(ap, n=L):
        return ap.rearrange("p (c l) -> p c l", l=n)

    def rv(tile_, dk):
        return bass.AP(tile_, T - 1, [[T, dk], [-1, T]])

    def proj_fm(nm, x, tx, cols=None):
        gi, o, wd = goff[nm]
        p, tp = PF.next()
        n = T if cols is None else 2
        for kc in range(8):
            rhs = x[:, kc, 1:T + 1] if cols is None else bass.AP(x, kc * (T + 2), [[8 * (T + 2), 128], [T + 1, 2]])
            k.op("pe", lambda e, p=p, kc=kc, rhs=rhs, o=o, wd=wd, n=n: e.matmul(p[0:wd, 0:n], lhsT=wfm[:, kc, o:o + wd], rhs=rhs,
                                                                              start=(kc == 0), stop=(kc == 7)), r=[tx, tc_], w=[tp])
        return p, tp, gi, wd

    last = L - 1 if asc else 0
    ref = L // 2 - 1 if asc else L // 2

    blocks = list(range(nblk)) if asc else list(range(nblk - 1, -1, -1))
    xs = {}

    def gm_block(b, par):
        t0 = b * T
        x, tx = xT.next()
        xs[b] = (x, tx)
        k.dma("sp", x[:], xt_fm.rearrange("(kc p) t -> p kc t", p=128)[:, :, t0:t0 + T + 2], tx, w=[tx])
        if layer == 1 and b == nblk - 1:
            k.op("pool", lambda e, x=x: e.tensor_copy(x[:, :, T + 1:T + 2], hal[:].unsqueeze(2)), r=[tx, tc_], w=[tx])
        if layer == 0:
            p, tp, gi, wd = proj_fm("rb", x, tx)
            k.op("act", lambda e, p=p, gi=gi: e.activation(out=rbs[:], in_=p[0:16, 0:T], func=AF.Identity, bias=bfm[0:16, gi:gi + 1], scale=1.0),
                 r=[tp, tc_], w=[t_rbs])
        else:
            p, tp, gi, wd = proj_fm("gd", x, tx)
            k.op("act", lambda e, p=p: e.copy(out=gds[:], in_=p[0:8, 0:T]), r=[tp], w=[t_gds])
            cs, tcs = cst.next()
            k.dma("sp", cs[:], P["cs_tab"][:, :, t0:t0 + T].rearrange("a p t -> p a t"), tcs, w=[tcs])
        def sg_body(si, sg):
                dk, h, typ = sg["dk"], sg["h"], sg["typ"]
                tw = [t_sg[par][si]]
                ts_ = tsets[si % NSET]
                Tq, t_Tq, Tk, t_Tk, Tlf, t_Tlf, TG, t_TG, Te1, t_Te1, Te2, t_Te2, Tsm, t_Tsm = ts_[:14]
                if layer == 1:
                    aext, t_aext = ts_[14:16]
                if typ == "A":
                    p, tp, gi, wd = proj_fm("qa%d" % h, x, tx)
                    yield k.op("act", lambda e, p=p, gi=gi: e.activation(out=Tq[:], in_=p[:, 0:T], func=AF.Silu, bias=bfm[:, gi:gi + 1], scale=1.0),
                         r=[tp, tc_], w=[t_Tq])
                    p, tp, gi, wd = proj_fm("fa%d" % h, x, tx)
                    yield k.op("act", lambda e, p=p, gi=gi: e.activation(out=Tk[:], in_=p[:, 0:T], func=AF.Sigmoid, bias=bfm[:, gi:gi + 1], scale=1.0),
                         r=[tp, tc_], w=[t_Tk])
                    yield k.op("dve", lambda e, h=h: e.tensor_scalar(out=Tk[:], in0=Tk[:], scalar1=oml[:, h, :], scalar2=lb[:, h, :], op0=ALU.mult, op1=ALU.add),
                         r=[t_Tk, tc_], w=[t_Tk])
                    yield k.op("act", lambda e: e.activation(out=Tlf[:], in_=Tk[:], func=AF.Ln), r=[t_Tk], w=[t_Tlf])
                    yield k.op("dve", lambda e: e.tensor_scalar(out=Tk[:], in0=Tk[:], scalar1=-1.0, scalar2=1.0, op0=ALU.mult, op1=ALU.add),
                         r=[t_Tk, t_Tlf], w=[t_Tk])
                elif typ == "B":
                    p, tp, gi, wd = proj_fm("qb%d" % h, x, tx)
                    yield k.op("act", lambda e, p=p, gi=gi: e.activation(out=Tq[0:64, :], in_=p[0:64, 0:T], func=AF.Identity, bias=bfm[0:64, gi:gi + 1], scale=1.0),
                         r=[tp, tc_], w=[t_Tq])
                    p, tp, gi, wd = proj_fm("kb%d" % h, x, tx)
                    yield k.op("act", lambda e, p=p, gi=gi: e.activation(out=Tk[0:64, :], in_=p[0:64, 0:T], func=AF.Identity, bias=bfm[0:64, gi:gi + 1], scale=1.0),
                         r=[tp, tc_], w=[t_Tk])
                    p, tp = PF.next()
                    k.op("pe", lambda e, p=p, h=h: e.matmul(p[0:64, 0:T], lhsT=gku[:, h * 64:(h + 1) * 64], rhs=rbs[:], start=True, stop=True),
                         r=[t_rbs, tc_], w=[tp])
                    yield k.op("act", lambda e, p=p, h=h: e.activation(out=Tlf[0:64, :], in_=p[0:64, 0:T], func=AF.Exp, bias=ngkb[:, h:h + 1], scale=-1.0),
                         r=[tp, tc_], w=[t_Tlf])
                    yield k.op("act", lambda e: e.activation(out=Tlf[0:64, :], in_=Tlf[0:64, :], func=AF.Ln, bias=1.0, scale=1.0), r=[t_Tlf], w=[t_Tlf])
                elif typ == "C":
                    for (dst, tdst, n1, n2) in ((Tq, t_Tq, "qc", "qs"), (Tk, t_Tk, "kc", "ks")):
                        p, tp, gi, wd = proj_fm("%s%d" % (n1, h), x, tx)
                        yield k.op("dve", lambda e, p=p, gi=gi, dst=dst: e.scalar_tensor_tensor(out=dst[0:64, :], in0=p[0:64, 0:T], scalar=bfm[0:64, gi:gi + 1],
                                                                                       in1=cs[:, 0, :], op0=ALU.add, op1=ALU.mult), r=[tp, tc_, tcs], w=[tdst])
                        p, tp, gi, wd = proj_fm("%s%d" % (n2, h), x, tx)
                        yield k.op("dve", lambda e, p=p, gi=gi: e.scalar_tensor_tensor(out=Te1[0:64, :], in0=p[0:64, 0:T], scalar=bfm[0:64, gi:gi + 1],
                                                                              in1=cs[:, 1, :], op0=ALU.add, op1=ALU.mult), r=[tp, tc_, tcs], w=[t_Te1])
                        yield k.op("dve", lambda e, dst=dst: e.tensor_tensor(out=dst[0:64, :], in0=dst[0:64, :], in1=Te1[0:64, :], op=ALU.add), r=[tdst, t_Te1], w=[tdst])
                    yield k.op("dve", lambda e, h=h: e.tensor_copy(Tlf[0:64, :], lgam[:, h:h + 1].broadcast_to([64, T])), r=[tc_], w=[t_Tlf])
                else:
                    for (dst, tdst, nm, ci) in ((Tq, t_Tq, "qd", h), (Tk, t_Tk, "kd", 4 + h)):
                        p, tp, gi, wd = proj_fm("%s%d" % (nm, h), x, tx)
                        yield k.op("act", lambda e, p=p, gi=gi: e.activation(out=aext[:, 1:T + 1], in_=p[:, 0:T], func=AF.Identity, bias=bfm[:, gi:gi + 1], scale=1.0),
                             r=[tp, tc_], w=[t_aext])
                        p, tp, gi, wd = proj_fm("%s%d" % (nm, h), x, tx, cols=2)
                        yield k.op("act", lambda e, p=p, gi=gi: e.activation(out=bass.AP(aext, 0, [[T + 2, 128], [T + 1, 2]]), in_=p[:, 0:2], func=AF.Identity,
                                                                      bias=bfm[:, gi:gi + 1], scale=1.0), r=[tp, tc_], w=[t_aext])
                        if b == 0:
                            yield k.op("pool", lambda e: e.memset(aext[:, 0:1], 0.0), r=[t_aext], w=[t_aext])
                        yield k.op("dve", lambda e, ci=ci: e.tensor_scalar(out=Te1[:], in0=aext[:, 0:T], scalar1=cw[:, ci, 0:1], scalar2=cb[:, ci:ci + 1],
                                                                   op0=ALU.mult, op1=ALU.add), r=[t_aext, tc_], w=[t_Te1])
                        yield k.op("dve", lambda e, ci=ci: e.scalar_tensor_tensor(out=Te1[:], in0=aext[:, 1:T + 1], scalar=cw[:, ci, 1:2], in1=Te1[:],
                                                                          op0=ALU.mult, op1=ALU.add), r=[t_aext, tc_, t_Te1], w=[t_Te1])
                        yield k.op("dve", lambda e, ci=ci: e.scalar_tensor_tensor(out=Te1[:], in0=aext[:, 2:T + 2], scalar=cw[:, ci, 2:3], in1=Te1[:],
                                                                          op0=ALU.mult, op1=ALU.add), r=[t_aext, tc_, t_Te1], w=[t_Te1])
                        yield k.op("act", lambda e, dst=dst: e.activation(out=dst[:], in_=Te1[:], func=AF.Silu), r=[t_Te1], w=[tdst])
                    p, tp = PF.next()
                    k.op("pe", lambda e, p=p, h=h: e.matmul(p[:, 0:T], lhsT=selm[:, h, :], rhs=gds[:], start=True, stop=True), r=[t_gds, tc_], w=[tp])
                    yield k.op("act", lambda e, p=p, h=h: e.activation(out=Te1[:], in_=p[:, 0:T], func=AF.Exp, bias=lib[:, h:h + 1], scale=1.0), r=[tp, tc_], w=[t_Te1])
                    yield k.op("dve", lambda e: e.tensor_tensor(out=Tk[:], in0=Tk[:], in1=Te1[:], op=ALU.mult), r=[t_Tk, t_Te1], w=[t_Tk])
                    p, tp = PF.next()
                    k.op("pe", lambda e, p=p, h=h: e.matmul(p[:, 0:T], lhsT=selm[:, 4 + h, :], rhs=gds[:], start=True, stop=True), r=[t_gds, tc_], w=[tp])
                    yield k.op("act", lambda e, p=p, h=h: e.activation(out=Tlf[:], in_=p[:, 0:T], func=AF.Exp, bias=nfb[:, h:h + 1], scale=-1.0), r=[tp, tc_], w=[t_Tlf])
                    yield k.op("act", lambda e: e.activation(out=Tlf[:], in_=Tlf[:], func=AF.Ln, bias=1.0, scale=1.0), r=[t_Tlf], w=[t_Tlf])
                ls = sg["lfscale"]
                if asc:
                    yield k.op("dve", lambda e, dk=dk: e.tensor_tensor_scan(out=TG[0:dk, :], data0=rmask[0:dk, :], data1=Tlf[0:dk, :], initial=0.0,
                                                                     op0=ALU.mult, op1=ALU.add), r=[t_Tlf, tc_], w=[t_TG])
                else:
                    yield k.op("dve", lambda e, dk=dk: e.tensor_tensor_scan(out=rv(TG, dk), data0=rv(rmask, dk), data1=rv(Tlf, dk), initial=0.0,
                                                                     op0=ALU.mult, op1=ALU.add), r=[t_Tlf, tc_], w=[t_TG])
                G3 = c3(TG[0:dk, :])
                gref = G3[:, :, ref:ref + 1]
                glast = G3[:, :, last:last + 1]
                yield k.op("dve", lambda e, dk=dk, G3=G3, gref=gref: e.tensor_tensor(out=c3(Te1[0:dk, :]), in0=G3, in1=gref.broadcast_to([dk, NCH, L]), op=ALU.subtract),
                     r=[t_TG], w=[t_Te1])
                yield k.op("act", lambda e, dk=dk, ls=ls: e.activation(out=Te2[0:dk, :], in_=Te1[0:dk, :], func=AF.Exp, scale=-ls), r=[t_Te1], w=[t_Te2])
                yield k.op("act", lambda e, dk=dk, ls=ls: e.activation(out=Te1[0:dk, :], in_=Te1[0:dk, :], func=AF.Exp, scale=ls), r=[t_Te1], w=[t_Te1])
                sm = Tsm[0:dk]
                yield k.op("dve", lambda e, dk=dk, sm=sm, glast=glast, gref=gref: e.tensor_tensor(out=sm[:, 2, :].unsqueeze(2), in0=glast, in1=gref, op=ALU.subtract),
                     r=[t_TG, t_Tsm], w=[t_Tsm])
                yield k.op("act", lambda e, sm=sm, gref=gref, ls=ls: e.activation(out=sm[:, 0, :].unsqueeze(2), in_=gref, func=AF.Exp, scale=ls), r=[t_TG, t_Tsm], w=[t_Tsm])
                yield k.op("act", lambda e, sm=sm, ls=ls: e.activation(out=sm[:, 1, :], in_=sm[:, 2, :], func=AF.Exp, scale=ls), r=[t_Tsm], w=[t_Tsm])
                yield k.op("act", lambda e, dk=dk, si=si, glast=glast, ls=ls: e.activation(out=DEC[par][si][0:dk, :].unsqueeze(2), in_=glast, func=AF.Exp, scale=ls),
                     r=[t_TG] + tw, w=tw)
                yield k.op("dve", lambda e, dk=dk, si=si, qs=sg["qscale"]: e.scalar_tensor_tensor(out=QT[par][si][0:dk, :], in0=Tq[0:dk, :], scalar=qs, in1=Te1[0:dk, :],
                                                                                         op0=ALU.mult, op1=ALU.mult), r=[t_Tq, t_Te1] + tw, w=tw)
                yield k.op("pool", lambda e, dk=dk, si=si: e.tensor_tensor(out=KT[par][si][0:dk, :], in0=Tk[0:dk, :], in1=Te2[0:dk, :], op=ALU.mult),
                     r=[t_Tk, t_Te2] + tw, w=tw)
                for a in range(2):
                    qv = QIP[par][si][0:dk].rearrange("p (t a) (b l) -> p t a b l", a=2, b=2)[:, :, a, a, :]
                    yield k.op("pool", lambda e, dk=dk, si=si, a=a, qv=qv, sm=sm: e.tensor_tensor(
                        out=qv, in0=QT[par][si][0:dk, :].rearrange("p (t a l) -> p t a l", a=2, l=L)[:, :, a, :],
                        in1=sm[:, 0, :].rearrange("p (t a) -> p t a", a=2)[:, :, a:a + 1].broadcast_to([dk, NTL, L]), op=ALU.mult),
                        r=[t_Tsm] + tw, w=tw)
                yield k.op("dve", lambda e, dk=dk, si=si, sm=sm: e.tensor_tensor(out=c3(KST[par][si][0:dk, :]), in0=c3(KT[par][si][0:dk, :]),
                                                                          in1=sm[:, 1, :].unsqueeze(2).broadcast_to([dk, NCH, L]), op=ALU.mult),
                     r=[t_Tsm] + tw, w=tw)

        GW = globals().get("MIX_GW") or NSET
        for g0 in range(0, len(sgs), GW):
            gens = [sg_body(si, sgs[si]) for si in range(g0, min(g0 + GW, len(sgs)))]
            while gens:
                for g_ in list(gens):
                    try:
                        next(g_)
                    except StopIteration:
                        gens.remove(g_)
            yield

    def scan_block(b, par):
        x, tx = xs[b]
        t0 = b * T
        tiles = list(range(NTL)) if asc else list(range(NTL - 1, -1, -1))
        for tl in tiles:
            r0 = t0 + tl * 128
            v, tv = V.next()
            for half in range(2):
                p, tp = PF.next()
                for kc in range(8):
                    k.op("pe", lambda e, p=p, kc=kc, tl=tl, half=half: e.matmul(p[:], lhsT=x[:, kc, 1 + tl * 128:1 + (tl + 1) * 128],
                                                                             rhs=wtm[:, kc, half * 512:(half + 1) * 512], start=(kc == 0), stop=(kc == 7)),
                         r=[tx, tc_], w=[tp])
                k.op("dve", lambda e, p=p, v=v, half=half: e.tensor_tensor(out=v[:, half * 4:(half + 1) * 4, 0:128], in0=p[:].rearrange("p (h d) -> p h d", d=128),
                                                                        in1=btm[:, half * 512:(half + 1) * 512].rearrange("p (h d) -> p h d", d=128), op=ALU.add),
                     r=[tp, tc_], w=[tv])
            yield
            p, tp = PF.next()
            pb = p[:].bitcast(BF16).rearrange("p (s d) -> p s d", d=128)
            for si, sg in enumerate(sgs):
                dk = sg["dk"]
                k.op("pe", lambda e, pb=pb, si=si, dk=dk, tl=tl: e.transpose(pb[:, si, 0:dk], KST[par][si][0:dk, tl * 128:(tl + 1) * 128], c["idb"][0:dk, 0:dk]),
                     r=[t_sg[par][si], c["t_id"]], w=[tp])
            for hf in range(2):
                for si in range(hf * 4, hf * 4 + 4):
                    dk = sgs[si]["dk"]
                    k.op("act", lambda e, pb=pb, si=si, dk=dk, tl=tl: e.copy(out=KS[si][:, tl, 0:dk], in_=pb[:, si, 0:dk]), r=[tp], w=[t_ks[si]])
            yield
            sc, tsc = SCP.next()
            for hf in range(2):
                ps__, tps = PX.next()
                ps_ = ps__[:].rearrange("p (s d) -> p s d", d=128)
                for j in range(4):
                    si = hf * 4 + j
                    dk = sgs[si]["dk"]
                    k.op("pe", lambda e, ps_=ps_, j=j, si=si, dk=dk, tl=tl: e.matmul(ps_[:, j, :], lhsT=KT[par][si][0:dk, tl * 128:(tl + 1) * 128],
                                                                                  rhs=QT[par][si][0:dk, tl * 128:(tl + 1) * 128], start=True, stop=True),
                         r=[t_sg[par][si]], w=[tps])
                k.op("dve", lambda e, sc=sc, ps_=ps_, hf=hf: e.tensor_tensor(out=sc[:, hf * 4:(hf + 1) * 4, :], in0=ps_,
                                                                          in1=smask[:].unsqueeze(1).broadcast_to([128, 4, 128]), op=ALU.mult),
                     r=[tps, tc_], w=[tsc])
            yield
            corder = (0, 1) if asc else (1, 0)
            for si, sg in enumerate(sgs):
                bk, col = oslots[si]
                dve_ = sg["dve"]
                k.op("pe", lambda e, bk=bk, col=col, dve_=dve_, sc=sc, si=si, v=v, hv=sg["hv"], first=(si in ofirst): e.matmul(
                    PO[bk][:, col:col + dve_], lhsT=sc[:, si, :], rhs=v[:, hv, 0:dve_], start=first, stop=False, skip_group_check=True),
                    r=[tsc, tv], w=[t_PO[bk]])
            for ci, cc in enumerate(corder):
                yield
                ch = tl * 2 + cc
                for si, sg in enumerate(sgs):
                    bk, col = oslots[si]
                    dk, dve_ = sg["dk"], sg["dve"]
                    k.op("pe", lambda e, bk=bk, col=col, si=si, dk=dk, dve_=dve_, ch=ch, ci=ci: e.matmul(
                        PO[bk][:, col:col + dve_], lhsT=QIP[par][si][0:dk, ch, :], rhs=Sb[si][0:dk, 0:dve_],
                        start=False, stop=(ci == 1), skip_group_check=True), r=[t_sg[par][si], t_Sb[si]], w=[t_PO[bk]])
                for grp in stgroups:
                    pst, tpst = PX.next()
                    pitch = 512 // len(grp)
                    for gj, si in enumerate(grp):
                        sg = sgs[si]
                        dk, dve_ = sg["dk"], sg["dve"]
                        col = gj * pitch
                        k.op("pe", lambda e, pst=pst, col=col, si=si, dk=dk, dve_=dve_, cc=cc, tl=tl, v=v, hv=sg["hv"]: e.matmul(
                            pst[0:dk, col:col + dve_], lhsT=KS[si][cc * 64:(cc + 1) * 64, tl, 0:dk], rhs=v[cc * 64:(cc + 1) * 64, hv, 0:dve_],
                            start=True, stop=True), r=[t_ks[si], tv], w=[tpst])
                    for gj, si in enumerate(grp):
                        sg = sgs[si]
                        dk, dve_ = sg["dk"], sg["dve"]
                        col = gj * pitch
                        k.op("dve", lambda e, si=si, dk=dk, dve_=dve_, ch=ch, pst=pst, col=col: e.scalar_tensor_tensor(
                            out=S32[si][0:dk, 0:dve_], in0=S32[si][0:dk, 0:dve_], scalar=DEC[par][si][0:dk, ch:ch + 1], in1=pst[0:dk, col:col + dve_],
                            op0=ALU.mult, op1=ALU.add), r=[t_S[si], t_sg[par][si], tpst], w=[t_S[si]])
                        k.op("act", lambda e, si=si, dk=dk, dve_=dve_: e.copy(out=Sb[si][0:dk, 0:dve_], in_=S32[si][0:dk, 0:dve_]), r=[t_S[si]], w=[t_Sb[si]])
            yield
            ot, tot = OT.next()
            if pidx == 2:
                o1, to1 = O1.next()
                k.dma("sp", o1[:], P["o1"][r0:r0 + 128, :].rearrange("p (h d) -> p h d", d=128), to1, w=[to1])
            for hf in range(2):
                if layer == 1 and hf == 1:
                    for bnk in range(2):
                        pv = PO[1 + bnk][:].rearrange("p (s d) -> p s d", d=256)
                        k.op("act", lambda e, pv=pv, bnk=bnk: e.activation(out=rden[:, bnk * 2:bnk * 2 + 2, :], in_=pv[:, :, 128:129], func=AF.Abs),
                             r=[t_PO[1 + bnk], t_rden], w=[t_rden])
                        k.op("dve", lambda e, bnk=bnk: e.tensor_scalar(out=rden[:, bnk * 2:bnk * 2 + 2, :], in0=rden[:, bnk * 2:bnk * 2 + 2, :], scalar1=1.0, scalar2=None,
                                                                      op0=ALU.max), r=[t_rden], w=[t_rden])
                        k.op("dve", lambda e, bnk=bnk: e.reciprocal(out=rden[:, bnk * 2:bnk * 2 + 2, :], in_=rden[:, bnk * 2:bnk * 2 + 2, :]), r=[t_rden], w=[t_rden])
                        k.op("dve", lambda e, pv=pv, bnk=bnk, ot=ot: e.tensor_tensor(out=ot[:, 4 + bnk * 2:6 + bnk * 2, :], in0=pv[:, :, 0:128],
                                                                                  in1=rden[:, bnk * 2:bnk * 2 + 2, :].broadcast_to([128, 2, 128]), op=ALU.mult),
                             r=[t_PO[1 + bnk], t_rden], w=[tot])
                        if pidx == 2:
                            k.op("pool", lambda e, bnk=bnk, ot=ot, o1=o1: e.tensor_tensor(out=ot[:, 4 + bnk * 2:6 + bnk * 2, :], in0=ot[:, 4 + bnk * 2:6 + bnk * 2, :],
                                                                                       in1=o1[:, 4 + bnk * 2:6 + bnk * 2, :], op=ALU.add), r=[to1, tot], w=[tot])
                else:
                    pv = PO[hf][:].rearrange("p (s d) -> p s d", d=128)
                    if pidx == 1:
                        k.op("act", lambda e, pv=pv, hf=hf, ot=ot: e.copy(out=ot[:, hf * 4:(hf + 1) * 4, :], in_=pv), r=[t_PO[hf]], w=[tot])
                    else:
                        k.op("dve", lambda e, pv=pv, hf=hf, ot=ot, o1=o1: e.tensor_tensor(out=ot[:, hf * 4:(hf + 1) * 4, :], in0=pv, in1=o1[:, hf * 4:(hf + 1) * 4, :], op=ALU.add),
                             r=[t_PO[hf], to1], w=[tot])
            if pidx == 1:
                k.dma("sp", P["o1"][r0:r0 + 128, :].rearrange("p (h d) -> p h d", d=128), ot[:], tot, r=[tot])
                continue
            yield
            xr, txr = XR.next()
            k.dma("sp", xr[:], P["x_tm"][r0:r0 + 128, :], txr, w=[txr])
            ga, tga = GA.next()
            for half in range(2):
                p, tp = PF.next()
                for kc in range(8):
                    k.op("pe", lambda e, p=p, kc=kc, tl=tl, half=half: e.matmul(p[:], lhsT=x[:, kc, 1 + tl * 128:1 + (tl + 1) * 128],
                                                                             rhs=wtm[:, kc, 1024 + half * 512:1024 + (half + 1) * 512], start=(kc == 0), stop=(kc == 7)),
                         r=[tx, tc_], w=[tp])
                k.op("dve", lambda e, p=p, ga=ga, half=half: e.tensor_tensor(out=ga[:, half * 512:(half + 1) * 512], in0=p[:],
                                                                          in1=btm[:, 1024 + half * 512:1024 + (half + 1) * 512], op=ALU.add), r=[tp, tc_], w=[tga])
                fn = AF.Sigmoid if (layer == 1 and half == 1) else AF.Silu
                k.op("act", lambda e, ga=ga, half=half, fn=fn: e.activation(out=ga[:, half * 512:(half + 1) * 512], in_=ga[:, half * 512:(half + 1) * 512], func=fn),
                     r=[tga], w=[tga])
            yield
            z, tz = ZT.next()
            SQ = z[:].rearrange("p (h d) -> p h d", d=128)
            k.op("pool", lambda e, ot=ot, SQ=SQ: e.tensor_tensor(out=SQ, in0=ot[:], in1=ot[:], op=ALU.mult), r=[tot], w=[tz])
            k.op("dve", lambda e, SQ=SQ: e.tensor_reduce(out=ssq[:], in_=SQ, axis=AX.X, op=ALU.add), r=[tz], w=[t_ssq])
            k.op("act", lambda e: e.activation(out=ssq[:], in_=ssq[:], func=AF.Ln, bias=NORM_EPS, scale=1.0 / 128.0), r=[t_ssq], w=[t_ssq])
            k.op("act", lambda e: e.activation(out=ssq[:], in_=ssq[:], func=AF.Exp, scale=-0.5), r=[t_ssq], w=[t_ssq])
            k.op("dve", lambda e, ot=ot: e.tensor_tensor(out=ot[:], in0=ot[:], in1=ssq[:].broadcast_to([128, 8, 128]), op=ALU.mult), r=[tot, t_ssq], w=[tot])
            otf = ot[:].rearrange("p h d -> p (h d)")
            k.op("pool", lambda e, otf=otf: e.tensor_tensor(out=otf, in0=otf, in1=gnb[:], op=ALU.mult), r=[tot, tc_], w=[tot])
            yield
            mx, tmx = MX.next()
            k.op("dve", lambda e, otf=otf, mx=mx, ga=ga: e.tensor_tensor(out=mx[:], in0=otf, in1=ga[:], op=ALU.mult), r=[tot, tga], w=[tmx])
            p, tp = PF.next()
            pb = p[:].bitcast(BF16).rearrange("p (s d) -> p s d", d=128)
            for kc in range(8):
                k.op("pe", lambda e, pb=pb, kc=kc, mx=mx: e.transpose(pb[:, kc, :], mx[:, kc * 128:(kc + 1) * 128], c["idb"][:]), r=[tmx, c["t_id"]], w=[tp])
            mt, tmt = MXT.next()
            k.op("act", lambda e, mt=mt, pb=pb: e.copy(out=mt[:], in_=pb), r=[tp], w=[tmt])
            for half in range(2):
                p, tp = PF.next()
                for kc in range(8):
                    k.op("pe", lambda e, p=p, kc=kc, mt=mt, half=half: e.matmul(p[:], lhsT=mt[:, kc, :], rhs=wout[:, kc, half * 512:(half + 1) * 512],
                                                                             start=(kc == 0), stop=(kc == 7)), r=[tmt, tc_], w=[tp])
                k.op("dve", lambda e, p=p, z=z, xr=xr, half=half: e.scalar_tensor_tensor(out=z[:, half * 512:(half + 1) * 512], in0=xr[:, half * 512:(half + 1) * 512],
                                                                                      scalar=ALPHA, in1=p[:], op0=ALU.mult, op1=ALU.add), r=[tp, txr], w=[tz])
            ho, tho = HO.next()
            emit_ln(k, z, tz, lgb, lbb, tc_, lst, t_lst, ho, tho)
            k.dma("sp", P["h_out"][r0:r0 + 128, :], ho[:], tho, r=[tho])
        yield

    def drive(gens):
        gens = list(gens)
        while gens:
            for g in list(gens):
                try:
                    next(g)
                except StopIteration:
                    gens.remove(g)

    drive([gm_block(blocks[0], 0)])
    for bi, b in enumerate(blocks):
        gens = [scan_block(b, bi % NPAR)]
        if NPAR == 1:
            drive(gens)
            gens = []
        if bi + 1 < len(blocks):
            gens.append(gm_block(blocks[bi + 1], (bi + 1) % NPAR))
        drive(gens)
    if pidx == 1:
        for i in range(nsg):
            k.dma("sp", P["state_out"][i], S32[i][:], t_S[i], r=[t_S[i]])
    k.end_scope()


AB_OFF = dict(qa=0, fa0=512, fa1=1024, ia=1536, ga=2048, qb=2560, kb=2816, vb=3072, gb=3584, rb0=4096, rb1=4112)
CD_OFF = dict(qc=0, kc=256, vc=512, gc=1024, qd=1536, kd=2048, vd=2560, od=3072, gates=3584)


def _ar(a, n):
    return np.arange(a, a + n)


def prep_mixer(inp, layer, d, flipped, pos, NT):
    f32 = np.float32
    fm, sgs = mixer_layout(layer)
    out = {}
    if layer == 0:
        w, b = inp["ab_w_in"][0], inp["ab_b_in"][0]
        cols = np.concatenate([_ar(AB_OFF["qa"], 512), _ar(AB_OFF["fa%d" % d], 512), _ar(AB_OFF["qb"], 256), _ar(AB_OFF["kb"], 256),
                               _ar(AB_OFF["rb%d" % d], 16)])
        tcols = np.concatenate([_ar(AB_OFF["ia"], 512), _ar(AB_OFF["vb"], 512), _ar(AB_OFF["ga"], 512), _ar(AB_OFF["gb"], 512)])
        out["hgrn_lb"] = np.ascontiguousarray(inp["hgrn_lb"].reshape(3, 4, 128).transpose(2, 1, 0)).astype(f32)
        out["gk_up"] = np.ascontiguousarray(inp["gla_gk_up"][0, d]).astype(f32)
        out["gk_b"] = np.ascontiguousarray(inp["gla_gk_b"][0, d].reshape(4, 64).T).astype(f32)
        out["norm_g"] = np.concatenate([inp["hgrn_norm"][0], inp["gla_norm"][0]])[None].astype(f32)
        out["w_out"] = np.ascontiguousarray(inp["ab_w_out"][0]).astype(f32)
    else:
        w, b = inp["cd_w_in"][0], inp["cd_b_in"][0]

        def sw(base):
            return np.concatenate([np.concatenate([_ar(base + h * 64 + 32, 32), _ar(base + h * 64, 32)]) for h in range(4)])
        g0 = CD_OFF["gates"]
        cols = np.concatenate([_ar(CD_OFF["qc"], 256), sw(CD_OFF["qc"]), _ar(CD_OFF["kc"], 256), sw(CD_OFF["kc"]),
                               _ar(CD_OFF["qd"], 512), _ar(CD_OFF["kd"], 512), _ar(g0 + 8 * d, 8)])
        tcols = np.concatenate([_ar(CD_OFF["vc"], 512), _ar(CD_OFF["vd"], 512), _ar(CD_OFF["gc"], 512), _ar(CD_OFF["od"], 512)])
        exps = 5.0 + 2.0 * np.arange(4, dtype=f32) + f32(d)
        lg = np.log1p(-np.exp2(-exps)).astype(f32)
        out["lgam"] = np.ascontiguousarray(np.broadcast_to(lg[None, :], (64, 4))).astype(f32)
        cwt = inp["mlstm_conv_w"][0]
        if flipped:
            cwt = cwt[::-1]
        out["conv_w"] = np.ascontiguousarray(cwt.reshape(3, 8, 128).transpose(2, 1, 0)).astype(f32)
        out["conv_b"] = np.ascontiguousarray(inp["mlstm_conv_b"][0].reshape(8, 128).T).astype(f32)
        sel = np.zeros((8, 8, 128), f32)
        for r in range(8):
            sel[r, r, :] = 1.0
        out["selm"] = sel
        out["fgb"] = np.ascontiguousarray(inp["mlstm_fgate_b"][0, d])[None].astype(f32)
        out["lib"] = np.ascontiguousarray(b[g0 + 8 * d: g0 + 8 * d + 4])[None].astype(f32)
        out["lfb"] = np.ascontiguousarray(b[g0 + 8 * d + 4: g0 + 8 * d + 8])[None].astype(f32)
        half = 32
        freqs = (f32(10000.0) ** (-np.arange(half, dtype=f32) / f32(half))).astype(f32)
        ang = (pos.astype(f32)[:, None] * freqs[None, :]).astype(f32)
        cosv, sinv = np.cos(ang).astype(f32), np.sin(ang).astype(f32)
        cs = np.zeros((2, 64, NT), f32)
        cs[0, :32], cs[0, 32:] = cosv.T, cosv.T
        cs[1, :32], cs[1, 32:] = -sinv.T, sinv.T
        out["cs_tab"] = cs
        out["norm_g"] = np.concatenate([inp["ret_norm"][0], inp["mlstm_norm"][0]])[None].astype(f32)
        out["w_out"] = np.ascontiguousarray(inp["cd_w_out"][0]).astype(f32)
    out["wfm"] = np.ascontiguousarray(w[:, cols]).astype(f32)
    bf = np.zeros((128, len(fm)), f32)
    o = 0
    for gi, (nm, wd) in enumerate(fm):
        bf[:wd, gi] = b[cols[o:o + wd]]
        o += wd
    out["bfm"] = bf
    out["wtm"] = np.ascontiguousarray(w[:, tcols]).astype(f32)
    out["btm"] = np.ascontiguousarray(b[tcols])[None].astype(f32)
    out["ln_g"] = np.ascontiguousarray(inp["ln_mix_g"][layer])[None].astype(f32)
    out["ln_b"] = np.ascontiguousarray(inp["ln_mix_b"][layer])[None].astype(f32)
    return out


MIX_SHAPES = {
    0: dict(hgrn_lb=[128, 4, 3], gk_up=[16, 256], gk_b=[64, 4]),
    1: dict(lgam=[64, 4], conv_w=[128, 8, 3], conv_b=[128, 8], selm=[8, 8, 128], fgb=[1, 4], lib=[1, 4], lfb=[1, 4]),
}


def declare_mixer_inputs(nc, layer, pidx, NT, pre):
    fm, sgs = mixer_layout(layer)
    CF = sum(wd for _, wd in fm)
    P = {}

    def inp(nm, shape):
        P[nm] = nc.dram_tensor(pre + nm, list(shape), F32, kind="ExternalInput").ap()
    inp("wfm", [D, CF])
    inp("bfm", [128, len(fm)])
    inp("wtm", [D, 2048])
    inp("btm", [1, 2048])
    for nm, shp in MIX_SHAPES[layer].items():
        inp(nm, shp)
    if layer == 1:
        inp("cs_tab", [2, 64, NT])
    if pidx == 2:
        inp("norm_g", [1, D])
        inp("w_out", [D, D])
        inp("ln_g", [1, D])
        inp("ln_b", [1, D])
    return P


def build_p1(layer, NT):
    nc = bass.Bass("TRN2", target_bir_lowering=False)
    h_in = nc.dram_tensor("h_in", [NT, D], F32, kind="ExternalInput").ap()
    P = declare_mixer_inputs(nc, layer, 1, NT, "m_")
    if layer == 1:
        P["halo"] = nc.dram_tensor("halo", [D], F32, kind="ExternalInput").ap()
    P["o1"] = nc.dram_tensor("o1", [NT, D], F32, kind="ExternalOutput").ap()
    P["state_out"] = nc.dram_tensor("st_out", [8, 128, VP], F32, kind="ExternalOutput").ap()
    xt = nc.dram_tensor("xt_scr", [D, NT + 2], BF16).ap()
    k = KB(nc)
    c = make_consts(k)
    emit_transpose_phase(k, c, h_in, xt, NT)
    emit_mixer_pass(k, c, layer, 1, NT, xt, P)
    k.finish()
    return nc, k


def build_p2(layer, NT, ne=NE, TQ=1024, with_moe=True):
    nc = bass.Bass("TRN2", target_bir_lowering=False)
    h_in = nc.dram_tensor("h_in", [NT, D], F32, kind="ExternalInput").ap()
    P = declare_mixer_inputs(nc, layer, 2, NT, "m_")
    if layer == 1:
        P["halo"] = nc.dram_tensor("halo", [D], F32, kind="ExternalInput").ap()
    P["o1"] = nc.dram_tensor("o1", [NT, D], F32, kind="ExternalInput").ap()
    P["state_in"] = nc.dram_tensor("st_in", [8, 128, VP], F32, kind="ExternalInput").ap()
    P["x_tm"] = h_in
    h_out = nc.dram_tensor("h_out", [NT, D], F32, kind="ExternalOutput").ap()
    xt = nc.dram_tensor("xt_scr", [D, NT + 2], BF16).ap()
    k = KB(nc)
    c = make_consts(k)
    emit_transpose_phase(k, c, h_in, xt, NT)
    if with_moe:
        h1 = nc.dram_tensor("h1_scr", [NT, D], F32).ap()
        P["h_out"] = h1
        w1 = nc.dram_tensor("w1", [ne, D, D], F32, kind="ExternalInput").ap()
        w3 = nc.dram_tensor("w3", [ne, D, D], F32, kind="ExternalInput").ap()
        w2 = nc.dram_tensor("w2", [ne, D, D], F32, kind="ExternalInput").ap()
        rw = nc.dram_tensor("rw", [D, NE], F32, kind="ExternalInput").ap()
        rb = nc.dram_tensor("rb", [1, NE], F32, kind="ExternalInput").ap()
        fg = nc.dram_tensor("fg", [1, D], F32, kind="ExternalInput").ap()
        fb = nc.dram_tensor("fb", [1, D], F32, kind="ExternalInput").ap()
        emit_mixer_pass(k, c, layer, 2, NT, xt, P)
        emit_moe_phase(k, c, h1, h_out, w1, w3, w2, rw, rb, fg, fb, NT, TQ=min(TQ, NT), ne=ne)
    else:
        P["h_out"] = h_out
        emit_mixer_pass(k, c, layer, 2, NT, xt, P)
    k.finish()
    return nc, k


def core_rows(inp_x, NT):
    B, S, _ = inp_x.shape
    rows, poss = [], []
    for b in range(B):
        rows.append(np.ascontiguousarray(inp_x[b, :NT]))
        poss.append(np.arange(NT))
        rows.append(np.ascontiguousarray(inp_x[b, S - 1:NT - 1:-1]))
        poss.append(S - 1 - np.arange(NT))
    return rows, poss


def run_layers(inputs, NT, ne=NE, layers=(0, 1), with_moe=True, runner=None):
    x = np.asarray(inputs["x"], np.float32)
    B, S, _ = x.shape
    ncores = 2 * B
    inp = {k_: np.asarray(v, np.float32) for k_, v in inputs.items()}
    h, poss = core_rows(x, NT)
    run = runner or (lambda nc, maps: run_bass_kernel_spmd(nc, maps, core_ids=list(range(ncores))).results)
    for layer in layers:
        halos = [h[c ^ 1][NT - 1].copy() for c in range(ncores)]
        nc1, _ = build_p1(layer, NT)
        maps = []
        for c in range(ncores):
            s = c % 2
            m = {"m_" + k_: v for k_, v in prep_mixer(inp, layer, s, s == 1, poss[c], NT).items()
                 if k_ not in ("norm_g", "w_out", "ln_g", "ln_b")}
            m["h_in"] = h[c]
            if layer == 1:
                m["halo"] = halos[c]
            maps.append(m)
        r1 = run(nc1, maps)
        run_layers.dbg.setdefault("r1", []).append(r1)
        nc2, _ = build_p2(layer, NT, ne=ne, with_moe=with_moe)
        maps = []
        for c in range(ncores):
            s = c % 2
            m = {"m_" + k_: v for k_, v in prep_mixer(inp, layer, 1 - s, s == 1, poss[c], NT).items()}
            m["h_in"] = h[c]
            if layer == 1:
                m["halo"] = halos[c]
            m["o1"] = r1[c]["o1"]
            m["st_in"] = r1[c ^ 1]["st_out"]
            if with_moe:
                m["w1"] = inp["moe_w1"][layer][:ne]
                m["w3"] = inp["moe_w3"][layer][:ne]
                m["w2"] = inp["moe_w2"][layer][:ne]
                m["rw"] = inp["router_w"]
                m["rb"] = inp["router_b"][None]
                m["fg"] = inp["ln_ffn_g"][layer][None]
                m["fb"] = inp["ln_ffn_b"][layer][None]
            maps.append(m)
        r2 = run(nc2, maps)
        h = [np.asarray(r2[c]["h_out"]) for c in range(ncores)]
        run_layers.dbg.setdefault("h", []).append(h)
    out = np.zeros((B, S, D), np.float32)
    for b in range(B):
        out[b, :NT] = h[2 * b]
        out[b, S - 1:NT - 1:-1] = h[2 * b + 1]
    return out


PAIRS = [[0, 1], [2, 3], [4, 5], [6, 7]]


def build_fused(NT, ne=NE, TQ=1024, groups=PAIRS, debug=False):
    nc = bass.Bass("TRN2", target_bir_lowering=False)
    x_in = nc.dram_tensor("x_in", [NT, D], F32, kind="ExternalInput").ap()
    sel = nc.dram_tensor("sel", [1, 2], F32, kind="ExternalInput").ap()
    out = nc.dram_tensor("out", [NT, D], F32, kind="ExternalOutput").ap()
    rw = nc.dram_tensor("rw", [D, NE], F32, kind="ExternalInput").ap()
    rb = nc.dram_tensor("rb", [1, NE], F32, kind="ExternalInput").ap()
    xt = nc.dram_tensor("xt_scr", [D, NT + 2], BF16).ap()
    h1 = nc.dram_tensor("h1_scr", [NT, D], F32).ap()
    hA = nc.dram_tensor("hA_scr", [NT, D], F32).ap()
    o1 = nc.dram_tensor("o1_scr", [NT, D], F32).ap()
    st_mine = nc.dram_tensor("st_mine", [8 * 128, VP], F32)
    st_pair = nc.dram_tensor("st_pair", [2 * 8 * 128, VP], F32)
    hl_mine = nc.dram_tensor("hl_mine", [1, D], F32)
    hl_pair = nc.dram_tensor("hl_pair", [2, D], F32)
    k = KB(nc)
    c = make_consts(k)
    for _ in range(globals().get("SALT", 0)):
        k.op("pool", lambda e: e.memset(c["idb"][0:1, 0:1], 1.0), r=[c["t_id"]], w=[c["t_id"]])
    t_cc = Tok("cc")
    t_hl = Tok("hl")
    h_cur = x_in
    for layer in range(2):
        h_nxt = hA if layer == 0 else out
        P1 = declare_mixer_inputs(nc, layer, 1, NT, "m%d1_" % layer)
        P2 = declare_mixer_inputs(nc, layer, 2, NT, "m%d2_" % layer)
        for P in (P1, P2):
            P["sel"] = sel
            P["o1"] = o1
            P["cc_tok"] = [t_cc]
            if layer == 1:
                P["halo_pair"] = hl_pair.ap()
        P1["state_out"] = st_mine.ap().rearrange("(s p) v -> s p v", p=128)
        P2["state_pair"] = st_pair.ap().rearrange("(r s p) v -> r s p v", r=2, p=128)
        P2["x_tm"] = h_cur
        P2["h_out"] = h1
        w1 = nc.dram_tensor("w1_%d" % layer, [ne, D, D], F32, kind="ExternalInput").ap()
        w3 = nc.dram_tensor("w3_%d" % layer, [ne, D, D], F32, kind="ExternalInput").ap()
        w2 = nc.dram_tensor("w2_%d" % layer, [ne, D, D], F32, kind="ExternalInput").ap()
        fg = nc.dram_tensor("fg_%d" % layer, [1, D], F32, kind="ExternalInput").ap()
        fb = nc.dram_tensor("fb_%d" % layer, [1, D], F32, kind="ExternalInput").ap()
        emit_transpose_phase(k, c, h_cur, xt, NT)
        emit_mixer_pass(k, c, layer, 1, NT, xt, P1)
        k.coll("AllGather", [st_mine.ap().opt()], [st_pair.ap().opt()], groups, t_cc, w=[t_cc])
        if debug and layer == 0:
            dbg = nc.dram_tensor("dbg_st", [2 * 8 * 128, VP], F32, kind="ExternalOutput").ap()
            dbg2 = nc.dram_tensor("dbg_o1", [NT, D], F32, kind="ExternalOutput").ap()
            td = Tok("dbg")
            k.dma("sp", dbg, st_pair.ap(), td, r=[t_cc], w=[td])
            k.dma("sp", dbg2, o1, td, w=[td])
        emit_mixer_pass(k, c, layer, 2, NT, xt, P2)
        if debug and layer == 0:
            dbg3 = nc.dram_tensor("dbg_h1", [NT, D], F32, kind="ExternalOutput").ap()
            k.dma("sp", dbg3, h1, td, w=[td])
        emit_moe_phase(k, c, h1, h_nxt, w1, w3, w2, rw, rb, fg, fb, NT, TQ=min(TQ, NT), ne=ne)
        if debug and layer == 0:
            dbg4 = nc.dram_tensor("dbg_hA", [NT, D], F32, kind="ExternalOutput").ap()
            k.dma("sp", dbg4, hA, td, w=[td])
        if layer == 0:
            k.dma("sp", hl_mine.ap(), hA[NT - 1:NT, :], t_hl, w=[t_hl])
            k.coll("AllGather", [hl_mine.ap().opt()], [hl_pair.ap().opt()], groups, t_cc, r=[t_hl], w=[t_cc])
        h_cur = h_nxt
    if globals().get("LATE_DEBUG", 0):
        td = Tok("dbgl")
        for nm, src in (("dbg_hA", hA), ("dbg_h1", h1), ("dbg_o1", o1)):
            dd = nc.dram_tensor(nm, [NT, D], F32, kind="ExternalOutput").ap()
            k.dma("sp", dd, src, td, w=[td])
        dd = nc.dram_tensor("dbg_st", [2 * 8 * 128, VP], F32, kind="ExternalOutput").ap()
        k.dma("sp", dd, st_pair.ap(), td, w=[td])
        dd = nc.dram_tensor("dbg_hl", [2, D], F32, kind="ExternalOutput").ap()
        k.dma("sp", dd, hl_pair.ap(), td, w=[td])
    k.finish()
    return nc, k


def fused_maps(inputs, NT, ne=NE):
    x = np.asarray(inputs["x"], np.float32)
    B, S, _ = x.shape
    ncores = 2 * B
    inp = {k_: np.asarray(v, np.float32) for k_, v in inputs.items()}
    h, poss = core_rows(x, NT)
    maps = []
    shared = {"rw": inp["router_w"], "rb": inp["router_b"][None]}
    for layer in range(2):
        shared["w1_%d" % layer] = inp["moe_w1"][layer][:ne]
        shared["w3_%d" % layer] = inp["moe_w3"][layer][:ne]
        shared["w2_%d" % layer] = inp["moe_w2"][layer][:ne]
        shared["fg_%d" % layer] = inp["ln_ffn_g"][layer][None]
        shared["fb_%d" % layer] = inp["ln_ffn_b"][layer][None]
    for c in range(ncores):
        s = c % 2
        m = dict(shared)
        m["x_in"] = h[c]
        m["sel"] = np.array([[1.0, 0.0]] if s == 1 else [[0.0, 1.0]], np.float32)
        for layer in range(2):
            for pidx, d in ((1, s), (2, 1 - s)):
                pm = prep_mixer(inp, layer, d, s == 1, poss[c], NT)
                for k_, v in pm.items():
                    if pidx == 1 and k_ in ("norm_g", "w_out", "ln_g", "ln_b"):
                        continue
                    m["m%d%d_%s" % (layer, pidx, k_)] = v
        maps.append(m)
    return maps, B, S


def run_fused(inputs, NT, ne=NE, TQ=1024, debug=False):
    maps, B, S = fused_maps(inputs, NT, ne)
    ncores = 2 * B
    nc, _ = build_fused(NT, ne=ne, TQ=TQ, groups=[[2 * b, 2 * b + 1] for b in range(B)], debug=debug)
    res = run_bass_kernel_spmd(nc, maps, core_ids=list(range(ncores))).results
    if debug:
        run_fused.dbg = res
    out = np.zeros((B, S, D), np.float32)
    for b in range(B):
        out[b, :NT] = res[2 * b]["out"]
        out[b, S - 1:NT - 1:-1] = res[2 * b + 1]["out"]
    return out


run_layers.dbg = {}


def kernel(**inputs):
    return run_fused(inputs, 4096)
```

```python
from contextlib import ExitStack
import types
import numpy as np
import concourse.bass as bass
import concourse.mybir as mybir
from concourse.bass_utils import run_bass_kernel_spmd

F32 = mybir.dt.float32
BF16 = mybir.dt.bfloat16
AF = mybir.ActivationFunctionType
ALU = mybir.AluOpType
AX = mybir.AxisListType

ENGS = ("pe", "dve", "act", "pool", "sp")
D = 1024
NE = 16
ALPHA = (2.0 * 2) ** 0.25
LN_EPS = 1e-5
NORM_EPS = 1e-6


def _snap(fn):
    if fn.__closure__ is None:
        return fn
    cells = []
    for c_ in fn.__closure__:
        try:
            cells.append(types.CellType(c_.cell_contents))
        except ValueError:
            cells.append(c_)
    return types.FunctionType(fn.__code__, fn.__globals__, fn.__name__, fn.__defaults__, tuple(cells))


class Tok:
    __slots__ = ("name", "last_w", "readers", "sem", "semcnt", "excl", "uid")
    _n = [0]

    def __init__(self, name="", excl=False):
        Tok._n[0] += 1
        self.uid = Tok._n[0]
        self.name = name
        self.excl = excl
        self.last_w = None
        self.readers = []
        self.sem = None
        self.semcnt = 0


class KB:
    def __init__(self, nc, same_eng_sync=None):
        if same_eng_sync is None:
            same_eng_sync = globals().get("SAME_ENG_SYNC", ("dve", "act", "pool"))
        self.nc = nc
        self.ctx = ExitStack()
        self.prog = {e: [] for e in ENGS}
        self.seen = {e: {} for e in ENGS}
        self.same = set(same_eng_sync)
        self.sems = {}
        self.dma_toks = []
        self.nsem = 0
        self.ninst = 0
        self.scopes = []
        self.scope_dma = []
        self.free_sems = {"sw": [], "hw": [], "cc": []}
        self.uid = 0
        self.phase = -1
        self.new_phase()

    def new_phase(self):
        self.phase += 1
        self.esem = {}
        for e in ENGS:
            if e == "sp":
                continue
            self.esem[e] = self.ctx.enter_context(self.nc.semaphore("s_%s%d" % (e, self.phase)))
            self.sems[("e", e, self.phase)] = self.esem[e]
            self.nsem += 1
        self.ecnt = {e: 0 for e in ENGS}

    def scope(self):
        st = ExitStack()
        self.scopes.append(st)
        return st

    def end_scope(self):
        self.barrier()
        self.scopes.pop().close()
        for (owner, cls) in self.scope_dma:
            self.free_sems[cls].append((owner.sem[cls], owner.semcnt[cls]))
            self.dma_toks.remove((owner, cls))
        self.scope_dma = []
        self.new_phase()

    def _dma_sem(self, owner, cls):
        if owner.sem is None:
            owner.sem = {}
            owner.semcnt = {}
        if cls not in owner.sem:
            if self.free_sems[cls]:
                owner.sem[cls], owner.semcnt[cls] = self.free_sems[cls].pop()
            else:
                owner.sem[cls] = self.ctx.enter_context(self.nc.semaphore("d%d" % self.nsem))
                owner.semcnt[cls] = 0
                self.nsem += 1
            self.sems[("d", owner.uid, cls)] = owner.sem[cls]
            self.dma_toks.append((owner, cls))
            if self.scopes:
                self.scope_dma.append((owner, cls))

    def sb(self, name, shape, dtype):
        self.uid += 1
        st = self.scopes[-1] if self.scopes else self.ctx
        return st.enter_context(self.nc.sbuf_tensor("%s_%d" % (name, self.uid), list(shape), dtype))

    def ps(self, name, shape, dtype=F32):
        self.uid += 1
        st = self.scopes[-1] if self.scopes else self.ctx
        return st.enter_context(self.nc.psum_tensor("%s_%d" % (name, self.uid), list(shape), dtype))

    def _need(self, eng, waits, ev):
        if ev is None:
            return
        key, val = ev
        if key[0] == "e" and key[1] == eng and eng not in self.same:
            return
        if self.seen[eng].get(key, 0) >= val:
            return
        if waits.get(key, 0) < val:
            waits[key] = val

    def _emit_waits(self, eng, waits):
        for key, val in waits.items():
            self.prog[eng].append(("wait", self.sems[key], val))
            self.seen[eng][key] = val

    def _deps(self, eng, r, w):
        waits = {}
        for t in r:
            self._need(eng, waits, t.last_w)
        for t in w:
            self._need(eng, waits, t.last_w)
            for ev in t.readers:
                self._need(eng, waits, ev)
        self._emit_waits(eng, waits)

    def op(self, eng, fn, r=(), w=()):
        ex = [t for t in r if t.excl]
        if ex:
            r = [t for t in r if not t.excl]
            w = list(w) + [t for t in ex if t not in w]
        self._deps(eng, r, w)
        self.ecnt[eng] += 1
        ev = (("e", eng, self.phase), self.ecnt[eng])
        self.prog[eng].append(("op", _snap(fn), self.esem[eng], 1))
        for t in r:
            t.readers.append(ev)
        for t in w:
            t.last_w = ev
            t.readers = []
        self.ninst += 1

    def dma(self, eng, out, in_, owner, r=(), w=(), **kw):
        self._deps(eng, r, w)
        cls = "sw" if eng == "pool" else "hw"
        self._dma_sem(owner, cls)
        owner.semcnt[cls] += 16
        ev = (("d", owner.uid, cls), owner.semcnt[cls])
        self.prog[eng].append(("op", lambda e, o=out, i=in_, k=kw: e.dma_start(out=o, in_=i, **k), owner.sem[cls], 16))
        for t in r:
            t.readers.append(ev)
        for t in w:
            t.last_w = ev
            t.readers = []
        self.ninst += 1

    def coll(self, kind, ins, outs, groups, owner, r=(), w=()):
        self._deps("pool", r, w)
        cls = "cc"
        self._dma_sem(owner, cls)
        owner.semcnt[cls] += 1
        ev = (("d", owner.uid, cls), owner.semcnt[cls])
        self.prog["pool"].append(("op", lambda e: e.collective_compute(kind, ALU.bypass, replica_groups=groups, ins=ins, outs=outs),
                                  owner.sem[cls], 1))
        for t in r:
            t.readers.append(ev)
        for t in w:
            t.last_w = ev
            t.readers = []
        self.ninst += 1

    def barrier(self):
        for eng in ENGS:
            waits = {}
            for e2 in ENGS:
                if e2 != eng and self.ecnt[e2] > 0:
                    self._need(eng, waits, (("e", e2, self.phase), self.ecnt[e2]))
            for (t, cls) in self.dma_toks:
                self._need(eng, waits, (("d", t.uid, cls), t.semcnt[cls]))
            self._emit_waits(eng, waits)

    def finish(self):
        self.barrier()
        nc = self.nc
        engmap = {"pe": "tensor", "dve": "vector", "act": "scalar", "pool": "gpsimd", "sp": "sync"}
        with nc.Block() as block:
            for e in ENGS:
                items = self.prog[e]

                def body(engobj, items=items):
                    for it in items:
                        if it[0] == "wait":
                            engobj.wait_ge(it[1], it[2])
                        else:
                            it[1](engobj).then_inc(it[2], it[3])
                getattr(block, engmap[e])(body)
        self.ctx.close()


class Bufs:
    def __init__(self, k, name, shape, dtype, n, space="sb"):
        mk = k.sb if space == "sb" else k.ps
        self.t = [mk(name, shape, dtype) for _ in range(n)]
        self.tok = [Tok(name, excl=(space == "ps")) for _ in range(n)]
        self.i = -1

    def next(self):
        self.i = (self.i + 1) % len(self.t)
        return self.t[self.i], self.tok[self.i]


def make_consts(k):
    c = {}
    idf = k.sb("identf", [128, 128], F32)
    idb = k.sb("identb", [128, 128], BF16)
    t = Tok("ident")
    k.op("pool", lambda e: e.memset(idf[:], 0.0), w=[t])
    k.op("pool", lambda e: e.affine_select(out=idf[:], in_=idf[:], pattern=[[-1, 128]], compare_op=ALU.not_equal,
                                           fill=1.0, base=0, channel_multiplier=1), r=[t], w=[t])
    k.op("pool", lambda e: e.tensor_copy(idb[:], idf[:]), r=[t], w=[t])
    c["idf"], c["idb"], c["t_id"] = idf, idb, t
    return c


def emit_transpose_phase(k, c, h_tm, xt_fm, NT):
    k.scope()
    ntile = NT // 128
    hf = Bufs(k, "tp_hf", [128, D], F32, 3)
    hb = Bufs(k, "tp_h", [128, D], BF16, 2)
    pt = Bufs(k, "tp_pt", [128, 8, 128], BF16, 2, "ps")
    ob = Bufs(k, "tp_o", [128, 8, 128], BF16, 2)
    zc = k.sb("tp_z", [128, 8, 1], BF16)
    tzc = Tok("tp_z")
    k.op("pool", lambda e: e.memset(zc[:], 0.0), w=[tzc])
    xv = xt_fm.rearrange("(kc p) t -> p kc t", p=128)
    for col in (0, NT + 1):
        k.dma("sp", xv[:, :, col:col + 1], zc[:], tzc, r=[tzc], allow_slow_non_contiguous=True)
    for tt in range(ntile):
        f, tf = hf.next()
        k.dma("sp", f[:], h_tm[tt * 128:(tt + 1) * 128, :], tf, w=[tf])
        h, th = hb.next()
        k.op("dve", lambda e: e.tensor_copy(h[:], f[:]), r=[tf], w=[th])
        p, tp = pt.next()
        for kc in range(8):
            k.op("pe", lambda e, kc=kc: e.transpose(p[:, kc, :], h[:, kc * 128:(kc + 1) * 128], c["idb"][:]),
                 r=[th, c["t_id"]], w=[tp])
        o, to = ob.next()
        k.op("act", lambda e: e.copy(out=o[:], in_=p[:]), r=[tp], w=[to])
        dst = xt_fm.rearrange("(kc p) t -> p kc t", p=128)[:, :, 1 + tt * 128: 1 + (tt + 1) * 128]
        k.dma("act", dst, o[:], to, r=[to])
    k.end_scope()


def emit_ln(k, z, tz, gbc, bbc, tgb, st, tst, out, tout, eng2="pool"):
    for hh in range(2):
        k.op("dve", lambda e, hh=hh: e.bn_stats(st[:, hh * 6:(hh + 1) * 6], z[:, hh * 512:(hh + 1) * 512]), r=[tz], w=[tst])
    k.op("dve", lambda e: e.bn_aggr(st[:, 12:14], st[:, 0:12]), r=[tst], w=[tst])
    k.op("act", lambda e: e.activation(out=st[:, 14:15], in_=st[:, 13:14], func=AF.Ln, bias=LN_EPS, scale=1.0), r=[tst], w=[tst])
    k.op("act", lambda e: e.activation(out=st[:, 14:15], in_=st[:, 14:15], func=AF.Exp, scale=-0.5), r=[tst], w=[tst])
    k.op("dve", lambda e: e.tensor_scalar(out=z[:], in0=z[:], scalar1=st[:, 12:13], scalar2=st[:, 14:15],
                                          op0=ALU.subtract, op1=ALU.mult), r=[tz, tst], w=[tz])
    k.op(eng2, lambda e: e.tensor_tensor(out=z[:], in0=z[:], in1=gbc[:], op=ALU.mult), r=[tz, tgb], w=[tz])
    k.op(eng2, lambda e: e.tensor_tensor(out=out[:], in0=z[:], in1=bbc[:], op=ALU.add), r=[tz, tgb], w=[tout])


def emit_moe_phase(k, c, h_tm, out_tm, w1, w3, w2, router_w, router_b, ln_g, ln_b, NT, TQ=1024, ne=NE):
    k.scope()
    nq = NT // TQ
    ntile = TQ // 128
    nblk = TQ // 512
    tconst = Tok("moe_const")
    rw = k.sb("rw", [128, 8, NE], F32)
    k.dma("sp", rw[:], router_w.rearrange("(kc p) e -> p kc e", p=128), tconst, w=[tconst])
    rb = k.sb("rb", [128, NE], F32)
    k.dma("sp", rb[:], router_b.partition_broadcast(128), tconst, w=[tconst])
    gbc = k.sb("gbc", [128, D], F32)
    bbc = k.sb("bbc", [128, D], F32)
    k.dma("sp", gbc[:], ln_g.partition_broadcast(128), tconst, w=[tconst])
    k.dma("sp", bbc[:], ln_b.partition_broadcast(128), tconst, w=[tconst])

    xT = k.sb("xT", [128, 8, TQ], BF16)
    t_xT = [Tok("xT%d" % i) for i in range(ntile)]
    acc = k.sb("acc", [128, ntile, D], F32)
    t_acc = [Tok("acc%d" % i) for i in range(ntile)]
    lg = k.sb("lg", [128, ntile, NE], F32)
    t_lg = Tok("lg")
    gates = k.sb("gates", [128, ntile, NE], F32)
    t_gates = Tok("gates")
    hst = Bufs(k, "hst", [128, D], F32, 2)
    xTf = Bufs(k, "xTf", [128, 8, 128], F32, 2)
    ptr = Bufs(k, "ptr", [128, 4, 128], F32, 1, "ps")
    plg = Bufs(k, "plg", [128, 512], F32, 1, "ps")
    p13 = Bufs(k, "p13", [128, 2, 512], F32, 2, "ps")
    py = Bufs(k, "py", [128, 512], F32, 2, "ps")
    wst = Bufs(k, "wst", [128, 2, D], F32, MOE_NWST)
    wring = Bufs(k, "wring", [128, 8, D], BF16, MOE_NRING)
    hid = Bufs(k, "hid", [128, 8, 512], BF16, 2)
    sil = Bufs(k, "sil", [128, 512], BF16, 2)
    rt = [k.sb("rt%d" % i, [128, ntile, NE], F32) for i in range(3)]
    rs = [k.sb("rs%d" % i, [128, ntile, 4], F32) for i in range(4)]
    t_rt = Tok("rt")
    st = k.sb("lnst", [128, 16], F32)
    t_st = Tok("lnst")
    ob = Bufs(k, "moe_o", [128, D], F32, 2)
    gmx = k.sb("gmx", [128, ntile, 1], F32)

    def load_w(wd, e, direct=False):
        wb, twb = wring.next()
        src = wd[e].rearrange("(kc p) n -> p kc n", p=128)
        if direct:
            for j in range(MOE_DSPLIT):
                n_ = 8 // MOE_DSPLIT
                k.dma("pool", wb[:, n_ * j:n_ * (j + 1), :], src[:, n_ * j:n_ * (j + 1), :], twb, w=[twb])
            return wb, twb
        for j in range(4):
            s, ts = wst.next()
            k.dma("sp", s[:], src[:, 2 * j:2 * j + 2, :], ts, w=[ts])
            if j == 3:
                k.op("pool", lambda e_, wb=wb, s=s, j=j: e_.tensor_copy(wb[:, 2 * j:2 * j + 2, :], s[:]), r=[ts], w=[twb])
            else:
                k.op("act", lambda e_, wb=wb, s=s, j=j: e_.copy(out=wb[:, 2 * j:2 * j + 2, :], in_=s[:]), r=[ts], w=[twb])
        return wb, twb

    for q in range(nq):
        t0 = q * TQ
        for tt in range(ntile):
            h, th = hst.next()
            k.dma("sp", h[:], h_tm[t0 + tt * 128: t0 + (tt + 1) * 128, :], th, w=[th])
            k.op("act", lambda e, h=h, tt=tt: e.mul(out=acc[:, tt, :], in_=h[:], mul=ALPHA), r=[th], w=[t_acc[tt]])
            xf, txf = xTf.next()
            for half in range(2):
                p, tp = ptr.next()
                for j in range(4):
                    kc = half * 4 + j
                    k.op("pe", lambda e, p=p, h=h, j=j, kc=kc: e.transpose(p[:, j, :], h[:, kc * 128:(kc + 1) * 128], c["idf"][:]),
                         r=[th, c["t_id"]], w=[tp])
                k.op("act", lambda e, xf=xf, p=p, half=half: e.copy(out=xf[:, half * 4:(half + 1) * 4, :], in_=p[:]), r=[tp], w=[txf])
                k.op("dve", lambda e, xf=xf, half=half, tt=tt: e.tensor_copy(xT[:, half * 4:(half + 1) * 4, tt * 128:(tt + 1) * 128],
                                                                            xf[:, half * 4:(half + 1) * 4, :]), r=[txf], w=[t_xT[tt]])
            pl, tpl = plg.next()
            for kc in range(8):
                k.op("pe", lambda e, pl=pl, xf=xf, kc=kc: e.matmul(pl[:, 0:NE], lhsT=xf[:, kc, :], rhs=rw[:, kc, :], start=(kc == 0), stop=(kc == 7)),
                     r=[txf, tconst], w=[tpl])
            k.op("dve", lambda e, pl=pl, tt=tt: e.tensor_copy(lg[:, tt, :], pl[:, 0:NE]), r=[tpl], w=[t_lg])
        R = [t_lg, t_rt, t_gates, tconst]

        def dv(fn):
            k.op("dve", fn, r=R, w=[t_rt, t_gates])
        mx, sm, den = rs[0][:, :, 0:1], rs[1][:, :, 0:1], rs[2][:, :, 0:1]
        bc16 = lambda a: a.broadcast_to([128, ntile, NE])
        dv(lambda e: e.tensor_reduce(out=mx, in_=lg[:], axis=AX.X, op=ALU.max))
        dv(lambda e: e.tensor_tensor(out=rt[0][:], in0=lg[:], in1=bc16(mx), op=ALU.subtract))
        k.op("act", lambda e: e.activation(out=rt[0][:], in_=rt[0][:], func=AF.Exp), r=R, w=[t_rt])
        dv(lambda e: e.tensor_reduce(out=sm, in_=rt[0][:], axis=AX.X, op=ALU.add))
        dv(lambda e: e.reciprocal(out=sm, in_=sm))
        dv(lambda e: e.tensor_tensor(out=rt[0][:], in0=rt[0][:], in1=bc16(sm), op=ALU.mult))
        dv(lambda e: e.tensor_tensor(out=rt[1][:], in0=rt[0][:], in1=rb[:].unsqueeze(1).broadcast_to([128, ntile, NE]), op=ALU.add))
        b4 = rt[1][:].rearrange("p t (g e) -> p t g e", e=4)
        w4 = rt[2][:].rearrange("p t (g e) -> p t g e", e=4)
        bc4 = lambda a: a.unsqueeze(3).broadcast_to([128, ntile, 4, 4])
        dv(lambda e: e.tensor_reduce(out=rs[0][:], in_=b4, axis=AX.X, op=ALU.max))
        dv(lambda e: e.tensor_tensor(out=w4, in0=b4, in1=bc4(rs[0][:]), op=ALU.is_equal))
        dv(lambda e: e.scalar_tensor_tensor(out=rt[2][:], in0=rt[2][:], scalar=-1e9, in1=rt[1][:], op0=ALU.mult, op1=ALU.add))
        dv(lambda e: e.tensor_reduce(out=rs[1][:], in_=w4, axis=AX.X, op=ALU.max))
        dv(lambda e: e.tensor_tensor(out=rs[2][:], in0=rs[0][:], in1=rs[1][:], op=ALU.add))
        dv(lambda e: e.tensor_reduce(out=gmx[:], in_=rs[2][:], axis=AX.X, op=ALU.max))
        dv(lambda e: e.tensor_tensor(out=rs[3][:], in0=rs[2][:], in1=gmx[:].broadcast_to([128, ntile, 4]), op=ALU.is_equal))
        dv(lambda e: e.tensor_tensor(out=w4, in0=b4, in1=bc4(rs[1][:]), op=ALU.is_ge))
        dv(lambda e: e.tensor_tensor(out=w4, in0=w4, in1=bc4(rs[3][:]), op=ALU.mult))
        dv(lambda e: e.tensor_tensor(out=rt[2][:], in0=rt[2][:], in1=rt[0][:], op=ALU.mult))
        dv(lambda e: e.tensor_reduce(out=den, in_=rt[2][:], axis=AX.X, op=ALU.add))
        dv(lambda e: e.reciprocal(out=den, in_=den))
        dv(lambda e: e.tensor_tensor(out=gates[:], in0=rt[2][:], in1=bc16(den), op=ALU.mult))
        for ex in range(ne):
            w1b, tw1 = load_w(w1, ex, direct=MOE_DIRECT_ALL)
            w3b, tw3 = load_w(w3, ex, direct=MOE_DIRECT_ALL)
            w2b, tw2 = load_w(w2, ex, direct=MOE_DIRECT_W2)
            for tb in range(nblk):
                hd, thd = hid.next()
                xtoks = t_xT[tb * 4:(tb + 1) * 4]
                for cc in range(8):
                    p, tp = p13.next()
                    for (wi, wb, tw) in ((0, w1b, tw1), (1, w3b, tw3)):
                        for kc in range(8):
                            k.op("pe", lambda e, p=p, wi=wi, wb=wb, kc=kc, cc=cc, tb=tb: e.matmul(
                                p[:, wi, :], lhsT=wb[:, kc, cc * 128:(cc + 1) * 128], rhs=xT[:, kc, tb * 512:(tb + 1) * 512],
                                start=(kc == 0), stop=(kc == 7)), r=[tw] + xtoks, w=[tp])
                    s, ts = sil.next()
                    k.op("act", lambda e, s=s, p=p: e.activation(out=s[:], in_=p[:, 0, :], func=AF.Silu), r=[tp], w=[ts])
                    k.op("dve", lambda e, hd=hd, cc=cc, s=s, p=p: e.tensor_tensor(out=hd[:, cc, :], in0=s[:], in1=p[:, 1, :], op=ALU.mult),
                         r=[ts, tp], w=[thd])
                for t4 in range(4):
                    tt = tb * 4 + t4
                    for half in range(2):
                        y, ty = py.next()
                        for cc in range(8):
                            k.op("pe", lambda e, y=y, hd=hd, cc=cc, t4=t4, half=half, w2b=w2b: e.matmul(
                                y[:], lhsT=hd[:, cc, t4 * 128:(t4 + 1) * 128], rhs=w2b[:, cc, half * 512:(half + 1) * 512],
                                start=(cc == 0), stop=(cc == 7)), r=[thd, tw2], w=[ty])
                        k.op("dve", lambda e, y=y, tt=tt, half=half, ex=ex: e.scalar_tensor_tensor(
                            out=acc[:, tt, half * 512:(half + 1) * 512], in0=y[:], scalar=gates[:, tt, ex:ex + 1],
                            in1=acc[:, tt, half * 512:(half + 1) * 512], op0=ALU.mult, op1=ALU.add),
                            r=[ty, t_gates, t_acc[tt]], w=[t_acc[tt]])
        for tt in range(ntile):
            o, to = ob.next()
            emit_ln(k, acc[:, tt, :], t_acc[tt], gbc, bbc, tconst, st, t_st, o, to, eng2="dve")
            k.dma("sp", out_tm[t0 + tt * 128: t0 + (tt + 1) * 128, :], o[:], to, r=[to])
    k.end_scope()


MOE_DIRECT_W2 = True
MOE_DIRECT_ALL = True
MOE_DSPLIT = 1
MOE_NWST = 1
MOE_NRING = 6
L = 64
VP = 130


def mixer_layout(layer):
    fm = []
    sgs = []
    if layer == 0:
        for h in range(4):
            fm.append(("qa%d" % h, 128))
        for h in range(4):
            fm.append(("fa%d" % h, 128))
        for h in range(4):
            fm.append(("qb%d" % h, 64))
        for h in range(4):
            fm.append(("kb%d" % h, 64))
        fm.append(("rb", 16))
        for h in range(4):
            sgs.append(dict(typ="A", h=h, dk=128, hv=h, qscale=1.0, lfscale=1.0, dve=128))
        for h in range(4):
            sgs.append(dict(typ="B", h=h, dk=64, hv=4 + h, qscale=0.125, lfscale=-1.0 / 16.0, dve=128))
    else:
        for nm in ("qc", "qs", "kc", "ks"):
            for h in range(4):
                fm.append(("%s%d" % (nm, h), 64))
        for h in range(4):
            fm.append(("qd%d" % h, 128))
        for h in range(4):
            fm.append(("kd%d" % h, 128))
        fm.append(("gd", 8))
        for h in range(4):
            sgs.append(dict(typ="C", h=h, dk=64, hv=h, qscale=0.125, lfscale=1.0, dve=128))
        for h in range(4):
            sgs.append(dict(typ="D", h=h, dk=128, hv=4 + h, qscale=128.0 ** -0.5, lfscale=-1.0, dve=129))
    return fm, sgs


def emit_mixer_pass(k, c, layer, pidx, NT, xt_fm, P, T=256):
    k.scope()
    fm, sgs = mixer_layout(layer)
    asc = (pidx == 1)
    NCH = T // L
    NTL = T // 128
    nblk = NT // T
    goff = {}
    off = 0
    for gi, (nm, wd) in enumerate(fm):
        goff[nm] = (gi, off, wd)
        off += wd
    CF = off
    CT = 1024 if pidx == 1 else 2048
    tc_ = Tok("mx_const")

    wfm = k.sb("wfm", [128, 8, CF], BF16)
    t_wfm = [Tok("wfm%d" % i) for i in range(8)]
    for kc in range(8):
        k.dma("pool", wfm[:, kc, :], P["wfm"][kc * 128:(kc + 1) * 128, :], t_wfm[kc], w=[t_wfm[kc]])
    bfm = k.sb("bfm", [128, len(fm)], F32)
    k.dma("sp", bfm[:], P["bfm"], tc_, w=[tc_])
    wtm = k.sb("wtm", [128, 8, CT], BF16)
    t_wtm = [Tok("wtm%d" % i) for i in range(8)]
    for kc in range(8):
        k.dma("pool", wtm[:, kc, :], P["wtm"][kc * 128:(kc + 1) * 128, 0:CT], t_wtm[kc], w=[t_wtm[kc]])
    btm = k.sb("btm", [128, CT], F32)
    k.dma("sp", btm[:], P["btm"][:, 0:CT].partition_broadcast(128), tc_, w=[tc_])
    rmask = k.sb("rmask", [128, T], F32)
    k.op("pool", lambda e: e.memset(rmask[:], 1.0), w=[tc_])
    rpos = 0 if asc else L - 1
    k.op("pool", lambda e: e.memset(rmask[:].rearrange("p (c l) -> p c l", l=L)[:, :, rpos:rpos + 1], 0.0), w=[tc_])
    smask = k.sb("smask", [128, 128], F32)
    k.op("pool", lambda e: e.memset(smask[:], 1.0), w=[tc_])
    if asc:
        k.op("pool", lambda e: e.affine_select(out=smask[:], in_=smask[:], pattern=[[1, 128]], compare_op=ALU.is_ge, fill=0.0,
                                               base=0, channel_multiplier=-1), w=[tc_])
    else:
        k.op("pool", lambda e: e.affine_select(out=smask[:], in_=smask[:], pattern=[[-1, 128]], compare_op=ALU.is_ge, fill=0.0,
                                               base=0, channel_multiplier=1), w=[tc_])
    k.op("pool", lambda e: e.memset(smask[0:64, 64:128], 0.0), w=[tc_])
    k.op("pool", lambda e: e.memset(smask[64:128, 0:64], 0.0), w=[tc_])

    ex = {}
    if layer == 0:
        hl = k.sb("hl", [128, 4, 3], F32)
        k.dma("sp", hl[:], P["hgrn_lb"], tc_, w=[tc_])
        k.op("act", lambda e: e.activation(out=hl[:], in_=hl[:], func=AF.Exp), r=[tc_], w=[tc_])
        hs = k.sb("hs", [128, 4, 1], F32)
        lb = k.sb("lb", [128, 4, 1], F32)
        oml = k.sb("oml", [128, 4, 1], F32)
        k.op("dve", lambda e: e.tensor_reduce(out=hs[:], in_=hl[:], axis=AX.X, op=ALU.add), r=[tc_], w=[tc_])
        k.op("dve", lambda e: e.reciprocal(out=hs[:], in_=hs[:]), r=[tc_], w=[tc_])
        k.op("dve", lambda e: e.tensor_tensor(out=lb[:], in0=hl[:, :, 0:1], in1=hs[:], op=ALU.mult), r=[tc_], w=[tc_])
        k.op("dve", lambda e: e.tensor_scalar(out=oml[:], in0=lb[:], scalar1=-1.0, scalar2=1.0, op0=ALU.mult, op1=ALU.add), r=[tc_], w=[tc_])
        gku = k.sb("gku", [16, 256], F32)
        k.dma("sp", gku[:], P["gk_up"], tc_, w=[tc_])
        ngkb = k.sb("ngkb", [64, 4], F32)
        k.dma("sp", ngkb[:], P["gk_b"], tc_, w=[tc_])
        k.op("dve", lambda e: e.tensor_scalar(out=ngkb[:], in0=ngkb[:], scalar1=-1.0, scalar2=None, op0=ALU.mult), r=[tc_], w=[tc_])
        rbs = k.sb("rbs", [16, T], F32)
        t_rbs = Tok("rbs")
    else:
        lgam = k.sb("lgam", [64, 4], F32)
        k.dma("sp", lgam[:], P["lgam"], tc_, w=[tc_])
        cw = k.sb("cw", [128, 8, 3], F32)
        k.dma("sp", cw[:], P["conv_w"], tc_, w=[tc_])
        cb = k.sb("cb", [128, 8], F32)
        k.dma("sp", cb[:], P["conv_b"], tc_, w=[tc_])
        selm = k.sb("selm", [8, 8, 128], F32)
        k.dma("sp", selm[:], P["selm"], tc_, w=[tc_])
        nfb = k.sb("nfb", [128, 4], F32)
        k.dma("sp", nfb[:], P["fgb"].partition_broadcast(128), tc_, w=[tc_])
        lfb = k.sb("lfb", [128, 4], F32)
        k.dma("sp", lfb[:], P["lfb"].partition_broadcast(128), tc_, w=[tc_])
        k.op("dve", lambda e: e.tensor_tensor(out=nfb[:], in0=nfb[:], in1=lfb[:], op=ALU.add), r=[tc_], w=[tc_])
        k.op("dve", lambda e: e.tensor_scalar(out=nfb[:], in0=nfb[:], scalar1=-1.0, scalar2=None, op0=ALU.mult), r=[tc_], w=[tc_])
        lib = k.sb("lib", [128, 4], F32)
        k.dma("sp", lib[:], P["lib"].partition_broadcast(128), tc_, w=[tc_])
        gds = k.sb("gds", [8, T], F32)
        t_gds = Tok("gds")
        hal = k.sb("hal", [128, 8], F32)
        if "halo_pair" in P:
            hal2 = k.sb("hal2", [128, 2, 8], F32)
            for r_ in range(2):
                k.dma("sp", hal2[:, r_, :], P["halo_pair"][r_].rearrange("(kc p) -> p kc", p=128), tc_, r=P["cc_tok"], w=[tc_], allow_slow_non_contiguous=True)
            selh = k.sb("selh", [128, 2], F32)
            k.dma("sp", selh[:], P["sel"].partition_broadcast(128), tc_, w=[tc_])
            k.op("dve", lambda e: e.tensor_scalar(out=hal[:], in0=hal2[:, 0, :], scalar1=selh[:, 0:1], scalar2=None, op0=ALU.mult), r=[tc_], w=[tc_])
            k.op("dve", lambda e: e.scalar_tensor_tensor(out=hal[:], in0=hal2[:, 1, :], scalar=selh[:, 1:2], in1=hal[:], op0=ALU.mult, op1=ALU.add),
                 r=[tc_], w=[tc_])
        else:
            k.dma("sp", hal[:], P["halo"].rearrange("(kc p) -> p kc", p=128), tc_, w=[tc_], allow_slow_non_contiguous=True)
        cst = Bufs(k, "cst", [64, 2, T], F32, 2)

    PF = Bufs(k, "PF", [128, 512], F32, 2, "ps")
    PX = Bufs(k, "PX", [128, 512], F32, 3, "ps")
    PO = [k.ps("PO", [128, 512], F32) for _ in range(3)]
    t_PO = [Tok("PO%d" % i, True) for i in range(3)]
    if layer == 0:
        oslots = [(j // 4, (j % 4) * 128) for j in range(8)]
        stgroups = [[0, 1, 2, 3], [4, 5, 6, 7]]
    else:
        oslots = [(0, j * 128) for j in range(4)] + [(1, 0), (1, 256), (2, 0), (2, 256)]
        stgroups = [[0, 1, 2, 3], [4, 5], [6, 7]]
    ofirst = set()
    seenb = set()
    for j, (bk, _) in enumerate(oslots):
        if bk not in seenb:
            seenb.add(bk)
            ofirst.add(j)

    xT = Bufs(k, "mxT", [128, 8, T + 2], BF16, 2)
    NSET = globals().get("MIX_NSET") or {(0, 1): 4, (1, 1): 4, (0, 2): 4, (1, 2): 2}[(layer, pidx)]
    tsets = []
    for _ in range(NSET):
        st_ = []
        for nm_ in ("Tq", "Tk", "Tlf", "TG", "Te1", "Te2"):
            st_ += [k.sb(nm_, [128, T], F32), Tok(nm_)]
        st_ += [k.sb("Tsm", [128, 4, NCH], F32), Tok("Tsm")]
        if layer == 1:
            st_ += [k.sb("aext", [128, T + 2], F32), Tok("aext")]
        tsets.append(st_)
    nsg = len(sgs)
    NPAR = globals().get("MIX_NPAR", {(0, 1): 1, (1, 1): 1, (0, 2): 1, (1, 2): 1})[(layer, pidx)]
    QT = [[k.sb("QT", [128, T], BF16) for _ in range(nsg)] for _ in range(NPAR)]
    KT = [[k.sb("KT", [128, T], BF16) for _ in range(nsg)] for _ in range(NPAR)]
    QIP = [[k.sb("QIP", [128, NCH, 128], BF16) for _ in range(nsg)] for _ in range(NPAR)]
    KST = [[k.sb("KST", [128, T], BF16) for _ in range(nsg)] for _ in range(NPAR)]
    KS = [k.sb("KS", [128, NTL, 128], BF16) for _ in range(nsg)]
    DEC = [[k.sb("DEC", [128, NCH], F32) for _ in range(nsg)] for _ in range(NPAR)]
    t_sg = [[Tok("sg%d" % i) for i in range(nsg)] for _ in range(NPAR)]
    t_ks = [Tok("ks%d" % i) for i in range(nsg)]
    for par in range(NPAR):
        for i in range(nsg):
            k.op("pool", lambda e, i=i, par=par: e.memset(QIP[par][i][:], 0.0), w=[t_sg[par][i]])
    if "sel" in P:
        selb = k.sb("selb", [128, 2], F32)
        k.dma("sp", selb[:], P["sel"].partition_broadcast(128), tc_, w=[tc_])
        stmp = Bufs(k, "stmp", [128, 2, VP], F32, 2)
    S32 = [k.sb("S32", [128, VP], F32) for _ in range(nsg)]
    Sb = [k.sb("Sb", [128, VP], BF16) for _ in range(nsg)]
    t_S = [Tok("S%d" % i) for i in range(nsg)]
    t_Sb = [Tok("Sb%d" % i) for i in range(nsg)]
    for i in range(nsg):
        if pidx == 1:
            k.op("pool", lambda e, i=i: e.memset(S32[i][:], 0.0), w=[t_S[i]])
        elif "state_pair" in P:
            sa, tsa = stmp.next()
            k.dma("sp", sa[:], P["state_pair"][:, i].rearrange("r p v -> p r v"), tsa, r=P["cc_tok"], w=[tsa])
            k.op("dve", lambda e, i=i, sa=sa: e.tensor_scalar(out=S32[i][:], in0=sa[:, 0, :], scalar1=selb[:, 0:1], scalar2=None, op0=ALU.mult),
                 r=[tsa, tc_], w=[t_S[i]])
            k.op("dve", lambda e, i=i, sa=sa: e.scalar_tensor_tensor(out=S32[i][:], in0=sa[:, 1, :], scalar=selb[:, 1:2], in1=S32[i][:],
                                                                   op0=ALU.mult, op1=ALU.add), r=[tsa, tc_, t_S[i]], w=[t_S[i]])
        else:
            k.dma("sp", S32[i][:], P["state_in"][i], t_S[i], w=[t_S[i]])
        k.op("pool", lambda e, i=i: e.tensor_copy(Sb[i][:], S32[i][:]), r=[t_S[i]], w=[t_Sb[i]])
    V = Bufs(k, "V", [128, 8, VP], BF16, 2)
    for vb_ in V.t:
        k.op("pool", lambda e, vb_=vb_: e.memset(vb_[:], 1.0), w=[tc_])
    SCP = Bufs(k, "SCP", [128, 8, 128], BF16, 2)
    OT = Bufs(k, "OT", [128, 8, 128], F32, 2)
    rden = k.sb("rden", [128, 4, 1], F32); t_rden = Tok("rden")
    if pidx == 2:
        wout = k.sb("wout", [128, 8, D], BF16)
        t_wout = [Tok("wout%d" % i) for i in range(8)]
        for kc in range(8):
            k.dma("pool", wout[:, kc, :], P["w_out"][kc * 128:(kc + 1) * 128, :], t_wout[kc], w=[t_wout[kc]])
        gnb = k.sb("gnb", [128, D], F32)
        k.dma("sp", gnb[:], P["norm_g"].partition_broadcast(128), tc_, w=[tc_])
        lgb = k.sb("lgb", [128, D], F32)
        lbb = k.sb("lbb", [128, D], F32)
        k.dma("sp", lgb[:], P["ln_g"].partition_broadcast(128), tc_, w=[tc_])
        k.dma("sp", lbb[:], P["ln_b"].partition_broadcast(128), tc_, w=[tc_])
        O1 = Bufs(k, "O1", [128, 8, 128], F32, 1)
        XR = Bufs(k, "XR", [128, D], F32, 1)
        GA = Bufs(k, "GA", [128, D], F32, 1)
        ssq = k.sb("ssq", [128, 8, 1], F32); t_ssq = Tok("ssq")
        MX = Bufs(k, "MX", [128, D], BF16, 1)
        MXT = Bufs(k, "MXT", [128, 8, 128], BF16, 1)
        ZT = Bufs(k, "ZT", [128, D], F32, 1)
        HO = Bufs(k, "HO", [128, D], F32, 1)
        lst = k.sb("mlnst", [128, 16], F32); t_lst = Tok("mlnst")

    def c3(ap, n=L):
        return ap.rearrange("p (c l) -> p c l", l=n)

    def rv(tile_, dk):
        return bass.AP(tile_, T - 1, [[T, dk], [-1, T]])

    def proj_fm(nm, x, tx, cols=None):
        gi, o, wd = goff[nm]
        p, tp = PF.next()
        n = T if cols is None else 2
        for kc in range(8):
            rhs = x[:, kc, 1:T + 1] if cols is None else bass.AP(x, kc * (T + 2), [[8 * (T + 2), 128], [T + 1, 2]])
            k.op("pe", lambda e, p=p, kc=kc, rhs=rhs, o=o, wd=wd, n=n: e.matmul(p[0:wd, 0:n], lhsT=wfm[:, kc, o:o + wd], rhs=rhs,
                                                                              start=(kc == 0), stop=(kc == 7)), r=[tx, tc_, t_wfm[kc]], w=[tp])
        return p, tp, gi, wd

    last = L - 1 if asc else 0
    ref = L // 2 - 1 if asc else L // 2

    blocks = list(range(nblk)) if asc else list(range(nblk - 1, -1, -1))
    xs = {}

    def gm_block(b, par):
        t0 = b * T
        x, tx = xT.next()
        xs[b] = (x, tx)
        k.dma("sp", x[:], xt_fm.rearrange("(kc p) t -> p kc t", p=128)[:, :, t0:t0 + T + 2], tx, w=[tx])
        if layer == 1 and b == nblk - 1:
            k.op("pool", lambda e, x=x: e.tensor_copy(x[:, :, T + 1:T + 2], hal[:].unsqueeze(2)), r=[tx, tc_], w=[tx])
        if layer == 0:
            p, tp, gi, wd = proj_fm("rb", x, tx)
            k.op("act", lambda e, p=p, gi=gi: e.activation(out=rbs[:], in_=p[0:16, 0:T], func=AF.Identity, bias=bfm[0:16, gi:gi + 1], scale=1.0),
                 r=[tp, tc_], w=[t_rbs])
        else:
            p, tp, gi, wd = proj_fm("gd", x, tx)
            k.op("act", lambda e, p=p: e.copy(out=gds[:], in_=p[0:8, 0:T]), r=[tp], w=[t_gds])
            cs, tcs = cst.next()
            k.dma("sp", cs[:], P["cs_tab"][:, :, t0:t0 + T].rearrange("a p t -> p a t"), tcs, w=[tcs])
        def sg_body(si, sg):
                dk, h, typ = sg["dk"], sg["h"], sg["typ"]
                tw = [t_sg[par][si]]
                ts_ = tsets[si % NSET]
                Tq, t_Tq, Tk, t_Tk, Tlf, t_Tlf, TG, t_TG, Te1, t_Te1, Te2, t_Te2, Tsm, t_Tsm = ts_[:14]
                if layer == 1:
                    aext, t_aext = ts_[14:16]
                if typ == "A":
                    p, tp, gi, wd = proj_fm("qa%d" % h, x, tx)
                    yield k.op("act", lambda e, p=p, gi=gi: e.activation(out=Tq[:], in_=p[:, 0:T], func=AF.Silu, bias=bfm[:, gi:gi + 1], scale=1.0),
                         r=[tp, tc_], w=[t_Tq])
                    p, tp, gi, wd = proj_fm("fa%d" % h, x, tx)
                    yield k.op("act", lambda e, p=p, gi=gi: e.activation(out=Tk[:], in_=p[:, 0:T], func=AF.Sigmoid, bias=bfm[:, gi:gi + 1], scale=1.0),
                         r=[tp, tc_], w=[t_Tk])
                    yield k.op("dve", lambda e, h=h: e.tensor_scalar(out=Tk[:], in0=Tk[:], scalar1=oml[:, h, :], scalar2=lb[:, h, :], op0=ALU.mult, op1=ALU.add),
                         r=[t_Tk, tc_], w=[t_Tk])
                    yield k.op("act", lambda e: e.activation(out=Tlf[:], in_=Tk[:], func=AF.Ln), r=[t_Tk], w=[t_Tlf])
                    yield k.op("dve", lambda e: e.tensor_scalar(out=Tk[:], in0=Tk[:], scalar1=-1.0, scalar2=1.0, op0=ALU.mult, op1=ALU.add),
                         r=[t_Tk, t_Tlf], w=[t_Tk])
                elif typ == "B":
                    p, tp, gi, wd = proj_fm("qb%d" % h, x, tx)
                    yield k.op("act", lambda e, p=p, gi=gi: e.activation(out=Tq[0:64, :], in_=p[0:64, 0:T], func=AF.Identity, bias=bfm[0:64, gi:gi + 1], scale=1.0),
                         r=[tp, tc_], w=[t_Tq])
                    p, tp, gi, wd = proj_fm("kb%d" % h, x, tx)
                    yield k.op("act", lambda e, p=p, gi=gi: e.activation(out=Tk[0:64, :], in_=p[0:64, 0:T], func=AF.Identity, bias=bfm[0:64, gi:gi + 1], scale=1.0),
                         r=[tp, tc_], w=[t_Tk])
                    p, tp = PF.next()
                    k.op("pe", lambda e, p=p, h=h: e.matmul(p[0:64, 0:T], lhsT=gku[:, h * 64:(h + 1) * 64], rhs=rbs[:], start=True, stop=True),
                         r=[t_rbs, tc_], w=[tp])
                    yield k.op("act", lambda e, p=p, h=h: e.activation(out=Tlf[0:64, :], in_=p[0:64, 0:T], func=AF.Exp, bias=ngkb[:, h:h + 1], scale=-1.0),
                         r=[tp, tc_], w=[t_Tlf])
                    yield k.op("act", lambda e: e.activation(out=Tlf[0:64, :], in_=Tlf[0:64, :], func=AF.Ln, bias=1.0, scale=1.0), r=[t_Tlf], w=[t_Tlf])
                elif typ == "C":
                    for (dst, tdst, n1, n2) in ((Tq, t_Tq, "qc", "qs"), (Tk, t_Tk, "kc", "ks")):
                        p, tp, gi, wd = proj_fm("%s%d" % (n1, h), x, tx)
                        yield k.op("dve", lambda e, p=p, gi=gi, dst=dst: e.scalar_tensor_tensor(out=dst[0:64, :], in0=p[0:64, 0:T], scalar=bfm[0:64, gi:gi + 1],
                                                                                       in1=cs[:, 0, :], op0=ALU.add, op1=ALU.mult), r=[tp, tc_, tcs], w=[tdst])
                        p, tp, gi, wd = proj_fm("%s%d" % (n2, h), x, tx)
                        yield k.op("dve", lambda e, p=p, gi=gi: e.scalar_tensor_tensor(out=Te1[0:64, :], in0=p[0:64, 0:T], scalar=bfm[0:64, gi:gi + 1],
                                                                              in1=cs[:, 1, :], op0=ALU.add, op1=ALU.mult), r=[tp, tc_, tcs], w=[t_Te1])
                        yield k.op("dve", lambda e, dst=dst: e.tensor_tensor(out=dst[0:64, :], in0=dst[0:64, :], in1=Te1[0:64, :], op=ALU.add), r=[tdst, t_Te1], w=[tdst])
                    yield k.op("dve", lambda e, h=h: e.tensor_copy(Tlf[0:64, :], lgam[:, h:h + 1].broadcast_to([64, T])), r=[tc_], w=[t_Tlf])
                else:
                    for (dst, tdst, nm, ci) in ((Tq, t_Tq, "qd", h), (Tk, t_Tk, "kd", 4 + h)):
                        p, tp, gi, wd = proj_fm("%s%d" % (nm, h), x, tx)
                        yield k.op("act", lambda e, p=p, gi=gi: e.activation(out=aext[:, 1:T + 1], in_=p[:, 0:T], func=AF.Identity, bias=bfm[:, gi:gi + 1], scale=1.0),
                             r=[tp, tc_], w=[t_aext])
                        p, tp, gi, wd = proj_fm("%s%d" % (nm, h), x, tx, cols=2)
                        yield k.op("act", lambda e, p=p, gi=gi: e.activation(out=bass.AP(aext, 0, [[T + 2, 128], [T + 1, 2]]), in_=p[:, 0:2], func=AF.Identity,
                                                                      bias=bfm[:, gi:gi + 1], scale=1.0), r=[tp, tc_], w=[t_aext])
                        if b == 0:
                            yield k.op("pool", lambda e: e.memset(aext[:, 0:1], 0.0), r=[t_aext], w=[t_aext])
                        yield k.op("dve", lambda e, ci=ci: e.tensor_scalar(out=Te1[:], in0=aext[:, 0:T], scalar1=cw[:, ci, 0:1], scalar2=cb[:, ci:ci + 1],
                                                                   op0=ALU.mult, op1=ALU.add), r=[t_aext, tc_], w=[t_Te1])
                        yield k.op("dve", lambda e, ci=ci: e.scalar_tensor_tensor(out=Te1[:], in0=aext[:, 1:T + 1], scalar=cw[:, ci, 1:2], in1=Te1[:],
                                                                          op0=ALU.mult, op1=ALU.add), r=[t_aext, tc_, t_Te1], w=[t_Te1])
                        yield k.op("dve", lambda e, ci=ci: e.scalar_tensor_tensor(out=Te1[:], in0=aext[:, 2:T + 2], scalar=cw[:, ci, 2:3], in1=Te1[:],
                                                                          op0=ALU.mult, op1=ALU.add), r=[t_aext, tc_, t_Te1], w=[t_Te1])
                        yield k.op("act", lambda e, dst=dst: e.activation(out=dst[:], in_=Te1[:], func=AF.Silu), r=[t_Te1], w=[tdst])
                    p, tp = PF.next()
                    k.op("pe", lambda e, p=p, h=h: e.matmul(p[:, 0:T], lhsT=selm[:, h, :], rhs=gds[:], start=True, stop=True), r=[t_gds, tc_], w=[tp])
                    yield k.op("act", lambda e, p=p, h=h: e.activation(out=Te1[:], in_=p[:, 0:T], func=AF.Exp, bias=lib[:, h:h + 1], scale=1.0), r=[tp, tc_], w=[t_Te1])
                    yield k.op("dve", lambda e: e.tensor_tensor(out=Tk[:], in0=Tk[:], in1=Te1[:], op=ALU.mult), r=[t_Tk, t_Te1], w=[t_Tk])
                    p, tp = PF.next()
                    k.op("pe", lambda e, p=p, h=h: e.matmul(p[:, 0:T], lhsT=selm[:, 4 + h, :], rhs=gds[:], start=True, stop=True), r=[t_gds, tc_], w=[tp])
                    yield k.op("act", lambda e, p=p, h=h: e.activation(out=Tlf[:], in_=p[:, 0:T], func=AF.Exp, bias=nfb[:, h:h + 1], scale=-1.0), r=[tp, tc_], w=[t_Tlf])
                    yield k.op("act", lambda e: e.activation(out=Tlf[:], in_=Tlf[:], func=AF.Ln, bias=1.0, scale=1.0), r=[t_Tlf], w=[t_Tlf])
                ls = sg["lfscale"]
                if asc:
                    yield k.op("dve", lambda e, dk=dk: e.tensor_tensor_scan(out=TG[0:dk, :], data0=rmask[0:dk, :], data1=Tlf[0:dk, :], initial=0.0,
                                                                     op0=ALU.mult, op1=ALU.add), r=[t_Tlf, tc_], w=[t_TG])
                else:
                    yield k.op("dve", lambda e, dk=dk: e.tensor_tensor_scan(out=rv(TG, dk), data0=rv(rmask, dk), data1=rv(Tlf, dk), initial=0.0,
                                                                     op0=ALU.mult, op1=ALU.add), r=[t_Tlf, tc_], w=[t_TG])
                G3 = c3(TG[0:dk, :])
                gref = G3[:, :, ref:ref + 1]
                glast = G3[:, :, last:last + 1]
                yield k.op("dve", lambda e, dk=dk, G3=G3, gref=gref: e.tensor_tensor(out=c3(Te1[0:dk, :]), in0=G3, in1=gref.broadcast_to([dk, NCH, L]), op=ALU.subtract),
                     r=[t_TG], w=[t_Te1])
                yield k.op("act", lambda e, dk=dk, ls=ls: e.activation(out=Te2[0:dk, :], in_=Te1[0:dk, :], func=AF.Exp, scale=-ls), r=[t_Te1], w=[t_Te2])
                yield k.op("act", lambda e, dk=dk, ls=ls: e.activation(out=Te1[0:dk, :], in_=Te1[0:dk, :], func=AF.Exp, scale=ls), r=[t_Te1], w=[t_Te1])
                sm = Tsm[0:dk]
                yield k.op("dve", lambda e, dk=dk, sm=sm, glast=glast, gref=gref: e.tensor_tensor(out=sm[:, 2, :].unsqueeze(2), in0=glast, in1=gref, op=ALU.subtract),
                     r=[t_TG, t_Tsm], w=[t_Tsm])
                yield k.op("act", lambda e, sm=sm, gref=gref, ls=ls: e.activation(out=sm[:, 0, :].unsqueeze(2), in_=gref, func=AF.Exp, scale=ls), r=[t_TG, t_Tsm], w=[t_Tsm])
                yield k.op("act", lambda e, sm=sm, ls=ls: e.activation(out=sm[:, 1, :], in_=sm[:, 2, :], func=AF.Exp, scale=ls), r=[t_Tsm], w=[t_Tsm])
                yield k.op("act", lambda e, dk=dk, si=si, glast=glast, ls=ls: e.activation(out=DEC[par][si][0:dk, :].unsqueeze(2), in_=glast, func=AF.Exp, scale=ls),
                     r=[t_TG] + tw, w=tw)
                yield k.op("dve", lambda e, dk=dk, si=si, qs=sg["qscale"]: e.scalar_tensor_tensor(out=QT[par][si][0:dk, :], in0=Tq[0:dk, :], scalar=qs, in1=Te1[0:dk, :],
                                                                                         op0=ALU.mult, op1=ALU.mult), r=[t_Tq, t_Te1] + tw, w=tw)
                yield k.op("pool", lambda e, dk=dk, si=si: e.tensor_tensor(out=KT[par][si][0:dk, :], in0=Tk[0:dk, :], in1=Te2[0:dk, :], op=ALU.mult),
                     r=[t_Tk, t_Te2] + tw, w=tw)
                for a in range(2):
                    qv = QIP[par][si][0:dk].rearrange("p (t a) (b l) -> p t a b l", a=2, b=2)[:, :, a, a, :]
                    yield k.op("pool", lambda e, dk=dk, si=si, a=a, qv=qv, sm=sm: e.tensor_tensor(
                        out=qv, in0=QT[par][si][0:dk, :].rearrange("p (t a l) -> p t a l", a=2, l=L)[:, :, a, :],
                        in1=sm[:, 0, :].rearrange("p (t a) -> p t a", a=2)[:, :, a:a + 1].broadcast_to([dk, NTL, L]), op=ALU.mult),
                        r=[t_Tsm] + tw, w=tw)
                yield k.op("dve", lambda e, dk=dk, si=si, sm=sm: e.tensor_tensor(out=c3(KST[par][si][0:dk, :]), in0=c3(KT[par][si][0:dk, :]),
                                                                          in1=sm[:, 1, :].unsqueeze(2).broadcast_to([dk, NCH, L]), op=ALU.mult),
                     r=[t_Tsm] + tw, w=tw)

        GW = globals().get("MIX_GW") or NSET
        for g0 in range(0, len(sgs), GW):
            gens = [sg_body(si, sgs[si]) for si in range(g0, min(g0 + GW, len(sgs)))]
            while gens:
                for g_ in list(gens):
                    try:
                        next(g_)
                    except StopIteration:
                        gens.remove(g_)
            yield

    def scan_block(b, par):
        x, tx = xs[b]
        t0 = b * T
        tiles = list(range(NTL)) if asc else list(range(NTL - 1, -1, -1))
        for tl in tiles:
            r0 = t0 + tl * 128
            v, tv = V.next()
            for half in range(2):
                p, tp = PF.next()
                for kc in range(8):
                    k.op("pe", lambda e, p=p, kc=kc, tl=tl, half=half: e.matmul(p[:], lhsT=x[:, kc, 1 + tl * 128:1 + (tl + 1) * 128],
                                                                             rhs=wtm[:, kc, half * 512:(half + 1) * 512], start=(kc == 0), stop=(kc == 7)),
                         r=[tx, tc_, t_wtm[kc]], w=[tp])
                k.op("dve", lambda e, p=p, v=v, half=half: e.tensor_tensor(out=v[:, half * 4:(half + 1) * 4, 0:128], in0=p[:].rearrange("p (h d) -> p h d", d=128),
                                                                        in1=btm[:, half * 512:(half + 1) * 512].rearrange("p (h d) -> p h d", d=128), op=ALU.add),
                     r=[tp, tc_], w=[tv])
            yield
            p, tp = PF.next()
            pb = p[:].bitcast(BF16).rearrange("p (s d) -> p s d", d=128)
            for si, sg in enumerate(sgs):
                dk = sg["dk"]
                k.op("pe", lambda e, pb=pb, si=si, dk=dk, tl=tl: e.transpose(pb[:, si, 0:dk], KST[par][si][0:dk, tl * 128:(tl + 1) * 128], c["idb"][0:dk, 0:dk]),
                     r=[t_sg[par][si], c["t_id"]], w=[tp])
            for hf in range(2):
                for si in range(hf * 4, hf * 4 + 4):
                    dk = sgs[si]["dk"]
                    k.op("act", lambda e, pb=pb, si=si, dk=dk, tl=tl: e.copy(out=KS[si][:, tl, 0:dk], in_=pb[:, si, 0:dk]), r=[tp], w=[t_ks[si]])
            yield
            sc, tsc = SCP.next()
            for hf in range(2):
                ps__, tps = PX.next()
                ps_ = ps__[:].rearrange("p (s d) -> p s d", d=128)
                for j in range(4):
                    si = hf * 4 + j
                    dk = sgs[si]["dk"]
                    k.op("pe", lambda e, ps_=ps_, j=j, si=si, dk=dk, tl=tl: e.matmul(ps_[:, j, :], lhsT=KT[par][si][0:dk, tl * 128:(tl + 1) * 128],
                                                                                  rhs=QT[par][si][0:dk, tl * 128:(tl + 1) * 128], start=True, stop=True),
                         r=[t_sg[par][si]], w=[tps])
                k.op("dve", lambda e, sc=sc, ps_=ps_, hf=hf: e.tensor_tensor(out=sc[:, hf * 4:(hf + 1) * 4, :], in0=ps_,
                                                                          in1=smask[:].unsqueeze(1).broadcast_to([128, 4, 128]), op=ALU.mult),
                     r=[tps, tc_], w=[tsc])
            yield
            corder = (0, 1) if asc else (1, 0)
            for si, sg in enumerate(sgs):
                bk, col = oslots[si]
                dve_ = sg["dve"]
                k.op("pe", lambda e, bk=bk, col=col, dve_=dve_, sc=sc, si=si, v=v, hv=sg["hv"], first=(si in ofirst): e.matmul(
                    PO[bk][:, col:col + dve_], lhsT=sc[:, si, :], rhs=v[:, hv, 0:dve_], start=first, stop=False, skip_group_check=True),
                    r=[tsc, tv], w=[t_PO[bk]])
            for ci, cc in enumerate(corder):
                yield
                ch = tl * 2 + cc
                for si, sg in enumerate(sgs):
                    bk, col = oslots[si]
                    dk, dve_ = sg["dk"], sg["dve"]
                    k.op("pe", lambda e, bk=bk, col=col, si=si, dk=dk, dve_=dve_, ch=ch, ci=ci: e.matmul(
                        PO[bk][:, col:col + dve_], lhsT=QIP[par][si][0:dk, ch, :], rhs=Sb[si][0:dk, 0:dve_],
                        start=False, stop=(ci == 1), skip_group_check=True), r=[t_sg[par][si], t_Sb[si]], w=[t_PO[bk]])
                for grp in stgroups:
                    pst, tpst = PX.next()
                    pitch = 512 // len(grp)
                    for gj, si in enumerate(grp):
                        sg = sgs[si]
                        dk, dve_ = sg["dk"], sg["dve"]
                        col = gj * pitch
                        k.op("pe", lambda e, pst=pst, col=col, si=si, dk=dk, dve_=dve_, cc=cc, tl=tl, v=v, hv=sg["hv"]: e.matmul(
                            pst[0:dk, col:col + dve_], lhsT=KS[si][cc * 64:(cc + 1) * 64, tl, 0:dk], rhs=v[cc * 64:(cc + 1) * 64, hv, 0:dve_],
                            start=True, stop=True), r=[t_ks[si], tv], w=[tpst])
                    for gj, si in enumerate(grp):
                        sg = sgs[si]
                        dk, dve_ = sg["dk"], sg["dve"]
                        col = gj * pitch
                        k.op("dve", lambda e, si=si, dk=dk, dve_=dve_, ch=ch, pst=pst, col=col: e.scalar_tensor_tensor(
                            out=S32[si][0:dk, 0:dve_], in0=S32[si][0:dk, 0:dve_], scalar=DEC[par][si][0:dk, ch:ch + 1], in1=pst[0:dk, col:col + dve_],
                            op0=ALU.mult, op1=ALU.add), r=[t_S[si], t_sg[par][si], tpst], w=[t_S[si]])
                        k.op("act", lambda e, si=si, dk=dk, dve_=dve_: e.copy(out=Sb[si][0:dk, 0:dve_], in_=S32[si][0:dk, 0:dve_]), r=[t_S[si]], w=[t_Sb[si]])
            yield
            ot, tot = OT.next()
            if pidx == 2:
                o1, to1 = O1.next()
                k.dma("sp", o1[:], P["o1"][r0:r0 + 128, :].rearrange("p (h d) -> p h d", d=128), to1, w=[to1])
            for hf in range(2):
                if layer == 1 and hf == 1:
                    for bnk in range(2):
                        pv = PO[1 + bnk][:].rearrange("p (s d) -> p s d", d=256)
                        k.op("act", lambda e, pv=pv, bnk=bnk: e.activation(out=rden[:, bnk * 2:bnk * 2 + 2, :], in_=pv[:, :, 128:129], func=AF.Abs),
                             r=[t_PO[1 + bnk], t_rden], w=[t_rden])
                        k.op("dve", lambda e, bnk=bnk: e.tensor_scalar(out=rden[:, bnk * 2:bnk * 2 + 2, :], in0=rden[:, bnk * 2:bnk * 2 + 2, :], scalar1=1.0, scalar2=None,
                                                                      op0=ALU.max), r=[t_rden], w=[t_rden])
                        k.op("dve", lambda e, bnk=bnk: e.reciprocal(out=rden[:, bnk * 2:bnk * 2 + 2, :], in_=rden[:, bnk * 2:bnk * 2 + 2, :]), r=[t_rden], w=[t_rden])
                        k.op("dve", lambda e, pv=pv, bnk=bnk, ot=ot: e.tensor_tensor(out=ot[:, 4 + bnk * 2:6 + bnk * 2, :], in0=pv[:, :, 0:128],
                                                                                  in1=rden[:, bnk * 2:bnk * 2 + 2, :].broadcast_to([128, 2, 128]), op=ALU.mult),
                             r=[t_PO[1 + bnk], t_rden], w=[tot])
                        if pidx == 2:
                            k.op("pool", lambda e, bnk=bnk, ot=ot, o1=o1: e.tensor_tensor(out=ot[:, 4 + bnk * 2:6 + bnk * 2, :], in0=ot[:, 4 + bnk * 2:6 + bnk * 2, :],
                                                                                       in1=o1[:, 4 + bnk * 2:6 + bnk * 2, :], op=ALU.add), r=[to1, tot], w=[tot])
                else:
                    pv = PO[hf][:].rearrange("p (s d) -> p s d", d=128)
                    if pidx == 1:
                        k.op("act", lambda e, pv=pv, hf=hf, ot=ot: e.copy(out=ot[:, hf * 4:(hf + 1) * 4, :], in_=pv), r=[t_PO[hf]], w=[tot])
                    else:
                        k.op("dve", lambda e, pv=pv, hf=hf, ot=ot, o1=o1: e.tensor_tensor(out=ot[:, hf * 4:(hf + 1) * 4, :], in0=pv, in1=o1[:, hf * 4:(hf + 1) * 4, :], op=ALU.add),
                             r=[t_PO[hf], to1], w=[tot])
            if pidx == 1:
                k.dma("sp", P["o1"][r0:r0 + 128, :].rearrange("p (h d) -> p h d", d=128), ot[:], tot, r=[tot])
                continue
            yield
            xr, txr = XR.next()
            k.dma("sp", xr[:], P["x_tm"][r0:r0 + 128, :], txr, w=[txr])
            ga, tga = GA.next()
            for half in range(2):
                p, tp = PF.next()
                for kc in range(8):
                    k.op("pe", lambda e, p=p, kc=kc, tl=tl, half=half: e.matmul(p[:], lhsT=x[:, kc, 1 + tl * 128:1 + (tl + 1) * 128],
                                                                             rhs=wtm[:, kc, 1024 + half * 512:1024 + (half + 1) * 512], start=(kc == 0), stop=(kc == 7)),
                         r=[tx, tc_, t_wtm[kc]], w=[tp])
                k.op("dve", lambda e, p=p, ga=ga, half=half: e.tensor_tensor(out=ga[:, half * 512:(half + 1) * 512], in0=p[:],
                                                                          in1=btm[:, 1024 + half * 512:1024 + (half + 1) * 512], op=ALU.add), r=[tp, tc_], w=[tga])
                fn = AF.Sigmoid if (layer == 1 and half == 1) else AF.Silu
                k.op("act", lambda e, ga=ga, half=half, fn=fn: e.activation(out=ga[:, half * 512:(half + 1) * 512], in_=ga[:, half * 512:(half + 1) * 512], func=fn),
                     r=[tga], w=[tga])
            yield
            z, tz = ZT.next()
            SQ = z[:].rearrange("p (h d) -> p h d", d=128)
            k.op("pool", lambda e, ot=ot, SQ=SQ: e.tensor_tensor(out=SQ, in0=ot[:], in1=ot[:], op=ALU.mult), r=[tot], w=[tz])
            k.op("dve", lambda e, SQ=SQ: e.tensor_reduce(out=ssq[:], in_=SQ, axis=AX.X, op=ALU.add), r=[tz], w=[t_ssq])
            k.op("act", lambda e: e.activation(out=ssq[:], in_=ssq[:], func=AF.Ln, bias=NORM_EPS, scale=1.0 / 128.0), r=[t_ssq], w=[t_ssq])
            k.op("act", lambda e: e.activation(out=ssq[:], in_=ssq[:], func=AF.Exp, scale=-0.5), r=[t_ssq], w=[t_ssq])
            k.op("dve", lambda e, ot=ot: e.tensor_tensor(out=ot[:], in0=ot[:], in1=ssq[:].broadcast_to([128, 8, 128]), op=ALU.mult), r=[tot, t_ssq], w=[tot])
            otf = ot[:].rearrange("p h d -> p (h d)")
            k.op("pool", lambda e, otf=otf: e.tensor_tensor(out=otf, in0=otf, in1=gnb[:], op=ALU.mult), r=[tot, tc_], w=[tot])
            yield
            mx, tmx = MX.next()
            k.op("dve", lambda e, otf=otf, mx=mx, ga=ga: e.tensor_tensor(out=mx[:], in0=otf, in1=ga[:], op=ALU.mult), r=[tot, tga], w=[tmx])
            p, tp = PF.next()
            pb = p[:].bitcast(BF16).rearrange("p (s d) -> p s d", d=128)
            for kc in range(8):
                k.op("pe", lambda e, pb=pb, kc=kc, mx=mx: e.transpose(pb[:, kc, :], mx[:, kc * 128:(kc + 1) * 128], c["idb"][:]), r=[tmx, c["t_id"]], w=[tp])
            mt, tmt = MXT.next()
            k.op("act", lambda e, mt=mt, pb=pb: e.copy(out=mt[:], in_=pb), r=[tp], w=[tmt])
            for half in range(2):
                p, tp = PF.next()
                for kc in range(8):
                    k.op("pe", lambda e, p=p, kc=kc, mt=mt, half=half: e.matmul(p[:], lhsT=mt[:, kc, :], rhs=wout[:, kc, half * 512:(half + 1) * 512],
                                                                             start=(kc == 0), stop=(kc == 7)), r=[tmt, tc_, t_wout[kc]], w=[tp])
                k.op("dve", lambda e, p=p, z=z, xr=xr, half=half: e.scalar_tensor_tensor(out=z[:, half * 512:(half + 1) * 512], in0=xr[:, half * 512:(half + 1) * 512],
                                                                                      scalar=ALPHA, in1=p[:], op0=ALU.mult, op1=ALU.add), r=[tp, txr], w=[tz])
            ho, tho = HO.next()
            emit_ln(k, z, tz, lgb, lbb, tc_, lst, t_lst, ho, tho)
            k.dma("sp", P["h_out"][r0:r0 + 128, :], ho[:], tho, r=[tho])
        yield

    def drive(gens):
        gens = list(gens)
        while gens:
            for g in list(gens):
                try:
                    next(g)
                except StopIteration:
                    gens.remove(g)

    drive([gm_block(blocks[0], 0)])
    for bi, b in enumerate(blocks):
        gens = [scan_block(b, bi % NPAR)]
        if NPAR == 1:
            drive(gens)
            gens = []
        if bi + 1 < len(blocks):
            gens.append(gm_block(blocks[bi + 1], (bi + 1) % NPAR))
        drive(gens)
    if pidx == 1:
        for i in range(nsg):
            k.dma("sp", P["state_out"][i], S32[i][:], t_S[i], r=[t_S[i]])
    k.end_scope()


AB_OFF = dict(qa=0, fa0=512, fa1=1024, ia=1536, ga=2048, qb=2560, kb=2816, vb=3072, gb=3584, rb0=4096, rb1=4112)
CD_OFF = dict(qc=0, kc=256, vc=512, gc=1024, qd=1536, kd=2048, vd=2560, od=3072, gates=3584)


def _ar(a, n):
    return np.arange(a, a + n)


def prep_mixer(inp, layer, d, flipped, pos, NT):
    f32 = np.float32
    fm, sgs = mixer_layout(layer)
    out = {}
    if layer == 0:
        w, b = inp["ab_w_in"][0], inp["ab_b_in"][0]
        cols = np.concatenate([_ar(AB_OFF["qa"], 512), _ar(AB_OFF["fa%d" % d], 512), _ar(AB_OFF["qb"], 256), _ar(AB_OFF["kb"], 256),
                               _ar(AB_OFF["rb%d" % d], 16)])
        tcols = np.concatenate([_ar(AB_OFF["ia"], 512), _ar(AB_OFF["vb"], 512), _ar(AB_OFF["ga"], 512), _ar(AB_OFF["gb"], 512)])
        out["hgrn_lb"] = np.ascontiguousarray(inp["hgrn_lb"].reshape(3, 4, 128).transpose(2, 1, 0)).astype(f32)
        out["gk_up"] = np.ascontiguousarray(inp["gla_gk_up"][0, d]).astype(f32)
        out["gk_b"] = np.ascontiguousarray(inp["gla_gk_b"][0, d].reshape(4, 64).T).astype(f32)
        out["norm_g"] = np.concatenate([inp["hgrn_norm"][0], inp["gla_norm"][0]])[None].astype(f32)
        out["w_out"] = np.ascontiguousarray(inp["ab_w_out"][0]).astype(f32)
    else:
        w, b = inp["cd_w_in"][0], inp["cd_b_in"][0]

        def sw(base):
            return np.concatenate([np.concatenate([_ar(base + h * 64 + 32, 32), _ar(base + h * 64, 32)]) for h in range(4)])
        g0 = CD_OFF["gates"]
        cols = np.concatenate([_ar(CD_OFF["qc"], 256), sw(CD_OFF["qc"]), _ar(CD_OFF["kc"], 256), sw(CD_OFF["kc"]),
                               _ar(CD_OFF["qd"], 512), _ar(CD_OFF["kd"], 512), _ar(g0 + 8 * d, 8)])
        tcols = np.concatenate([_ar(CD_OFF["vc"], 512), _ar(CD_OFF["vd"], 512), _ar(CD_OFF["gc"], 512), _ar(CD_OFF["od"], 512)])
        exps = 5.0 + 2.0 * np.arange(4, dtype=f32) + f32(d)
        lg = np.log1p(-np.exp2(-exps)).astype(f32)
        out["lgam"] = np.ascontiguousarray(np.broadcast_to(lg[None, :], (64, 4))).astype(f32)
        cwt = inp["mlstm_conv_w"][0]
        if flipped:
            cwt = cwt[::-1]
        out["conv_w"] = np.ascontiguousarray(cwt.reshape(3, 8, 128).transpose(2, 1, 0)).astype(f32)
        out["conv_b"] = np.ascontiguousarray(inp["mlstm_conv_b"][0].reshape(8, 128).T).astype(f32)
        sel = np.zeros((8, 8, 128), f32)
        for r in range(8):
            sel[r, r, :] = 1.0
        out["selm"] = sel
        out["fgb"] = np.ascontiguousarray(inp["mlstm_fgate_b"][0, d])[None].astype(f32)
        out["lib"] = np.ascontiguousarray(b[g0 + 8 * d: g0 + 8 * d + 4])[None].astype(f32)
        out["lfb"] = np.ascontiguousarray(b[g0 + 8 * d + 4: g0 + 8 * d + 8])[None].astype(f32)
        half = 32
        freqs = (f32(10000.0) ** (-np.arange(half, dtype=f32) / f32(half))).astype(f32)
        ang = (pos.astype(f32)[:, None] * freqs[None, :]).astype(f32)
        cosv, sinv = np.cos(ang).astype(f32), np.sin(ang).astype(f32)
        cs = np.zeros((2, 64, NT), f32)
        cs[0, :32], cs[0, 32:] = cosv.T, cosv.T
        cs[1, :32], cs[1, 32:] = -sinv.T, sinv.T
        out["cs_tab"] = cs
        out["norm_g"] = np.concatenate([inp["ret_norm"][0], inp["mlstm_norm"][0]])[None].astype(f32)
        out["w_out"] = np.ascontiguousarray(inp["cd_w_out"][0]).astype(f32)
    out["wfm"] = np.ascontiguousarray(w[:, cols]).astype(f32)
    bf = np.zeros((128, len(fm)), f32)
    o = 0
    for gi, (nm, wd) in enumerate(fm):
        bf[:wd, gi] = b[cols[o:o + wd]]
        o += wd
    out["bfm"] = bf
    out["wtm"] = np.ascontiguousarray(w[:, tcols]).astype(f32)
    out["btm"] = np.ascontiguousarray(b[tcols])[None].astype(f32)
    out["ln_g"] = np.ascontiguousarray(inp["ln_mix_g"][layer])[None].astype(f32)
    out["ln_b"] = np.ascontiguousarray(inp["ln_mix_b"][layer])[None].astype(f32)
    return out


MIX_SHAPES = {
    0: dict(hgrn_lb=[128, 4, 3], gk_up=[16, 256], gk_b=[64, 4]),
    1: dict(lgam=[64, 4], conv_w=[128, 8, 3], conv_b=[128, 8], selm=[8, 8, 128], fgb=[1, 4], lib=[1, 4], lfb=[1, 4]),
}


def declare_mixer_inputs(nc, layer, pidx, NT, pre):
    fm, sgs = mixer_layout(layer)
    CF = sum(wd for _, wd in fm)
    P = {}

    def inp(nm, shape):
        P[nm] = nc.dram_tensor(pre + nm, list(shape), F32, kind="ExternalInput").ap()
    inp("wfm", [D, CF])
    inp("bfm", [128, len(fm)])
    inp("wtm", [D, 2048])
    inp("btm", [1, 2048])
    for nm, shp in MIX_SHAPES[layer].items():
        inp(nm, shp)
    if layer == 1:
        inp("cs_tab", [2, 64, NT])
    if pidx == 2:
        inp("norm_g", [1, D])
        inp("w_out", [D, D])
        inp("ln_g", [1, D])
        inp("ln_b", [1, D])
    return P


def build_p1(layer, NT):
    nc = bass.Bass("TRN2", target_bir_lowering=False)
    h_in = nc.dram_tensor("h_in", [NT, D], F32, kind="ExternalInput").ap()
    P = declare_mixer_inputs(nc, layer, 1, NT, "m_")
    if layer == 1:
        P["halo"] = nc.dram_tensor("halo", [D], F32, kind="ExternalInput").ap()
    P["o1"] = nc.dram_tensor("o1", [NT, D], F32, kind="ExternalOutput").ap()
    P["state_out"] = nc.dram_tensor("st_out", [8, 128, VP], F32, kind="ExternalOutput").ap()
    xt = nc.dram_tensor("xt_scr", [D, NT + 2], BF16).ap()
    k = KB(nc)
    c = make_consts(k)
    emit_transpose_phase(k, c, h_in, xt, NT)
    emit_mixer_pass(k, c, layer, 1, NT, xt, P)
    k.finish()
    return nc, k


def build_p2(layer, NT, ne=NE, TQ=1024, with_moe=True):
    nc = bass.Bass("TRN2", target_bir_lowering=False)
    h_in = nc.dram_tensor("h_in", [NT, D], F32, kind="ExternalInput").ap()
    P = declare_mixer_inputs(nc, layer, 2, NT, "m_")
    if layer == 1:
        P["halo"] = nc.dram_tensor("halo", [D], F32, kind="ExternalInput").ap()
    P["o1"] = nc.dram_tensor("o1", [NT, D], F32, kind="ExternalInput").ap()
    P["state_in"] = nc.dram_tensor("st_in", [8, 128, VP], F32, kind="ExternalInput").ap()
    P["x_tm"] = h_in
    h_out = nc.dram_tensor("h_out", [NT, D], F32, kind="ExternalOutput").ap()
    xt = nc.dram_tensor("xt_scr", [D, NT + 2], BF16).ap()
    k = KB(nc)
    c = make_consts(k)
    emit_transpose_phase(k, c, h_in, xt, NT)
    if with_moe:
        h1 = nc.dram_tensor("h1_scr", [NT, D], F32).ap()
        P["h_out"] = h1
        w1 = nc.dram_tensor("w1", [ne, D, D], F32, kind="ExternalInput").ap()
        w3 = nc.dram_tensor("w3", [ne, D, D], F32, kind="ExternalInput").ap()
        w2 = nc.dram_tensor("w2", [ne, D, D], F32, kind="ExternalInput").ap()
        rw = nc.dram_tensor("rw", [D, NE], F32, kind="ExternalInput").ap()
        rb = nc.dram_tensor("rb", [1, NE], F32, kind="ExternalInput").ap()
        fg = nc.dram_tensor("fg", [1, D], F32, kind="ExternalInput").ap()
        fb = nc.dram_tensor("fb", [1, D], F32, kind="ExternalInput").ap()
        emit_mixer_pass(k, c, layer, 2, NT, xt, P)
        emit_moe_phase(k, c, h1, h_out, w1, w3, w2, rw, rb, fg, fb, NT, TQ=min(TQ, NT), ne=ne)
    else:
        P["h_out"] = h_out
        emit_mixer_pass(k, c, layer, 2, NT, xt, P)
    k.finish()
    return nc, k


def core_rows(inp_x, NT):
    B, S, _ = inp_x.shape
    rows, poss = [], []
    for b in range(B):
        rows.append(np.ascontiguousarray(inp_x[b, :NT]))
        poss.append(np.arange(NT))
        rows.append(np.ascontiguousarray(inp_x[b, S - 1:NT - 1:-1]))
        poss.append(S - 1 - np.arange(NT))
    return rows, poss


def run_layers(inputs, NT, ne=NE, layers=(0, 1), with_moe=True, runner=None):
    x = np.asarray(inputs["x"], np.float32)
    B, S, _ = x.shape
    ncores = 2 * B
    inp = {k_: np.asarray(v, np.float32) for k_, v in inputs.items()}
    h, poss = core_rows(x, NT)
    run = runner or (lambda nc, maps: run_bass_kernel_spmd(nc, maps, core_ids=list(range(ncores))).results)
    for layer in layers:
        halos = [h[c ^ 1][NT - 1].copy() for c in range(ncores)]
        nc1, _ = build_p1(layer, NT)
        maps = []
        for c in range(ncores):
            s = c % 2
            m = {"m_" + k_: v for k_, v in prep_mixer(inp, layer, s, s == 1, poss[c], NT).items()
                 if k_ not in ("norm_g", "w_out", "ln_g", "ln_b")}
            m["h_in"] = h[c]
            if layer == 1:
                m["halo"] = halos[c]
            maps.append(m)
        r1 = run(nc1, maps)
        run_layers.dbg.setdefault("r1", []).append(r1)
        nc2, _ = build_p2(layer, NT, ne=ne, with_moe=with_moe)
        maps = []
        for c in range(ncores):
            s = c % 2
            m = {"m_" + k_: v for k_, v in prep_mixer(inp, layer, 1 - s, s == 1, poss[c], NT).items()}
            m["h_in"] = h[c]
            if layer == 1:
                m["halo"] = halos[c]
            m["o1"] = r1[c]["o1"]
            m["st_in"] = r1[c ^ 1]["st_out"]
            if with_moe:
                m["w1"] = inp["moe_w1"][layer][:ne]
                m["w3"] = inp["moe_w3"][layer][:ne]
                m["w2"] = inp["moe_w2"][layer][:ne]
                m["rw"] = inp["router_w"]
                m["rb"] = inp["router_b"][None]
                m["fg"] = inp["ln_ffn_g"][layer][None]
                m["fb"] = inp["ln_ffn_b"][layer][None]
            maps.append(m)
        r2 = run(nc2, maps)
        h = [np.asarray(r2[c]["h_out"]) for c in range(ncores)]
        run_layers.dbg.setdefault("h", []).append(h)
    out = np.zeros((B, S, D), np.float32)
    for b in range(B):
        out[b, :NT] = h[2 * b]
        out[b, S - 1:NT - 1:-1] = h[2 * b + 1]
    return out


PAIRS = [[0, 1], [2, 3], [4, 5], [6, 7]]


def build_fused(NT, ne=NE, TQ=1024, groups=PAIRS, debug=False):
    nc = bass.Bass("TRN2", target_bir_lowering=False)
    x_in = nc.dram_tensor("x_in", [NT, D], F32, kind="ExternalInput").ap()
    sel = nc.dram_tensor("sel", [1, 2], F32, kind="ExternalInput").ap()
    out = nc.dram_tensor("out", [NT, D], F32, kind="ExternalOutput").ap()
    rw = nc.dram_tensor("rw", [D, NE], F32, kind="ExternalInput").ap()
    rb = nc.dram_tensor("rb", [1, NE], F32, kind="ExternalInput").ap()
    xt = nc.dram_tensor("xt_scr", [D, NT + 2], BF16).ap()
    h1 = nc.dram_tensor("h1_scr", [NT, D], F32).ap()
    hA = nc.dram_tensor("hA_scr", [NT, D], F32).ap()
    o1 = nc.dram_tensor("o1_scr", [NT, D], F32).ap()
    st_mine = nc.dram_tensor("st_mine", [8 * 128, VP], F32)
    st_pair = nc.dram_tensor("st_pair", [2 * 8 * 128, VP], F32)
    hl_mine = nc.dram_tensor("hl_mine", [1, D], F32)
    hl_pair = nc.dram_tensor("hl_pair", [2, D], F32)
    k = KB(nc)
    c = make_consts(k)
    for _ in range(globals().get("SALT", 0)):
        k.op("pool", lambda e: e.memset(c["idb"][0:1, 0:1], 1.0), r=[c["t_id"]], w=[c["t_id"]])
    t_cc = Tok("cc")
    t_hl = Tok("hl")
    h_cur = x_in
    for layer in range(2):
        h_nxt = hA if layer == 0 else out
        P1 = declare_mixer_inputs(nc, layer, 1, NT, "m%d1_" % layer)
        P2 = declare_mixer_inputs(nc, layer, 2, NT, "m%d2_" % layer)
        for P in (P1, P2):
            P["sel"] = sel
            P["o1"] = o1
            P["cc_tok"] = [t_cc]
            if layer == 1:
                P["halo_pair"] = hl_pair.ap()
        P1["state_out"] = st_mine.ap().rearrange("(s p) v -> s p v", p=128)
        P2["state_pair"] = st_pair.ap().rearrange("(r s p) v -> r s p v", r=2, p=128)
        P2["x_tm"] = h_cur
        P2["h_out"] = h1
        w1 = nc.dram_tensor("w1_%d" % layer, [ne, D, D], F32, kind="ExternalInput").ap()
        w3 = nc.dram_tensor("w3_%d" % layer, [ne, D, D], F32, kind="ExternalInput").ap()
        w2 = nc.dram_tensor("w2_%d" % layer, [ne, D, D], F32, kind="ExternalInput").ap()
        fg = nc.dram_tensor("fg_%d" % layer, [1, D], F32, kind="ExternalInput").ap()
        fb = nc.dram_tensor("fb_%d" % layer, [1, D], F32, kind="ExternalInput").ap()
        emit_transpose_phase(k, c, h_cur, xt, NT)
        emit_mixer_pass(k, c, layer, 1, NT, xt, P1)
        k.coll("AllGather", [st_mine.ap().opt()], [st_pair.ap().opt()], groups, t_cc, w=[t_cc])
        if debug and layer == 0:
            dbg = nc.dram_tensor("dbg_st", [2 * 8 * 128, VP], F32, kind="ExternalOutput").ap()
            dbg2 = nc.dram_tensor("dbg_o1", [NT, D], F32, kind="ExternalOutput").ap()
            td = Tok("dbg")
            k.dma("sp", dbg, st_pair.ap(), td, r=[t_cc], w=[td])
            k.dma("sp", dbg2, o1, td, w=[td])
        emit_mixer_pass(k, c, layer, 2, NT, xt, P2)
        if debug and layer == 0:
            dbg3 = nc.dram_tensor("dbg_h1", [NT, D], F32, kind="ExternalOutput").ap()
            k.dma("sp", dbg3, h1, td, w=[td])
        emit_moe_phase(k, c, h1, h_nxt, w1, w3, w2, rw, rb, fg, fb, NT, TQ=min(TQ, NT), ne=ne)
        if debug and layer == 0:
            dbg4 = nc.dram_tensor("dbg_hA", [NT, D], F32, kind="ExternalOutput").ap()
            k.dma("sp", dbg4, hA, td, w=[td])
        if layer == 0:
            k.dma("sp", hl_mine.ap(), hA[NT - 1:NT, :], t_hl, w=[t_hl])
            k.coll("AllGather", [hl_mine.ap().opt()], [hl_pair.ap().opt()], groups, t_cc, r=[t_hl], w=[t_cc])
        h_cur = h_nxt
    if globals().get("LATE_DEBUG", 0):
        td = Tok("dbgl")
        for nm, src in (("dbg_hA", hA), ("dbg_h1", h1), ("dbg_o1", o1)):
            dd = nc.dram_tensor(nm, [NT, D], F32, kind="ExternalOutput").ap()
            k.dma("sp", dd, src, td, w=[td])
        dd = nc.dram_tensor("dbg_st", [2 * 8 * 128, VP], F32, kind="ExternalOutput").ap()
        k.dma("sp", dd, st_pair.ap(), td, w=[td])
        dd = nc.dram_tensor("dbg_hl", [2, D], F32, kind="ExternalOutput").ap()
        k.dma("sp", dd, hl_pair.ap(), td, w=[td])
    k.finish()
    return nc, k


def fused_maps(inputs, NT, ne=NE):
    x = np.asarray(inputs["x"], np.float32)
    B, S, _ = x.shape
    ncores = 2 * B
    inp = {k_: np.asarray(v, np.float32) for k_, v in inputs.items()}
    h, poss = core_rows(x, NT)
    maps = []
    shared = {"rw": inp["router_w"], "rb": inp["router_b"][None]}
    for layer in range(2):
        shared["w1_%d" % layer] = inp["moe_w1"][layer][:ne]
        shared["w3_%d" % layer] = inp["moe_w3"][layer][:ne]
        shared["w2_%d" % layer] = inp["moe_w2"][layer][:ne]
        shared["fg_%d" % layer] = inp["ln_ffn_g"][layer][None]
        shared["fb_%d" % layer] = inp["ln_ffn_b"][layer][None]
    for c in range(ncores):
        s = c % 2
        m = dict(shared)
        m["x_in"] = h[c]
        m["sel"] = np.array([[1.0, 0.0]] if s == 1 else [[0.0, 1.0]], np.float32)
        for layer in range(2):
            for pidx, d in ((1, s), (2, 1 - s)):
                pm = prep_mixer(inp, layer, d, s == 1, poss[c], NT)
                for k_, v in pm.items():
                    if pidx == 1 and k_ in ("norm_g", "w_out", "ln_g", "ln_b"):
                        continue
                    m["m%d%d_%s" % (layer, pidx, k_)] = v
        maps.append(m)
    return maps, B, S


def run_fused(inputs, NT, ne=NE, TQ=1024, debug=False):
    maps, B, S = fused_maps(inputs, NT, ne)
    ncores = 2 * B
    nc, _ = build_fused(NT, ne=ne, TQ=TQ, groups=[[2 * b, 2 * b + 1] for b in range(B)], debug=debug)
    res = run_bass_kernel_spmd(nc, maps, core_ids=list(range(ncores))).results
    if debug:
        run_fused.dbg = res
    out = np.zeros((B, S, D), np.float32)
    for b in range(B):
        out[b, :NT] = res[2 * b]["out"]
        out[b, S - 1:NT - 1:-1] = res[2 * b + 1]["out"]
    return out


run_layers.dbg = {}


def kernel(**inputs):
    return run_fused(inputs, 4096)
```
